# Optimizing a Trainium2 kernel written in Bass

```python
import jax, jax.numpy as jnp
from jax import lax
import numpy as np

D_MODEL = 1024
BATCH = 4
SEQ = 8192
DEPTH = 1

CHUNK = 64
Q_BLOCK = 128
NORM_EPS = 1e-6
LRU_WIDTH = D_MODEL
LRU_BLOCKS = 8
LRU_BLOCK_DIM = LRU_WIDTH // LRU_BLOCKS
CONV_WIDTH = 4
LRU_C = 8.0
HEAD_DIM = 128
N_HEADS = D_MODEL // HEAD_DIM
N_KV_HEADS = max(1, N_HEADS // 4)
IDX_HEADS = 8
IDX_DIM = 64
MAX_TOPK = 256
ROPE_THETA = 500000.0
ROT_FRACTION = 4
N_GROUPS = 4
EXPERTS_PER_GROUP = 8
N_EXPERTS = N_GROUPS * EXPERTS_PER_GROUP
TOP_K_EXPERTS = 2
EXPERT_FF = D_MODEL // 2
MOE_BLOCK = 256
COL_SIZES = (LRU_WIDTH, LRU_WIDTH, N_HEADS * HEAD_DIM, N_KV_HEADS * HEAD_DIM, N_KV_HEADS * HEAD_DIM,
             IDX_HEADS * IDX_DIM, IDX_DIM, IDX_HEADS, 2 * D_MODEL)
IN_COLS = 2 * LRU_WIDTH + (N_HEADS + 2 * N_KV_HEADS) * HEAD_DIM + IDX_HEADS * IDX_DIM + IDX_DIM + IDX_HEADS + 2 * D_MODEL

kernel_name = "hybrid_rglru_dsa_hiermoe_block"


def rmsnorm(x, g):
    xf = x.astype(jnp.float32)
    y = xf * lax.rsqrt(jnp.mean(xf * xf, axis=-1, keepdims=True) + NORM_EPS)
    return (y * g.astype(jnp.float32)).astype(x.dtype)


def split_columns(proj):
    offsets = np.cumsum((0,) + COL_SIZES)
    return tuple(proj[..., int(offsets[i]):int(offsets[i + 1])] for i in range(len(COL_SIZES)))


def rope_tables(positions, rot_dim, dtype):
    inv_freq = ROPE_THETA ** (-jnp.arange(0, rot_dim, 2, dtype=jnp.float32) / rot_dim)
    ang = positions.astype(jnp.float32)[..., None] * inv_freq
    return jnp.cos(ang)[:, :, None, :].astype(dtype), jnp.sin(ang)[:, :, None, :].astype(dtype)


def partial_rope(x, cos, sin):
    half = cos.shape[-1]
    x1, x2, rest = x[..., :half], x[..., half:2 * half], x[..., 2 * half:]
    return jnp.concatenate([x1 * cos - x2 * sin, x2 * cos + x1 * sin, rest], axis=-1)


def causal_depthwise_conv(x, w, b):
    S = x.shape[1]
    xp = jnp.pad(x, ((0, 0), (CONV_WIDTH - 1, 0), (0, 0)))
    y = b
    for k in range(CONV_WIDTH):
        y = y + xp[:, k:k + S] * w[k]
    return y


def rg_lru(x, w_r, b_r, w_i, b_i, a_param):
    B, S, W = x.shape
    xb = x.reshape(B, S, LRU_BLOCKS, LRU_BLOCK_DIM)
    r = jax.nn.sigmoid(jnp.einsum('bsnc,ncd->bsnd', xb, w_r) + b_r).reshape(B, S, W)
    i = jax.nn.sigmoid(jnp.einsum('bsnc,ncd->bsnd', xb, w_i) + b_i).reshape(B, S, W)
    log_a = -LRU_C * r.astype(jnp.float32) * jax.nn.softplus(-a_param.astype(jnp.float32))
    a = jnp.exp(log_a)
    u = jnp.sqrt(-jnp.expm1(2.0 * log_a)) * (i * x).astype(jnp.float32)

    def combine(left, right):
        a1, b1 = left
        a2, b2 = right
        return a1 * a2, a2 * b1 + b2

    _, h = lax.associative_scan(combine, (a, u), axis=1)
    return h.astype(x.dtype)


def dsa_attention(q, k, v, q_idx, k_idx, w_idx):
    B, S = q.shape[:2]
    topk = min(MAX_TOPK, S // 4)
    n_blocks = S // Q_BLOCK
    key_chunk = jnp.arange(S) // CHUNK
    k_idx32 = k_idx.astype(jnp.float32)
    gather = jax.vmap(lambda t, ix: t[ix])

    def to_blocks(t):
        return t.reshape((B, n_blocks, Q_BLOCK) + t.shape[2:]).swapaxes(0, 1)

    def one_block(args):
        qb, qib, wb, blk = args
        qpos = blk * Q_BLOCK + jnp.arange(Q_BLOCK)
        rel = jax.nn.relu(jnp.einsum('bthd,bsd->bths', qib.astype(jnp.float32), k_idx32))
        score = jnp.einsum('bth,bths->bts', wb.astype(jnp.float32), rel)
        admissible = key_chunk[None, :] <= (qpos // CHUNK)[:, None]
        score = jnp.where(admissible[None], score, -jnp.inf)
        top_val, top_idx = lax.top_k(score, topk)
        valid = jnp.isfinite(top_val)
        k_sel = gather(k, top_idx)
        v_sel = gather(v, top_idx)
        qg = qb.reshape(B, Q_BLOCK, N_KV_HEADS, N_HEADS // N_KV_HEADS, HEAD_DIM)
        s = jnp.einsum('btjgd,btnjd->btjgn', qg, k_sel).astype(jnp.float32) * (HEAD_DIM ** -0.5)
        s = jnp.where(valid[:, :, None, None, :], s, -1e30)
        p = jax.nn.softmax(s, axis=-1).astype(v.dtype)
        o = jnp.einsum('btjgn,btnjd->btjgd', p, v_sel)
        return o.reshape(B, Q_BLOCK, N_HEADS * HEAD_DIM)

    out = lax.map(one_block, (to_blocks(q), to_blocks(q_idx), to_blocks(w_idx), jnp.arange(n_blocks)))
    return out.swapaxes(0, 1).reshape(B, S, N_HEADS * HEAD_DIM)


def hierarchical_moe(h, w_grp, b_grp, w_exp, b_exp, w13, w2):
    B, S, D = h.shape
    N = B * S
    hf = h.reshape(N, D)
    grp_logits = (hf @ w_grp).astype(jnp.float32) + b_grp
    g_star = jnp.argmax(grp_logits, axis=-1)
    p_grp = jnp.take_along_axis(jax.nn.softmax(grp_logits, axis=-1), g_star[:, None], axis=-1)
    exp_logits = ((hf @ w_exp).astype(jnp.float32) + b_exp).reshape(N, N_GROUPS, EXPERTS_PER_GROUP)
    in_grp = jnp.take_along_axis(exp_logits, g_star[:, None, None], axis=1)[:, 0]
    top_val, top_j = lax.top_k(in_grp, TOP_K_EXPERTS)
    gate = (p_grp * jax.nn.softmax(top_val, axis=-1)).reshape(-1)
    expert_id = (g_star[:, None] * EXPERTS_PER_GROUP + top_j).reshape(-1).astype(jnp.int32)
    token_id = jnp.repeat(jnp.arange(N, dtype=jnp.int32), TOP_K_EXPERTS)
    n_assign = N * TOP_K_EXPERTS
    order = jnp.argsort(expert_id)
    sorted_e, sorted_tok, sorted_gate = expert_id[order], token_id[order], gate[order]
    counts = jnp.bincount(expert_id, length=N_EXPERTS)
    starts = jnp.cumsum(counts) - counts
    pad_counts = (counts + MOE_BLOCK - 1) // MOE_BLOCK * MOE_BLOCK
    pad_ends = jnp.cumsum(pad_counts)
    pad_starts = pad_ends - pad_counts
    dest = pad_starts[sorted_e] + jnp.arange(n_assign, dtype=jnp.int32) - starts[sorted_e]
    n_slots = (-(-n_assign // MOE_BLOCK) + N_EXPERTS) * MOE_BLOCK
    n_blocks = n_slots // MOE_BLOCK
    slot_tok = jnp.full((n_slots,), N, dtype=jnp.int32).at[dest].set(sorted_tok)
    slot_gate = jnp.zeros((n_slots,), jnp.float32).at[dest].set(sorted_gate)
    block_e = jnp.minimum(jnp.searchsorted(pad_ends, jnp.arange(n_blocks) * MOE_BLOCK, side='right'),
                          N_EXPERTS - 1)
    h_pad = jnp.concatenate([hf, jnp.zeros((1, D), hf.dtype)], axis=0)
    xb = h_pad[slot_tok].reshape(n_blocks, MOE_BLOCK, D)

    def expert_block(args):
        xblk, e = args
        gu = xblk @ w13[e]
        g, u = jnp.split(gu, 2, axis=-1)
        return (jax.nn.silu(g) * u) @ w2[e]

    y = lax.map(expert_block, (xb, block_e)).reshape(n_slots, D)
    y = y * slot_gate[:, None].astype(y.dtype)
    return jax.ops.segment_sum(y, slot_tok, num_segments=N + 1)[:N].reshape(B, S, D)


def hybrid_layer(x, cos, sin, cos_i, sin_i, norm1_g, w_in, conv_w, conv_b, lru_w_r, lru_b_r, lru_w_i,
                 lru_b_i, lru_a_param, q_norm_g, k_norm_g, idx_k_norm_g, w_lru_out, w_attn_out, w_o,
                 norm2_g, w_grp, b_grp, w_exp, b_exp, w13, w2):
    B, S, _ = x.shape
    h = rmsnorm(x, norm1_g)
    lru_x, lru_gate, q, k, v, q_idx, k_idx, w_idx, merge = split_columns(h @ w_in)
    xa = causal_depthwise_conv(lru_x, conv_w, conv_b)
    ya = rg_lru(xa, lru_w_r, lru_b_r, lru_w_i, lru_b_i, lru_a_param) * jax.nn.gelu(lru_gate)
    q = partial_rope(rmsnorm(q.reshape(B, S, N_HEADS, HEAD_DIM), q_norm_g), cos, sin)
    k = partial_rope(rmsnorm(k.reshape(B, S, N_KV_HEADS, HEAD_DIM), k_norm_g), cos, sin)
    v = v.reshape(B, S, N_KV_HEADS, HEAD_DIM)
    q_idx = partial_rope(q_idx.reshape(B, S, IDX_HEADS, IDX_DIM), cos_i, sin_i)
    k_idx = partial_rope(rmsnorm(k_idx, idx_k_norm_g)[:, :, None, :], cos_i, sin_i)[:, :, 0]
    w_idx = w_idx * ((IDX_HEADS ** -0.5) * (IDX_DIM ** -0.5))
    yb = dsa_attention(q, k, v, q_idx, k_idx, w_idx)
    gate_a, gate_b = jnp.split(jax.nn.sigmoid(merge), 2, axis=-1)
    mixed = gate_a * (ya @ w_lru_out) + gate_b * (yb @ w_attn_out)
    x = x + mixed @ w_o
    return x + hierarchical_moe(rmsnorm(x, norm2_g), w_grp, b_grp, w_exp, b_exp, w13, w2)


def setup_inputs(seed: int = 0) -> dict:
    key = jax.random.key(seed)
    ks = jax.random.split(key, 26)
    f32 = jnp.float32
    nrm = lambda k, shape, scale: jax.random.normal(k, shape, f32) * scale
    u = jax.random.uniform(ks[9], (DEPTH, LRU_WIDTH), f32, minval=0.9, maxval=0.999)
    a0 = u ** (1.0 / LRU_C)
    offsets = jax.random.randint(ks[1], (BATCH, 1), 0, 4096, dtype=jnp.int32)
    return {
        "x": nrm(ks[0], (BATCH, SEQ, D_MODEL), 1.0),
        "positions": offsets + jnp.arange(SEQ, dtype=jnp.int32)[None, :],
        "norm1_g": 1.0 + nrm(ks[2], (DEPTH, D_MODEL), 0.02),
        "w_in": nrm(ks[3], (DEPTH, D_MODEL, IN_COLS), D_MODEL ** -0.5),
        "conv_w": nrm(ks[4], (DEPTH, CONV_WIDTH, LRU_WIDTH), CONV_WIDTH ** -0.5),
        "conv_b": nrm(ks[5], (DEPTH, LRU_WIDTH), 0.01),
        "lru_w_r": nrm(ks[6], (DEPTH, LRU_BLOCKS, LRU_BLOCK_DIM, LRU_BLOCK_DIM), LRU_BLOCK_DIM ** -0.5),
        "lru_b_r": nrm(ks[7], (DEPTH, LRU_BLOCKS, LRU_BLOCK_DIM), 0.01),
        "lru_w_i": nrm(ks[8], (DEPTH, LRU_BLOCKS, LRU_BLOCK_DIM, LRU_BLOCK_DIM), LRU_BLOCK_DIM ** -0.5),
        "lru_b_i": nrm(ks[10], (DEPTH, LRU_BLOCKS, LRU_BLOCK_DIM), 0.01),
        "lru_a_param": jnp.log(a0) - jnp.log1p(-a0),
        "q_norm_g": 1.0 + nrm(ks[11], (DEPTH, HEAD_DIM), 0.02),
        "k_norm_g": 1.0 + nrm(ks[12], (DEPTH, HEAD_DIM), 0.02),
        "idx_k_norm_g": 1.0 + nrm(ks[13], (DEPTH, IDX_DIM), 0.02),
        "w_lru_out": nrm(ks[14], (DEPTH, LRU_WIDTH, D_MODEL), LRU_WIDTH ** -0.5),
        "w_attn_out": nrm(ks[15], (DEPTH, N_HEADS * HEAD_DIM, D_MODEL), (N_HEADS * HEAD_DIM) ** -0.5),
        "w_o": nrm(ks[16], (DEPTH, D_MODEL, D_MODEL), D_MODEL ** -0.5),
        "norm2_g": 1.0 + nrm(ks[17], (DEPTH, D_MODEL), 0.02),
        "w_grp": nrm(ks[18], (DEPTH, D_MODEL, N_GROUPS), D_MODEL ** -0.5),
        "b_grp": nrm(ks[19], (DEPTH, N_GROUPS), 0.01),
        "w_exp": nrm(ks[20], (DEPTH, D_MODEL, N_EXPERTS), D_MODEL ** -0.5),
        "b_exp": nrm(ks[21], (DEPTH, N_EXPERTS), 0.01),
        "w13": nrm(ks[22], (DEPTH, N_EXPERTS, D_MODEL, 2 * EXPERT_FF), D_MODEL ** -0.5),
        "w2": nrm(ks[23], (DEPTH, N_EXPERTS, EXPERT_FF, D_MODEL), EXPERT_FF ** -0.5),
    }


def reference(x, positions, norm1_g, w_in, conv_w, conv_b, lru_w_r, lru_b_r, lru_w_i, lru_b_i,
              lru_a_param, q_norm_g, k_norm_g, idx_k_norm_g, w_lru_out, w_attn_out, w_o, norm2_g,
              w_grp, b_grp, w_exp, b_exp, w13, w2):
    cos, sin = rope_tables(positions, HEAD_DIM // ROT_FRACTION, x.dtype)
    cos_i, sin_i = rope_tables(positions, IDX_DIM // ROT_FRACTION, x.dtype)
    for l in range(DEPTH):
        x = hybrid_layer(x, cos, sin, cos_i, sin_i, norm1_g[l], w_in[l], conv_w[l], conv_b[l],
                         lru_w_r[l], lru_b_r[l], lru_w_i[l], lru_b_i[l], lru_a_param[l], q_norm_g[l],
                         k_norm_g[l], idx_k_norm_g[l], w_lru_out[l], w_attn_out[l], w_o[l], norm2_g[l],
                         w_grp[l], b_grp[l], w_exp[l], b_exp[l], w13[l], w2[l])
    return x
```

```python
import numpy as np
from contextlib import ExitStack
import concourse.bass as bass
import concourse.mybir as mybir
from concourse.bass_utils import run_bass_kernel_spmd

F32 = mybir.dt.float32; BF16 = mybir.dt.bfloat16; I32 = mybir.dt.int32
ALU = mybir.AluOpType; AF = mybir.ActivationFunctionType; AX = mybir.AxisListType

D = 1024; S = 8192; SO = 4096; NCOL = 6216
EPS = 1e-6
BIG = 30000.0
TWO_PI = 6.283185307179586
PI_HI = 6.28125
PI_LO = TWO_PI - PI_HI
NBIS = 16
BLK = 512
NS_ = BLK // 128
NBLK = (2 * SO) // BLK + 32
NSLOT = NBLK * BLK
TOPK = 256
NG_DEBUG = 1000
CUT_DEBUG = 99
NQ_DEBUG = 1000
NG2_DEBUG = 1000
NG3_DEBUG = 1000
NB_DEBUG = 1000
HE_CUT = 99


class Sched:
    def __init__(s, nc, es):
        s.nc = nc; s.es = es
        s.sems = {}; s.cnt = {}
        s.ops = {e: [] for e in ("pe", "act", "dve", "pool", "sp")}
        s.waited = {e: {} for e in s.ops}
        s.lw = {}; s.lr = {}
        for e in ("pe", "act", "dve", "pool"):
            s.newsem("E_" + e)
        s.nd = 0

    def newsem(s, name):
        s.sems[name] = s.es.enter_context(s.nc.semaphore(name)); s.cnt[name] = 0
        return name

    def dsem(s):
        s.nd += 1
        return s.newsem("D%d" % s.nd)

    def op(s, eng, fn, r=(), w=(), dsem=None):
        deps = {}
        def add(t):
            if t is None: return
            k, v, e = t
            if deps.get(k, (0, None))[0] < v: deps[k] = (v, e)
        for x in r:
            add(s.lw.get(x))
            if x.startswith("ps"):
                for k, (v, e) in s.lr.get(x, {}).items():
                    if e != eng: add((k, v, e))
        for x in w:
            add(s.lw.get(x))
            for k, (v, e) in s.lr.get(x, {}).items():
                add((k, v, e))
        fns = fn if isinstance(fn, list) else [fn]
        if dsem is None:
            sk = "E_" + eng; s.cnt[sk] += 1; inc = 1
        else:
            sk = dsem; s.cnt[sk] += 16 * len(fns); inc = 16
        tok = (sk, s.cnt[sk], eng)
        wd = s.waited[eng]; waits = []
        for k, (v, e) in deps.items():
            if eng == "pe" and k == "E_pe": continue
            if wd.get(k, 0) >= v: continue
            waits.append((k, v)); wd[k] = v
        for i, f in enumerate(fns):
            s.ops[eng].append((f, waits if i == 0 else [], sk, inc))
        for x in w:
            s.lw[x] = tok; s.lr[x] = {}
        for x in r:
            d = s.lr.setdefault(x, {})
            if d.get(sk, (0, None))[0] < tok[1]: d[sk] = (tok[1], eng)
        return tok

    def barrier(s):
        for eng in s.ops:
            wd = s.waited[eng]; waits = []
            for k, c in s.cnt.items():
                if c > 0 and wd.get(k, 0) < c and not (eng == "pe" and k == "E_pe"):
                    waits.append((k, c)); wd[k] = c
            if waits:
                s.ops[eng].append((None, waits, None, 0))

    def emit(s):
        nc = s.nc
        def replay(eng, e):
            for fn, waits, sk, inc in s.ops[eng]:
                for k, v in waits:
                    e.wait_ge(s.sems[k], v)
                if fn is not None:
                    fn(e).then_inc(s.sems[sk], inc)
        with nc.Block() as block:
            @block.tensor
            def _(e): replay("pe", e)
            @block.scalar
            def _(e): replay("act", e)
            @block.vector
            def _(e): replay("dve", e)
            @block.gpsimd
            def _(e): replay("pool", e)
            @block.sync
            def _(e): replay("sp", e)


def host_consts(par):
    c = {}
    c["ident"] = np.eye(128, dtype=np.float32)
    p = np.arange(128)
    c["triu"] = (p[:, None] < p[None, :]).astype(np.float32)
    c["ones"] = np.ones((128, 128), np.float32)
    rt = np.zeros((128, 128), np.float32)
    for d in range(16):
        rt[d + 16, d] = -1.0; rt[d, d + 16] = 1.0
    c["rtm"] = rt
    ri = np.zeros((128, 128), np.float32)
    for base in (0, 64):
        for d in range(8):
            ri[base + d + 8, base + d] = -1.0; ri[base + d, base + d + 8] = 1.0
    c["rti"] = ri
    c["blkones"] = (p[:, None] // 64 == p[None, :] // 64).astype(np.float32)
    invm = np.zeros((128, 1), np.float32)
    f = (np.float32(500000.0) ** (-np.arange(0, 32, 2, dtype=np.float32) / np.float32(32))).astype(np.float32)
    for d in range(32): invm[d, 0] = f[d % 16]
    invi = np.zeros((128, 1), np.float32)
    fi = (np.float32(500000.0) ** (-np.arange(0, 16, 2, dtype=np.float32) / np.float32(16))).astype(np.float32)
    for base in (0, 64):
        for d in range(16): invi[base + d, 0] = fi[d % 8]
    c["invf"] = np.concatenate([invm, invi], axis=1)
    sel = np.zeros((128, 2), np.float32); sel[:, par] = 1.0
    c["sel"] = sel
    q = np.arange(128); kk = np.arange(256)
    m = np.zeros((128, 256), np.float32)
    if par == 0:
        adm = np.zeros((128, 256), bool)
        adm[:, :128] = (kk[None, :128] // 64) <= (q[:, None] // 64)
    else:
        adm = np.ones((128, 256), bool)
        adm[:, 128:] = (kk[None, :128] // 64) <= (q[:, None] // 64)
    m[~adm] = -BIG
    c["amask"] = m
    c["piota"] = np.arange(128, dtype=np.float32).reshape(128, 1)
    c["blkpos"] = np.tile((np.arange(NBLK, dtype=np.float32) * BLK)[None, :], (128, 1))
    return c


CONST_SHAPES = {"ident": [128, 128], "triu": [128, 128], "ones": [128, 128], "rtm": [128, 128], "rti": [128, 128],
                "blkones": [128, 128], "invf": [128, 2], "sel": [128, 2], "amask": [128, 256], "piota": [128, 1],
                "blkpos": [128, NBLK]}

PARAMS = {"norm1_g": [1, 1024], "w_in": [1, 1024, NCOL], "conv_w": [1, 4, 1024], "conv_b": [1, 1024],
          "lru_w_r": [1, 8, 128, 128], "lru_b_r": [1, 8, 128], "lru_w_i": [1, 8, 128, 128], "lru_b_i": [1, 8, 128],
          "lru_a_param": [1, 1024], "q_norm_g": [1, 128], "k_norm_g": [1, 128], "idx_k_norm_g": [1, 64],
          "w_lru_out": [1, 1024, 1024], "w_attn_out": [1, 1024, 1024], "w_o": [1, 1024, 1024], "norm2_g": [1, 1024],
          "w_grp": [1, 1024, 4], "b_grp": [1, 4], "w_exp": [1, 1024, 32], "b_exp": [1, 32],
          "w13": [1, 32, 1024, 1024], "w2": [1, 32, 512, 1024]}


def build(stage=99, dbg=False):
    nc = bass.Bass("TRN2", target_bir_lowering=False)
    es = ExitStack()
    sc = Sched(nc, es)
    op = sc.op
    I = {}
    I["x_full"] = nc.dram_tensor("x_full", [S, D], F32, kind="ExternalInput").ap()
    I["x_own"] = nc.dram_tensor("x_own", [SO, D], F32, kind="ExternalInput").ap()
    I["pos_full"] = nc.dram_tensor("pos_full", [1, S], I32, kind="ExternalInput").ap()
    I["pos_own"] = nc.dram_tensor("pos_own", [1, SO], I32, kind="ExternalInput").ap()
    for k, shp in PARAMS.items():
        I[k] = nc.dram_tensor(k, shp, F32, kind="ExternalInput").ap()
    for k, shp in CONST_SHAPES.items():
        I["c_" + k] = nc.dram_tensor("c_" + k, shp, F32, kind="ExternalInput").ap()
    out = nc.dram_tensor("out", [SO, D], F32, kind="ExternalOutput").ap()
    def scratch(name, shp, dt):
        if dbg and name in dbg:
            return nc.dram_tensor(name, shp, dt, kind="ExternalOutput").ap()
        return nc.dram_tensor(name, shp, dt).ap()
    hT_d = scratch("hT_d", [8, 128, S], BF16)
    kT_d = scratch("kT_d", [2, 128, S], BF16)
    kiT_d = scratch("kiT_d", [128, S], BF16)
    V_d = scratch("V_d", [S, 256], BF16)
    qT_d = scratch("qT_d", [8, 128, SO], BF16)
    qiT_d = scratch("qiT_d", [4, 128, SO], BF16)
    yaT_d = scratch("yaT_d", [8, 128, SO], BF16)
    ybT_d = scratch("ybT_d", [8, 128, SO], BF16)
    mgT_d = scratch("mgT_d", [16, 128, SO], BF16)
    X1_d = scratch("X1_d", [SO, D], F32)
    H2_d = scratch("H2_d", [SO + 128, D], BF16)
    Y_d = scratch("Y_d", [NSLOT, D], F32)
    ST_d = scratch("ST_d", [NSLOT, 1], I32)
    D_ = {}

    _bregs = {}
    def breg(e, val):
        if val not in _bregs:
            _bregs[val] = e.to_reg(val)
        return _bregs[val]

    def sb(stack, name, shp, dt=F32):
        return stack.enter_context(nc.sbuf_tensor(name, shp, dt))

    PS = [es.enter_context(nc.psum_tensor("ps%d" % i, [128, 512], F32)) for i in range(8)]
    psn = ["ps%d" % i for i in range(8)]
    rot = {"i": 0}
    def nextbank(lo=2, hi=8):
        i = lo + rot["i"] % (hi - lo); rot["i"] += 1
        return PS[i], psn[i]

    C = {}
    cs = sc.dsem()
    TMPC = ("triu", "ones", "rtm", "rti", "blkones")
    CB = {}
    for k in ("ident", "triu", "ones", "rtm", "rti", "blkones"):
        CB[k] = sb(es, "kb_" + k, [128, 128], BF16)
    wq = sb(es, "wq", [128, 32, 8], F32)
    for k, shp in CONST_SHAPES.items():
        if k not in TMPC:
            C[k] = sb(es, "k_" + k, shp, F32)
    with ExitStack() as tst:
        for k in TMPC:
            C[k] = sb(tst, "k_" + k, CONST_SHAPES[k], F32)
        op("sp", [lambda e, k=k: e.dma_start(out=C[k][:], in_=I["c_" + k]) for k in CONST_SHAPES], w=["k_" + k for k in CONST_SHAPES], dsem=cs)
        for k in ("ident", "triu", "ones", "rtm", "rti", "blkones"):
            op("dve", lambda e, k=k: e.tensor_copy(out=CB[k][:], in_=C[k][:]), r=["k_" + k], w=["kb_" + k])
        sc.barrier()

    def dma_T(dst, src_row, n, name=None, sem=None):
        fn = lambda e: e.dma_start(out=dst, in_=src_row.rearrange("(c p) -> p c", p=128), allow_slow_non_contiguous=True)
        if sem is not None:
            op("sp", fn, w=[name], dsem=sem)
        return fn

    def rope_tables(stk, pos_ap, g, ntok, pfx):
        T = {}
        posi = sb(stk, pfx + "posi", [128, ntok], I32)
        posf = sb(stk, pfx + "posf", [128, ntok], F32)
        ang = sb(stk, pfx + "ang", [128, ntok], F32)
        kq = sb(stk, pfx + "kq", [128, ntok], I32)
        kf = sb(stk, pfx + "kf", [128, ntok], F32)
        for nm in ("cm", "sm", "ci", "si"):
            T[nm] = sb(stk, pfx + nm, [128, ntok], F32)
        return dict(T=T, posi=posi, posf=posf, ang=ang, kq=kq, kf=kf, pfx=pfx, sem=sc.dsem(), pos=pos_ap, ntok=ntok)

    def rope_fill(RT_, g):
        T = RT_["T"]; pfx = RT_["pfx"]; ntok = RT_["ntok"]
        posi, posf, ang, kq, kf = RT_["posi"], RT_["posf"], RT_["ang"], RT_["kq"], RT_["kf"]
        src = RT_["pos"][0, g * ntok:(g + 1) * ntok].partition_broadcast(128)
        op("sp", lambda e: e.dma_start(out=posi[:], in_=src), w=[pfx + "posi"], dsem=RT_["sem"])
        op("dve", lambda e: e.tensor_copy(out=posf[:], in_=posi[:]), r=[pfx + "posi"], w=[pfx + "posf"])
        for col, (sn, cn) in enumerate((("sm", "cm"), ("si", "ci"))):
            for nm, shift in ((sn, 0.0), (cn, np.pi / 2)):
                t = T[nm]; tn = pfx + nm
                op("dve", lambda e, col=col, shift=shift, t=t: e.tensor_scalar(out=t[:], in0=posf[:], scalar1=C["invf"][:, col:col + 1],
                                                                              scalar2=float(shift), op0=ALU.mult, op1=ALU.add),
                   r=[pfx + "posf", "k_invf"], w=[tn])
                op("dve", lambda e, t=t: e.tensor_scalar(out=kq[:], in0=t[:], scalar1=float(1.0 / TWO_PI), scalar2=None, op0=ALU.mult),
                   r=[tn], w=[pfx + "kq"])
                op("dve", lambda e: e.tensor_copy(out=kf[:], in_=kq[:]), r=[pfx + "kq"], w=[pfx + "kf"])
                yield
                op("dve", lambda e, t=t: e.scalar_tensor_tensor(out=t[:], in0=kf[:], scalar=float(-PI_HI), in1=t[:], op0=ALU.mult, op1=ALU.add),
                   r=[pfx + "kf", tn], w=[tn])
                op("dve", lambda e, t=t: e.scalar_tensor_tensor(out=t[:], in0=kf[:], scalar=float(-PI_LO), in1=t[:], op0=ALU.mult, op1=ALU.add),
                   r=[pfx + "kf", tn], w=[tn])
                op("dve", lambda e, t=t: e.tensor_scalar(out=t[:], in0=t[:], scalar1=3.1415925, scalar2=-3.1415925, op0=ALU.min, op1=ALU.max),
                   r=[tn], w=[tn])
                yield
        for nm in ("sm", "cm", "si", "ci"):
            op("act", lambda e, t=T[nm]: e.activation(out=t[:], in_=t[:], func=AF.Sin), r=[pfx + nm], w=[pfx + nm])
        yield

    def make_xT(stk, pfx, nslot=2):
        R_ = dict(pfx=pfx, n=0, nslot=nslot)
        R_["xin"] = [sb(stk, pfx + "xin%d" % i, [128, 4, D], BF16) for i in range(nslot)]
        R_["xn"] = [sb(stk, pfx + "xn%d" % i, [128, 4, D], BF16) for i in range(nslot)]
        R_["junk"] = sb(stk, pfx + "junk", [128, D], BF16)
        R_["ss"] = sb(stk, pfx + "ss", [128, 8], F32)
        R_["sems"] = [sc.dsem() for _ in range(nslot)]
        return R_

    def xT_group(R_, x_ap, g, dst, dstname):
        pfx = R_["pfx"]; sl = R_["n"] % R_["nslot"]; R_["n"] += 1
        xin = R_["xin"][sl]; xn = R_["xn"][sl]; ss = R_["ss"]; junk = R_["junk"]
        xinn = pfx + "xin%d" % sl; xnn = pfx + "xn%d" % sl
        src = x_ap[g * 512:(g + 1) * 512, :].rearrange("(t p) d -> p t d", p=128)
        op("pool", [lambda e, t=t: e.dma_start(out=xin[:, t, :], in_=src[:, t, :]) for t in range(4)],
           w=[xinn + "_%d" % t for t in range(4)], dsem=R_["sems"][sl])
        for t in range(4):
            op("act", lambda e, t=t: e.activation(out=junk[:], in_=xin[:, t, :], func=AF.Square, accum_out=ss[:, t:t + 1]),
               r=[xinn + "_%d" % t], w=[pfx + "junk", pfx + "ss%d" % t])
            op("act", lambda e, t=t: e.activation(out=ss[:, 4 + t:5 + t], in_=ss[:, t:t + 1], func=AF.Ln, scale=float(1.0 / D), bias=float(EPS)),
               r=[pfx + "ss%d" % t], w=[pfx + "sr%d" % t])
            op("act", lambda e, t=t: e.activation(out=ss[:, 4 + t:5 + t], in_=ss[:, 4 + t:5 + t], func=AF.Exp, scale=-0.5), r=[pfx + "sr%d" % t], w=[pfx + "sr%d" % t])
            op("dve", lambda e, t=t: e.tensor_scalar(out=xn[:, t, :], in0=xin[:, t, :], scalar1=ss[:, 4 + t:5 + t], scalar2=None, op0=ALU.mult),
               r=[xinn + "_%d" % t, pfx + "sr%d" % t], w=[xnn + "_%d" % t])
            yield
        for cp in range(4):
            bank = PS[cp % 2]; bn = psn[cp % 2]
            bv = bank[:].bitcast(BF16)
            for cc in range(2):
                c = cp * 2 + cc
                for t in range(4):
                    op("pe", lambda e, c=c, t=t, cc=cc, bv=bv: e.transpose(out=bv[:, cc * 512 + t * 128: cc * 512 + (t + 1) * 128],
                                                                      in_=xn[:, t, c * 128:(c + 1) * 128], identity=CB["ident"][:]),
                       r=[xnn + "_%d" % t, "kb_ident"], w=[bn])
            eng = "act" if cp % 2 == 0 else "dve"
            if eng == "act":
                op("act", lambda e, cp=cp, bv=bv: e.activation(out=dst[:, 2 * cp:2 * cp + 2, :], in_=bv.rearrange("p (c n) -> p c n", c=2), func=AF.Copy),
                   r=[bn], w=[dstname])
            else:
                op("dve", lambda e, cp=cp, bv=bv: e.tensor_copy(out=dst[:, 2 * cp:2 * cp + 2, :], in_=bv.rearrange("p (c n) -> p c n", c=2)),
                   r=[bn], w=[dstname])
            yield

    def load_w_cols(dst, dstname, w2d, c0, ncols, gsb, gname, sem, dcol=0):
        src = w2d.rearrange("(c p) n -> p c n", p=128)
        op("pool", [lambda e, c=c: e.dma_start(out=dst[:, c, dcol:dcol + ncols], in_=src[:, c, c0:c0 + ncols]) for c in range(8)],
           w=[dstname + "_%d" % c for c in range(8)], dsem=sem)
        if gsb is not None:
            for c in range(8):
                op("dve", lambda e, c=c: e.tensor_scalar(out=dst[:, c, dcol:dcol + ncols], in0=dst[:, c, dcol:dcol + ncols],
                                                         scalar1=gsb[:, c:c + 1], scalar2=None, op0=ALU.mult),
                   r=[dstname + "_%d" % c, gname], w=[dstname + "_%d" % c])

    def wdeps(name):
        return [name + "_%d" % c for c in range(8)]

    g1 = sb(es, "g1", [128, 8], F32)
    dma_T(g1[:], I["norm1_g"][0], 8, "g1", sc.dsem())

    def head_epilogue(stk_t, pb, pbn, gcol, gname, ones_lhsT, ones_name, rt_lhsT, rt_name, Ct, Cn, St, Sn, dst, dstnames, norm=True, tp="he"):
        t0, sq, rtt, tnb, o1, o2 = stk_t
        if norm:
            op("act", lambda e: e.activation(out=sq[:], in_=pb[:], func=AF.Square), r=[pbn], w=[tp + "sq"])
            op("dve", lambda e: e.tensor_scalar(out=t0[:], in0=pb[:], scalar1=gcol, scalar2=None, op0=ALU.mult), r=[pbn, gname, tp + "sq"], w=[tp + "t0"])
            yield
            p2, p2n = nextbank()
            op("pe", lambda e: e.matmul(p2[:], lhsT=ones_lhsT, rhs=sq[:], start=True, stop=True), r=[tp + "sq", ones_name], w=[p2n])
            nrm = float(1.0 / 128) if ones_name == "kb_ones" else float(1.0 / 64)
            op("act", lambda e: e.activation(out=rtt[:], in_=p2[:], func=AF.Ln, scale=nrm, bias=float(EPS)), r=[p2n], w=[tp + "rt"])
            yield
            op("act", lambda e: e.activation(out=rtt[:], in_=rtt[:], func=AF.Exp, scale=-0.5), r=[tp + "rt"], w=[tp + "rt"])
            op("dve", lambda e: e.tensor_tensor(out=tnb[:], in0=t0[:], in1=rtt[:], op=ALU.mult), r=[tp + "t0", tp + "rt"], w=[tp + "tnb"])
        else:
            op("act", lambda e: e.activation(out=tnb[:], in_=pb[:], func=AF.Copy), r=[pbn], w=[tp + "tnb"])
        yield
        op("dve", lambda e: e.tensor_tensor(out=o1[:], in0=tnb[:], in1=Ct[:], op=ALU.mult), r=[tp + "tnb", Cn], w=[tp + "o1"])
        yield
        p3, p3n = nextbank()
        op("pe", lambda e: e.matmul(p3[:], lhsT=rt_lhsT, rhs=tnb[:], start=True, stop=True), r=[tp + "tnb", rt_name], w=[p3n])
        op("dve", lambda e: e.tensor_tensor(out=o2[:], in0=p3[:], in1=St[:], op=ALU.mult), r=[p3n, Sn], w=[tp + "o2"])
        op("dve", lambda e: e.tensor_tensor(out=dst, in0=o1[:], in1=o2[:], op=ALU.add), r=[tp + "o1", tp + "o2"], w=dstnames)

    def he_tiles(stk, tp="he"):
        return (sb(stk, tp + "t0", [128, 512], F32), sb(stk, tp + "sq", [128, 512], BF16), sb(stk, tp + "rt", [128, 512], F32),
                sb(stk, tp + "tnb", [128, 512], BF16), sb(stk, tp + "o1", [128, 512], F32), sb(stk, tp + "o2", [128, 512], F32))

    if stage >= 1:
        with ExitStack() as st:
            W1 = sb(st, "W1", [128, 8, 1664], BF16)
            ws = sc.dsem()
            w_in2 = I["w_in"][0]
            load_w_cols(W1, "W1a", w_in2, 0, 1024, g1, "g1", sc.dsem(), 0)
            load_w_cols(W1, "W1b", w_in2, 3072, 512, g1, "g1", sc.dsem(), 1024)
            load_w_cols(W1, "W1c", w_in2, 4096, 64, g1, "g1", sc.dsem(), 1536)
            load_w_cols(W1, "W1d", w_in2, 4096, 64, g1, "g1", sc.dsem(), 1600)
            W1names = wdeps("W1a") + wdeps("W1b") + wdeps("W1c") + wdeps("W1d")
            psem = sc.dsem()
            cw = sb(st, "cw", [128, 4, 8], F32); cb = sb(st, "cb", [128, 8], F32)
            br = sb(st, "br", [128, 8], F32); bi = sb(st, "bi", [128, 8], F32); lap = sb(st, "lap", [128, 8], F32)
            nsp = sb(st, "nsp", [128, 8], F32)
            fl = [dma_T(cw[:, k, :], I["conv_w"][0, k], 8) for k in range(4)]
            fl.append(dma_T(cb[:], I["conv_b"][0], 8))
            fl.append(dma_T(br[:], I["lru_b_r"][0].rearrange("n d -> (n d)"), 8))
            fl.append(dma_T(bi[:], I["lru_b_i"][0].rearrange("n d -> (n d)"), 8))
            fl.append(dma_T(lap[:], I["lru_a_param"][0], 8))
            op("sp", fl, w=["cw0", "cw1", "cw2", "cw3", "cb", "br", "bi", "lap"], dsem=psem)
            op("act", lambda e: e.activation(out=nsp[:], in_=lap[:], func=AF.Exp, scale=-1.0), r=["lap"], w=["nsp"])
            op("act", lambda e: e.activation(out=nsp[:], in_=nsp[:], func=AF.Ln, bias=1.0), r=["nsp"], w=["nsp"])
            op("dve", lambda e: e.tensor_scalar(out=nsp[:], in0=nsp[:], scalar1=-8.0, scalar2=None, op0=ALU.mult), r=["nsp"], w=["nsp"])
            wr = sb(st, "wr", [128, 8, 128], BF16); wi = sb(st, "wi", [128, 8, 128], BF16)
            op("pool", [lambda e: e.dma_start(out=wr[:], in_=I["lru_w_r"][0].rearrange("n c d -> c n d")),
                        lambda e: e.dma_start(out=wi[:], in_=I["lru_w_i"][0].rearrange("n c d -> c n d"))], w=["wr", "wi"], dsem=sc.dsem())
            gk = sb(st, "gk", [128, 2], F32)
            op("sp", [lambda e: e.dma_start(out=gk[:, 0:1], in_=I["k_norm_g"][0].rearrange("(p o) -> p o", o=1)),
                      lambda e: e.dma_start(out=gk[0:64, 1:2], in_=I["idx_k_norm_g"][0].rearrange("(p o) -> p o", o=1)),
                      lambda e: e.dma_start(out=gk[64:128, 1:2], in_=I["idx_k_norm_g"][0].rearrange("(p o) -> p o", o=1))], w=["gk"], dsem=sc.dsem())
            XR = make_xT(st, "p1")
            xT = [sb(st, "p1xT%d" % i, [128, 8, 512], BF16) for i in range(2)]
            RP = rope_tables(st, I["pos_full"], 0, 512, "r1")
            HT = he_tiles(st)
            lxs = sb(st, "lxs", [128, 8, 515], F32)
            op("dve", lambda e: e.memset(lxs[:, :, 0:3], 0.0), w=["lxs%d" % n for n in range(8)])
            state = sb(st, "lstate", [128, 8], F32)
            op("dve", lambda e: e.memset(state[:], 0.0), w=["lstate%d" % n for n in range(8)])
            LS = [{nm: sb(st, "L%d" % k_ + nm, [128, 512], F32) for nm in ("xa", "r", "i", "a", "a2", "u", "h")} for k_ in range(3)]
            xabS = [sb(st, "L%dxab" % k_, [128, 512], BF16) for k_ in range(3)]
            hst = [sb(st, "hst%d" % i, [128, 8, 512], BF16) for i in range(2)]
            kst = [sb(st, "kst%d" % i, [128, 3, 512], BF16) for i in range(2)]
            vst = [sb(st, "vst%d" % i, [128, 4, 256], BF16) for i in range(2)]
            hsem = [sc.dsem() for _ in range(2)]; ksem = [sc.dsem() for _ in range(2)]; vsem = [sc.dsem() for _ in range(2)]

            def delay(gen, n):
                for _ in range(n):
                    yield
                for _ in gen:
                    yield

            def rr_merge(gens):
                gens = [g_ for g_ in gens if g_ is not None]
                while gens:
                    for g_ in list(gens):
                        try:
                            next(g_)
                        except StopIteration:
                            gens.remove(g_)

            def lru_chain(n, k_, sl, xTg, xTn):
                L = LS[k_]; xab = xabS[k_]; P_ = "L%d" % k_
                pb, pbn = nextbank()
                for c in range(8):
                    op("pe", lambda e, c=c: e.matmul(pb[:], lhsT=W1[:, c, n * 128:(n + 1) * 128], rhs=xTg[:, c, :], start=(c == 0), stop=(c == 7)),
                       r=[xTn, "W1a_%d" % c], w=[pbn])
                ln = "lxs%d" % n
                op("act", lambda e: e.activation(out=lxs[:, n, 3:515], in_=pb[:], func=AF.Copy), r=[pbn], w=[ln])
                yield
                op("dve", lambda e: e.tensor_scalar(out=L["xa"][:], in0=lxs[:, n, 3:515], scalar1=cw[:, 3, n:n + 1], scalar2=cb[:, n:n + 1],
                                                    op0=ALU.mult, op1=ALU.add), r=[ln, "cw3", "cb"], w=[P_ + "xa"])
                for k in range(3):
                    op("dve", lambda e, k=k: e.scalar_tensor_tensor(out=L["xa"][:], in0=lxs[:, n, k:k + 512], scalar=cw[:, k, n:n + 1], in1=L["xa"][:],
                                                                    op0=ALU.mult, op1=ALU.add), r=[ln, "cw%d" % k, P_ + "xa"], w=[P_ + "xa"])
                    yield
                op("dve", lambda e: e.tensor_copy(out=lxs[:, n, 0:3], in_=lxs[:, n, 512:515]), r=[ln], w=[ln])
                op("act", lambda e: e.activation(out=xab[:], in_=L["xa"][:], func=AF.Copy), r=[P_ + "xa"], w=[P_ + "xab"])
                yield
                pr, prn = nextbank(); pi_, pin = nextbank()
                op("pe", lambda e: e.matmul(pr[:], lhsT=wr[:, n, :], rhs=xab[:], start=True, stop=True), r=[P_ + "xab", "wr"], w=[prn])
                op("pe", lambda e: e.matmul(pi_[:], lhsT=wi[:, n, :], rhs=xab[:], start=True, stop=True), r=[P_ + "xab", "wi"], w=[pin])
                op("act", lambda e: e.activation(out=L["r"][:], in_=pr[:], func=AF.Sigmoid, bias=br[:, n:n + 1]), r=[prn, "br"], w=[P_ + "r"])
                op("act", lambda e: e.activation(out=L["i"][:], in_=pi_[:], func=AF.Sigmoid, bias=bi[:, n:n + 1]), r=[pin, "bi"], w=[P_ + "i"])
                yield
                op("act", lambda e: e.activation(out=L["a"][:], in_=L["r"][:], func=AF.Exp, scale=nsp[:, n:n + 1]), r=[P_ + "r", "nsp"], w=[P_ + "a"])
                yield
                op("act", lambda e: e.activation(out=L["a2"][:], in_=L["a"][:], func=AF.Square), r=[P_ + "a"], w=[P_ + "a2"])
                yield
                op("dve", lambda e: e.tensor_scalar(out=L["a2"][:], in0=L["a2"][:], scalar1=-1.0, scalar2=1.0000001, op0=ALU.mult, op1=ALU.add), r=[P_ + "a2"], w=[P_ + "a2"])
                yield
                op("act", lambda e: e.activation(out=L["a2"][:], in_=L["a2"][:], func=AF.Ln), r=[P_ + "a2"], w=[P_ + "a2"])
                yield
                op("act", lambda e: e.activation(out=L["a2"][:], in_=L["a2"][:], func=AF.Exp, scale=0.5), r=[P_ + "a2"], w=[P_ + "a2"])
                yield
                op("dve", lambda e: e.tensor_tensor(out=L["u"][:], in0=L["a2"][:], in1=L["i"][:], op=ALU.mult), r=[P_ + "a2", P_ + "i"], w=[P_ + "u"])
                yield
                op("dve", lambda e: e.tensor_tensor(out=L["u"][:], in0=L["u"][:], in1=L["xa"][:], op=ALU.mult), r=[P_ + "u", P_ + "xa"], w=[P_ + "u"])
                yield
                op("dve", lambda e: e.tensor_tensor_scan(out=L["h"][:], data0=L["a"][:], data1=L["u"][:], initial=state[:, n:n + 1],
                                                         op0=ALU.mult, op1=ALU.add), r=[P_ + "a", P_ + "u", "lstate%d" % n], w=[P_ + "h"])
                op("dve", lambda e: e.tensor_copy(out=state[:, n:n + 1], in_=L["h"][:, 511:512]), r=[P_ + "h"], w=["lstate%d" % n])
                yield
                op("act", lambda e: e.activation(out=hst[sl][:, n, :], in_=L["h"][:], func=AF.Copy), r=[P_ + "h"], w=["hst%d" % sl])
                yield

            def rope_chain(g):
                for _ in rope_fill(RP, g):
                    yield

            def khead_chain(j, sl, xTg, xTn):
                pb, pbn = nextbank()
                c0 = 1024 + j * 128 if j < 2 else 1536
                wn = "W1b_%d" if j < 2 else "W1c_%d"
                for c in range(8):
                    op("pe", lambda e, c=c: e.matmul(pb[:], lhsT=W1[:, c, c0:c0 + 128], rhs=xTg[:, c, :], start=(c == 0), stop=(c == 7)),
                       r=[xTn, wn % c, "W1d_%d" % c], w=[pbn])
                if j < 2:
                    gen = head_epilogue(HT, pb, pbn, gk[:, 0:1], "gk", CB["ones"][:], "kb_ones", CB["rtm"][:], "kb_rtm",
                                        RP["T"]["cm"], "r1cm", RP["T"]["sm"], "r1sm", kst[sl][:, j, :], ["kst%d" % sl])
                else:
                    gen = head_epilogue(HT, pb, pbn, gk[:, 1:2], "gk", CB["blkones"][:], "kb_blkones", CB["rti"][:], "kb_rti",
                                        RP["T"]["ci"], "r1ci", RP["T"]["si"], "r1si", kst[sl][:, j, :], ["kst%d" % sl])
                for _ in gen:
                    yield

            def v_chain(sl, xTg, xTn):
                for t in range(4):
                    pb, pbn = nextbank()
                    for c in range(8):
                        op("pe", lambda e, c=c, t=t, pb=pb: e.matmul(pb[:, 0:256], lhsT=xTg[:, c, t * 128:(t + 1) * 128], rhs=W1[:, c, 1280:1536], start=(c == 0), stop=(c == 7)),
                           r=[xTn, "W1b_%d" % c], w=[pbn])
                    op("act", lambda e, t=t, pb=pb: e.activation(out=vst[sl][:, t, :], in_=pb[:, 0:256], func=AF.Copy), r=[pbn], w=["vst%d" % sl])
                    yield

            NG1 = min(S // 512, NG_DEBUG)
            if NG1 > 0:
                for _ in xT_group(XR, I["x_full"], 0, xT[0], "p1xT0"): pass
            for g in range(NG1):
                sl = g % 2
                xTg = xT[sl]; xTn = "p1xT%d" % sl
                nxt = xT_group(XR, I["x_full"], g + 1, xT[1 - sl], "p1xT%d" % (1 - sl)) if g + 1 < NG1 else None
                rr_merge([lru_chain(0, 0, sl, xTg, xTn), delay(lru_chain(1, 1, sl, xTg, xTn), 5), delay(lru_chain(2, 2, sl, xTg, xTn), 10), rope_chain(g)])
                rr_merge([lru_chain(3, 0, sl, xTg, xTn), delay(lru_chain(4, 1, sl, xTg, xTn), 5), delay(lru_chain(5, 2, sl, xTg, xTn), 10), khead_chain(0, sl, xTg, xTn)])
                rr_merge([lru_chain(6, 0, sl, xTg, xTn), delay(lru_chain(7, 1, sl, xTg, xTn), 5), delay(khead_chain(1, sl, xTg, xTn), 3), nxt])
                rr_merge([khead_chain(2, sl, xTg, xTn), v_chain(sl, xTg, xTn)])
                op("sp", lambda e, g=g, sl=sl: e.dma_start(out=hT_d[:, :, g * 512:(g + 1) * 512].rearrange("c p t -> p c t"), in_=hst[sl][:]),
                   r=["hst%d" % sl], w=["hT_d"], dsem=hsem[sl])
                op("sp", [lambda e, g=g, sl=sl: e.dma_start(out=kT_d[:, :, g * 512:(g + 1) * 512].rearrange("c p t -> p c t"), in_=kst[sl][:, 0:2, :]),
                          lambda e, g=g, sl=sl: e.dma_start(out=kiT_d[:, g * 512:(g + 1) * 512], in_=kst[sl][:, 2, :])],
                   r=["kst%d" % sl], w=["kT_d", "kiT_d"], dsem=ksem[sl])
                op("sp", lambda e, g=g, sl=sl: e.dma_start(out=V_d[g * 512:(g + 1) * 512, :].rearrange("(t p) d -> p t d", p=128), in_=vst[sl][:]),
                   r=["vst%d" % sl], w=["V_d"], dsem=vsem[sl])
            sc.barrier()

    if stage >= 2:
        with ExitStack() as st:
            W2 = sb(st, "W2", [128, 8, 4616], BF16)
            w_in2 = I["w_in"][0]
            load_w_cols(W2, "W2a", w_in2, 1024, 2048, g1, "g1", sc.dsem(), 0)
            load_w_cols(W2, "W2b", w_in2, 3584, 512, g1, "g1", sc.dsem(), 2048)
            load_w_cols(W2, "W2c", w_in2, 4160, 2056, g1, "g1", sc.dsem(), 2560)
            gq = sb(st, "gq", [128, 1], F32)
            op("sp", lambda e: e.dma_start(out=gq[:], in_=I["q_norm_g"][0].rearrange("(p o) -> p o", o=1)), w=["gq"], dsem=sc.dsem())
            XR2 = make_xT(st, "p2", nslot=1)
            xT2 = [sb(st, "p2xT%d" % i, [128, 8, 512], BF16) for i in range(2)]
            RP2 = rope_tables(st, I["pos_own"], 0, 512, "r2")
            HT2 = [he_tiles(st, "h2"), he_tiles(st, "h3")]
            hh = [sb(st, "hh%d" % i, [128, 1024], BF16) for i in range(2)]
            hhsem = [sc.dsem() for _ in range(2)]
            GS = [{nm: sb(st, "G%d" % k_ + nm, [128, 512], F32) for nm in ("z2", "t", "sg", "hs")} for k_ in range(2)]
            yst = [sb(st, "yst0", [128, 8, 512], BF16)] * 2
            qst = [sb(st, "qst0", [128, 8, 512], BF16)] * 2
            qist = [sb(st, "qist0", [128, 4, 512], BF16)] * 2
            mst = [sb(st, "mst%d" % i, [128, 8, 512], BF16) for i in range(2)]
            ysem = [sc.dsem()] * 2; qsem = [sc.dsem()] * 2
            qisem = [sc.dsem()] * 2; msem = [sc.dsem() for _ in range(2)]

            def rr_merge2(gens):
                gens = [g_ for g_ in gens if g_ is not None]
                while gens:
                    for g_ in list(gens):
                        try:
                            next(g_)
                        except StopIteration:
                            gens.remove(g_)

            def gate_chain(n, k_, g, xTg, xTn):
                G = GS[k_]; P_ = "G%d" % k_
                op("sp", lambda e: e.dma_start(out=hh[k_][:], in_=hT_d[n, :, g * 1024:(g + 1) * 1024]), r=["hT_d"], w=["hh%d" % k_], dsem=hhsem[k_])
                pb, pbn = nextbank()
                for c in range(8):
                    op("pe", lambda e, c=c: e.matmul(pb[:], lhsT=W2[:, c, n * 128:(n + 1) * 128], rhs=xTg[:, c, :], start=(c == 0), stop=(c == 7)),
                       r=[xTn, "W2a_%d" % c], w=[pbn])
                op("act", lambda e: e.activation(out=G["z2"][:], in_=pb[:], func=AF.Copy), r=[pbn], w=[P_ + "z2"])
                yield
                op("act", lambda e: e.activation(out=G["t"][:], in_=G["z2"][:], func=AF.Square), r=[P_ + "z2"], w=[P_ + "t"])
                yield
                op("dve", lambda e: e.tensor_scalar(out=G["t"][:], in0=G["t"][:], scalar1=0.044715, scalar2=1.0, op0=ALU.mult, op1=ALU.add), r=[P_ + "t"], w=[P_ + "t"])
                yield
                op("dve", lambda e: e.tensor_tensor(out=G["t"][:], in0=G["t"][:], in1=G["z2"][:], op=ALU.mult), r=[P_ + "t", P_ + "z2"], w=[P_ + "t"])
                yield
                op("act", lambda e: e.activation(out=G["sg"][:], in_=G["t"][:], func=AF.Sigmoid, scale=1.5957691216), r=[P_ + "t"], w=[P_ + "sg"])
                yield
                op("dve", lambda e: e.tensor_tensor(out=G["sg"][:], in0=G["sg"][:], in1=G["z2"][:], op=ALU.mult), r=[P_ + "sg", P_ + "z2"], w=[P_ + "sg"])
                yield
                hv = hh[k_][:].rearrange("p (t two q) -> p t two q", two=2, q=128)
                hsv = G["hs"][:].rearrange("p (t q) -> p t q", q=128)
                op("dve", lambda e: e.tensor_scalar(out=hsv, in0=hv[:, :, 0, :], scalar1=C["sel"][:, 0:1], scalar2=None, op0=ALU.mult),
                   r=["hh%d" % k_, "k_sel"], w=[P_ + "hs"])
                yield
                op("dve", lambda e: e.scalar_tensor_tensor(out=hsv, in0=hv[:, :, 1, :], scalar=C["sel"][:, 1:2], in1=hsv, op0=ALU.mult, op1=ALU.add),
                   r=["hh%d" % k_, "k_sel", P_ + "hs"], w=[P_ + "hs"])
                yield
                op("dve", lambda e: e.tensor_tensor(out=yst[0][:, n, :], in0=G["hs"][:], in1=G["sg"][:], op=ALU.mult), r=[P_ + "hs", P_ + "sg"], w=["yst0"])
                yield

            def qhead_chain(hd, k_, xTg, xTn):
                pb, pbn = nextbank()
                for c in range(8):
                    op("pe", lambda e, c=c: e.matmul(pb[:], lhsT=W2[:, c, 1024 + hd * 128:1024 + (hd + 1) * 128], rhs=xTg[:, c, :], start=(c == 0), stop=(c == 7)),
                       r=[xTn, "W2a_%d" % c], w=[pbn])
                for _ in head_epilogue(HT2[k_], pb, pbn, gq[:, 0:1], "gq", CB["ones"][:], "kb_ones", CB["rtm"][:], "kb_rtm",
                                       RP2["T"]["cm"], "r2cm", RP2["T"]["sm"], "r2sm", qst[0][:, hd, :], ["qst0"], tp="h%d" % (2 + k_)):
                    yield

            def qidx_chain(hp, k_, xTg, xTn):
                pb, pbn = nextbank()
                for c in range(8):
                    op("pe", lambda e, c=c: e.matmul(pb[:], lhsT=W2[:, c, 2048 + hp * 128:2048 + (hp + 1) * 128], rhs=xTg[:, c, :], start=(c == 0), stop=(c == 7)),
                       r=[xTn, "W2b_%d" % c], w=[pbn])
                for _ in head_epilogue(HT2[k_], pb, pbn, None, None, None, None, CB["rti"][:], "kb_rti",
                                       RP2["T"]["ci"], "r2ci", RP2["T"]["si"], "r2si", qist[0][:, hp, :], ["qist0"], norm=False, tp="h%d" % (2 + k_)):
                    yield

            def widx_chain(g, xTg, xTn):
                for t in range(4):
                    pb, pbn = nextbank()
                    for c in range(8):
                        op("pe", lambda e, c=c, t=t, pb=pb: e.matmul(pb[:, 0:8], lhsT=xTg[:, c, t * 128:(t + 1) * 128], rhs=W2[:, c, 2560:2568], start=(c == 0), stop=(c == 7)),
                           r=[xTn, "W2c_%d" % c], w=[pbn])
                    op("act", lambda e, t=t, pb=pb: e.activation(out=wq[:, g * 4 + t, :], in_=pb[:, 0:8], func=AF.Copy, scale=float((8 ** -0.5) * (64 ** -0.5))),
                       r=[pbn], w=["wq"])
                    yield

            def mg_chain(g, mh, m8s, xTg, xTn, last):
                for m8 in m8s:
                    m = mh * 8 + m8
                    pb, pbn = nextbank()
                    for c in range(8):
                        op("pe", lambda e, c=c, m=m, pb=pb: e.matmul(pb[:], lhsT=W2[:, c, 2568 + m * 128:2568 + (m + 1) * 128], rhs=xTg[:, c, :], start=(c == 0), stop=(c == 7)),
                           r=[xTn, "W2c_%d" % c], w=[pbn])
                    op("act", lambda e, m8=m8, pb=pb: e.activation(out=mst[mh][:, m8, :], in_=pb[:], func=AF.Sigmoid), r=[pbn], w=["mst%d" % mh])
                    yield
                if last:
                    op("sp", lambda e: e.dma_start(out=mgT_d[mh * 8:(mh + 1) * 8, :, g * 512:(g + 1) * 512].rearrange("c p t -> p c t"), in_=mst[mh][:]),
                       r=["mst%d" % mh], w=["mgT_d"], dsem=msem[mh])

            def rope2_chain(g):
                for _ in rope_fill(RP2, g):
                    yield

            NG2 = min(SO // 512, NG2_DEBUG)
            if NG2 > 0:
                for _ in xT_group(XR2, I["x_own"], 0, xT2[0], "p2xT0"): pass
            for g in range(NG2):
                sl = g % 2
                xTg = xT2[sl]; xTn = "p2xT%d" % sl
                nxt = xT_group(XR2, I["x_own"], g + 1, xT2[1 - sl], "p2xT%d" % (1 - sl)) if g + 1 < NG2 else None
                rr_merge2([gate_chain(0, 0, g, xTg, xTn), gate_chain(1, 1, g, xTg, xTn), rope2_chain(g), mg_chain(g, 0, range(0, 4), xTg, xTn, False)])
                rr_merge2([gate_chain(2, 0, g, xTg, xTn), gate_chain(3, 1, g, xTg, xTn), qhead_chain(0, 0, xTg, xTn), qhead_chain(1, 1, xTg, xTn),
                           mg_chain(g, 0, range(4, 8), xTg, xTn, True)])
                rr_merge2([gate_chain(4, 0, g, xTg, xTn), gate_chain(5, 1, g, xTg, xTn), qhead_chain(2, 0, xTg, xTn), qhead_chain(3, 1, xTg, xTn),
                           mg_chain(g, 1, range(0, 4), xTg, xTn, False)])
                rr_merge2([gate_chain(6, 0, g, xTg, xTn), gate_chain(7, 1, g, xTg, xTn), qhead_chain(4, 0, xTg, xTn), qhead_chain(5, 1, xTg, xTn),
                           mg_chain(g, 1, range(4, 8), xTg, xTn, True)])
                op("sp", lambda e, g=g: e.dma_start(out=yaT_d[:, :, g * 512:(g + 1) * 512].rearrange("c p t -> p c t"), in_=yst[0][:]),
                   r=["yst0"], w=["yaT_d"], dsem=ysem[0])
                rr_merge2([qhead_chain(6, 0, xTg, xTn), qhead_chain(7, 1, xTg, xTn), widx_chain(g, xTg, xTn), nxt])
                op("sp", lambda e, g=g: e.dma_start(out=qT_d[:, :, g * 512:(g + 1) * 512].rearrange("c p t -> p c t"), in_=qst[0][:]),
                   r=["qst0"], w=["qT_d"], dsem=qsem[0])
                rr_merge2([qidx_chain(0, 0, xTg, xTn), qidx_chain(1, 1, xTg, xTn)])
                rr_merge2([qidx_chain(2, 0, xTg, xTn), qidx_chain(3, 1, xTg, xTn)])
                op("sp", lambda e, g=g: e.dma_start(out=qiT_d[:, :, g * 512:(g + 1) * 512].rearrange("c p t -> p c t"), in_=qist[0][:]),
                   r=["qist0"], w=["qiT_d"], dsem=qisem[0])
            sc.barrier()

    if stage >= 3:
        with ExitStack() as st:
            kT = sb(st, "kT", [128, 2, S], BF16); kiT = sb(st, "kiT", [128, S], BF16); Vs = sb(st, "Vs", [128, 64, 256], BF16)
            lsem = sc.dsem()
            fl = []
            NQ4 = min(4, (min(NG_DEBUG * 512, S) + 2047) // 2048)
            for j in range(2):
                for q4 in range(NQ4):
                    fl.append(lambda e, j=j, q4=q4: e.dma_start(out=kT[:, j, q4 * 2048:(q4 + 1) * 2048], in_=kT_d[j, :, q4 * 2048:(q4 + 1) * 2048]))
            for q4 in range(NQ4):
                fl.append(lambda e, q4=q4: e.dma_start(out=kiT[:, q4 * 2048:(q4 + 1) * 2048], in_=kiT_d[:, q4 * 2048:(q4 + 1) * 2048]))
            for q4 in range(NQ4):
                fl.append(lambda e, q4=q4: e.dma_start(out=Vs[:, q4 * 16:(q4 + 1) * 16, :], in_=V_d[q4 * 2048:(q4 + 1) * 2048, :].rearrange("(t p) d -> p t d", p=128)))
            op("sp", fl, r=["kT_d", "kiT_d", "V_d"], w=["kT", "kiT", "Vs"], dsem=lsem)
            scoresL = [sb(st, "scores%d" % i, [128, S], F32) for i in range(2)]
            junk = sb(st, "ajunk", [128, S], mybir.dt.uint8)
            maskb = [sb(st, "maskb%d" % i, [128, S], BF16) for i in range(2)]
            iselb = sb(st, "iselb", [128, 4, 128], BF16)
            for h4 in range(4):
                op("dve", lambda e, h4=h4: e.tensor_scalar(out=iselb[:, h4, :], in0=CB["ident"][:], scalar1=float(BIG), scalar2=None, op0=ALU.mult),
                   r=["kb_ident"], w=["iselb"])
            qTb = [sb(st, "qTb%d" % i, [128, 8, 128], BF16) for i in range(2)]
            qiTb = [sb(st, "qiTb%d" % i, [128, 4, 128], BF16) for i in range(2)]
            qsm = [sc.dsem() for _ in range(2)]; qsm2 = [sc.dsem() for _ in range(2)]
            Rt = [sb(st, "Rt%d" % i, [128, 512], BF16) for i in range(2)]
            diagw = sb(st, "diagw", [128, 8, 128], BF16)
            Et = [sb(st, "Et%d" % i, [128, 512], BF16) for i in range(3)]
            bs = sb(st, "bs", [128, 8], F32)
            Wk = sb(st, "Wk", [128, NBIS], F32)
            p2k = sb(st, "p2k", [128, NBIS], F32)
            for k in range(NBIS):
                op("dve", lambda e, k=k: e.memset(p2k[:, k:k + 1], float(0.5 ** (k + 1))), w=["p2k"])
            rden = sb(st, "rden", [128, 512], F32)
            ybst = [sb(st, "ybst0", [128, 8, 128], BF16)] * 2
            ybsem = [sc.dsem()] * 2
            SCALE = float(128 ** -0.5)
            cnts = {"R": 0, "E": 0, "z": 0}

            def gen_I(i):
                sl = i % 2
                nkt = 2 * i + 2; Lk = nkt * 128
                scores = scoresL[sl]; scn = "scores%d" % sl
                op("sp", lambda e: e.dma_start(out=qiTb[sl][:], in_=qiT_d[:, :, i * 128:(i + 1) * 128].rearrange("c p t -> p c t")),
                   r=["qiT_d"], w=["qiTb%d" % sl], dsem=qsm[sl])
                for h in range(8):
                    op("dve", lambda e, h=h: e.tensor_scalar(out=diagw[:, h, :], in0=CB["ident"][:], scalar1=wq[:, i, h:h + 1], scalar2=None, op0=ALU.mult),
                       r=["kb_ident", "wq"], w=["diagw"])
                yield
                for kg in range((nkt + 3) // 4):
                    W_ = min(4, nkt - 4 * kg) * 128
                    psc, pscn = PS[6 + kg % 2], psn[6 + kg % 2]
                    pend_ = None
                    for h in range(9):
                        if h < 8:
                            zi = 4 + cnts["z"] % 2; cnts["z"] += 1
                            pz, pzn = PS[zi], psn[zi]
                            pb0 = 64 * (h % 2)
                            op("pe", lambda e, h=h, kg=kg, W_=W_, pz=pz, pb0=pb0: e.matmul(pz[:, 0:W_], lhsT=qiTb[sl][pb0:pb0 + 64, h // 2, :],
                                                                                       rhs=kiT[pb0:pb0 + 64, kg * 512:kg * 512 + W_], start=True, stop=True),
                               r=["qiTb%d" % sl, "kiT"], w=[pzn])
                            rt_ = cnts["R"] % 2; cnts["R"] += 1
                            op("act", lambda e, W_=W_, pz=pz, rt_=rt_: e.activation(out=Rt[rt_][:, 0:W_], in_=pz[:, 0:W_], func=AF.Relu), r=[pzn], w=["Rt%d" % rt_])
                        if pend_ is not None:
                            ph, prt = pend_
                            op("pe", lambda e, ph=ph, W_=W_, psc=psc, prt=prt: e.matmul(psc[:, 0:W_], lhsT=diagw[:, ph, :], rhs=Rt[prt][:, 0:W_], start=(ph == 0), stop=(ph == 7)),
                               r=["Rt%d" % prt, "diagw"], w=[pscn])
                        pend_ = (h, rt_) if h < 8 else None
                    op("act", lambda e, kg=kg, W_=W_, psc=psc: e.activation(out=scores[:, kg * 512:kg * 512 + W_], in_=psc[:, 0:W_], func=AF.Copy), r=[pscn], w=[scn])
                    yield

            def gen_T(i):
                sl = i % 2
                nkt = 2 * i + 2; Lk = nkt * 128
                scores = scoresL[sl]; scn = "scores%d" % sl
                op("dve", lambda e: e.tensor_tensor(out=scores[:, Lk - 256:Lk], in0=scores[:, Lk - 256:Lk], in1=C["amask"][:], op=ALU.add),
                   r=[scn, "k_amask"], w=[scn])
                if i == 0:
                    op("dve", lambda e: e.memset(bs[:, 0:1], float(-BIG / 2)), w=["bs"])
                else:
                    op("dve", lambda e: e.tensor_reduce(out=bs[:, 1:2], in_=scores[:, 0:Lk], axis=AX.X, op=ALU.max), r=[scn], w=["bs"])
                    op("dve", lambda e: e.tensor_reduce(out=bs[:, 0:1], in_=scores[:, 0:Lk - 256], axis=AX.X, op=ALU.min), r=[scn, "bs"], w=["bs"])
                    op("dve", lambda e: e.scalar_tensor_tensor(out=bs[:, 1:2], in0=bs[:, 1:2], scalar=1.0, in1=bs[:, 0:1], op0=ALU.add, op1=ALU.subtract), r=["bs"], w=["bs"])
                    op("dve", lambda e: e.tensor_scalar(out=Wk[:], in0=p2k[:], scalar1=bs[:, 1:2], scalar2=None, op0=ALU.mult), r=["bs", "p2k"], w=["Wk"])
                    yield
                    for k in range(NBIS):
                        op("dve", lambda e, k=k: e.tensor_tensor(out=bs[:, 2:3], in0=bs[:, 0:1], in1=Wk[:, k:k + 1], op=ALU.add), r=["bs", "Wk"], w=["bs"])
                        op("dve", lambda e: e.tensor_scalar(out=junk[:, 0:Lk], in0=scores[:, 0:Lk], scalar1=bs[:, 2:3], scalar2=None, op0=ALU.is_ge, op1=ALU.add,
                                                            accum_out=bs[:, 3:4]), r=[scn, "bs"], w=["bs", "ajunk"])
                        op("dve", lambda e, k=k: e.scalar_tensor_tensor(out=bs[:, 4:5], in0=bs[:, 3:4], scalar=float(TOPK - 0.5), in1=Wk[:, k:k + 1], op0=ALU.is_ge, op1=ALU.mult),
                           r=["bs", "Wk"], w=["bs"])
                        op("dve", lambda e: e.tensor_tensor(out=bs[:, 0:1], in0=bs[:, 0:1], in1=bs[:, 4:5], op=ALU.add), r=["bs"], w=["bs"])
                        yield
                op("dve", lambda e: e.tensor_scalar(out=maskb[sl][:, 0:Lk], in0=scores[:, 0:Lk], scalar1=bs[:, 0:1], scalar2=1.0, op0=ALU.is_ge, op1=ALU.subtract),
                   r=[scn, "bs"], w=["maskb%d" % sl])
                yield

            def gen_A(i):
                sl = i % 2
                nkt = 2 * i + 2
                mb = maskb[sl]; mbn = "maskb%d" % sl
                op("sp", lambda e: e.dma_start(out=qTb[sl][:], in_=qT_d[:, :, i * 128:(i + 1) * 128].rearrange("c p t -> p c t")),
                   r=["qT_d"], w=["qTb%d" % sl], dsem=qsm2[sl])
                for j in range(2):
                    prev = None
                    for kt in range(nkt + 1):
                        if kt < nkt:
                            si = cnts["E"] % 2
                            ps_, psn_ = PS[si], psn[si]
                            op("pe", lambda e, kt=kt, ps_=ps_, j=j: e.matmul(ps_[:], lhsT=kT[:, j, kt * 128:(kt + 1) * 128], rhs=qTb[sl][:, 4 * j:4 * j + 4, :],
                                                                           start=True, stop=False), r=["kT", "qTb%d" % sl], w=[psn_])
                            op("pe", lambda e, kt=kt, ps_=ps_: e.matmul(ps_[:], lhsT=mb[:, kt * 128:(kt + 1) * 128], rhs=iselb[:].rearrange("p h q -> p (h q)"),
                                                                      start=False, stop=True), r=[mbn, "iselb"], w=[psn_])
                            et = cnts["E"] % 3; cnts["E"] += 1
                            op("act", lambda e, ps_=ps_, et=et: e.activation(out=Et[et][:], in_=ps_[:], func=AF.Exp, scale=SCALE), r=[psn_], w=["Et%d" % et])
                        if prev is not None:
                            pkt, pet = prev
                            op("pe", lambda e, pkt=pkt, pet=pet, j=j: e.matmul(PS[2][:], lhsT=Vs[:, pkt, j * 128:(j + 1) * 128], rhs=Et[pet][:], start=(pkt == 0), stop=(pkt == nkt - 1)),
                               r=["Vs", "Et%d" % pet], w=[psn[2]])
                            op("pe", lambda e, pkt=pkt, pet=pet: e.matmul(PS[3][:], lhsT=CB["ones"][:], rhs=Et[pet][:], start=(pkt == 0), stop=(pkt == nkt - 1)),
                               r=["kb_ones", "Et%d" % pet], w=[psn[3]])
                        prev = (kt, et) if kt < nkt else None
                        yield
                    op("act", lambda e: e.activation(out=rden[:], in_=PS[3][:], func=AF.Ln), r=[psn[3]], w=["rden"])
                    op("act", lambda e: e.activation(out=rden[:], in_=rden[:], func=AF.Exp, scale=-1.0), r=["rden"], w=["rden"])
                    op("dve", lambda e, j=j: e.tensor_tensor(out=ybst[sl][:, 4 * j:4 * j + 4, :], in0=PS[2][:].rearrange("p (h q) -> p h q", q=128),
                                                            in1=rden[:].rearrange("p (h q) -> p h q", q=128), op=ALU.mult),
                       r=[psn[2], "rden"], w=["ybst0"])
                op("sp", lambda e: e.dma_start(out=ybT_d[:, :, i * 128:(i + 1) * 128].rearrange("c p t -> p c t"), in_=ybst[sl][:]),
                   r=["ybst0"], w=["ybT_d"], dsem=ybsem[sl])
                yield

            def n_I(i):
                return 1 + (2 * i + 2 + 3) // 4

            def n_T(i):
                return (0 if i == 0 else 1 + NBIS) + 1

            def n_A(i):
                return 2 * (2 * i + 3) + 1

            def merge(gens):
                st_ = [[g, n, 0, False] for g, n in gens if g is not None]
                while any(not x[3] for x in st_):
                    best = None
                    for x in st_:
                        if x[3]: continue
                        frac = x[2] / float(x[1])
                        if best is None or frac < best[0]: best = (frac, x)
                    x = best[1]
                    try:
                        next(x[0]); x[2] += 1
                    except StopIteration:
                        x[3] = True

            NQ = min(SO // 128, NQ_DEBUG)
            for s_ in range(-1, NQ + 1):
                gl = []
                if 0 <= s_ - 1 < NQ: gl.append((gen_A(s_ - 1), n_A(s_ - 1)))
                if 0 <= s_ < NQ: gl.append((gen_T(s_), n_T(s_)))
                if 0 <= s_ + 1 < NQ: gl.append((gen_I(s_ + 1), n_I(s_ + 1)))
                merge(gl)
            sc.barrier()

    NT = SO // 128
    if stage >= 4:
        NTd = min(NT, NG3_DEBUG * 4)
        rt_ = es.enter_context(ExitStack())
        sel1 = sb(rt_, "sel1", [128, NT, 32], F32); sel2 = sb(rt_, "sel2", [128, NT, 32], F32)
        rank = sb(rt_, "rank", [128, NT, 32], F32); gates = sb(rt_, "gates", [128, NT, 2], F32)
        Asum = sb(rt_, "Asum", [128, 32], BF16)
        d1i = sb(rt_, "d1i", [128, NT], I32); d2i = sb(rt_, "d2i", [128, NT], I32)
        op("dve", lambda e: e.memset(Asum[:], 0.0), w=["Asum"])
        op("dve", lambda e: e.memset(sel1[:], 0.0), w=["sel1"])
        op("dve", lambda e: e.memset(sel2[:], 0.0), w=["sel2"])
        op("dve", lambda e: e.memset(rank[:], 0.0), w=["rank"])
        with ExitStack() as st:
            WA = sb(st, "WA", [128, 8, 1024], BF16); WB = sb(st, "WB", [128, 8, 1024], BF16); WO = sb(st, "WO", [128, 8, 1024], BF16)
            load_w_cols(WA, "WA", I["w_lru_out"][0], 0, 1024, None, None, sc.dsem())
            load_w_cols(WB, "WB", I["w_attn_out"][0], 0, 1024, None, None, sc.dsem())
            load_w_cols(WO, "WO", I["w_o"][0], 0, 1024, None, None, sc.dsem())
            WR = sb(st, "WR", [128, 8, 36], F32)
            op("sp", [lambda e: e.dma_start(out=WR[:, :, 0:4], in_=I["w_grp"][0].rearrange("(c p) n -> p c n", p=128)),
                      lambda e: e.dma_start(out=WR[:, :, 4:36], in_=I["w_exp"][0].rearrange("(c p) n -> p c n", p=128))], w=["WR"], dsem=sc.dsem())
            g2bc = sb(st, "g2bc", [128, 1024], F32); bbc = sb(st, "bbc", [128, 36], F32)
            op("sp", [lambda e: e.dma_start(out=g2bc[:], in_=I["norm2_g"][0].partition_broadcast(128)),
                      lambda e: e.dma_start(out=bbc[:, 0:4], in_=I["b_grp"][0].partition_broadcast(128)),
                      lambda e: e.dma_start(out=bbc[:, 4:36], in_=I["b_exp"][0].partition_broadcast(128))], w=["g2bc", "bbc"], dsem=sc.dsem())
            ya_sL = [sb(st, "ya_s%d" % i, [128, 8, 512], BF16) for i in range(2)]; yb_sL = [sb(st, "yb_s%d" % i, [128, 8, 512], BF16) for i in range(2)]
            mg_sL = [sb(st, "mg_s%d" % i, [128, 16, 512], BF16) for i in range(2)]
            insem = [sc.dsem() for _ in range(2)]
            mixTL = [sb(st, "mixT%d" % i, [128, 8, 512], BF16) for i in range(2)]
            t1 = sb(st, "ot1", [128, 512], F32); t2 = sb(st, "ot2", [128, 512], F32)
            xin = [sb(st, "oxin%d" % i, [128, 1024], F32) for i in range(2)]; xsem = [sc.dsem() for _ in range(2)]
            x1 = [sb(st, "ox1_%d" % i, [128, 1024], F32) for i in range(2)]; x1sem = [sc.dsem() for _ in range(2)]
            h2L = [sb(st, "oh2_%d" % i, [128, 1024], F32) for i in range(2)]
            h2b = [sb(st, "oh2b%d" % i, [128, 1024], BF16) for i in range(2)]; h2sem = [sc.dsem() for _ in range(2)]
            h2TL = [sb(st, "oh2T%d" % i, [128, 8, 128], F32) for i in range(2)]
            ojunk = sb(st, "ojunk", [128, 1024], BF16)
            RS = []
            for i_ in range(2):
                RS.append(dict(rs=sb(st, "ors%d" % i_, [128, 16], F32), lg=sb(st, "olg%d" % i_, [128, 36], F32), ohg=sb(st, "ohg%d" % i_, [128, 4], F32),
                               tmp32=sb(st, "otmp32_%d" % i_, [128, 32], F32), ig=sb(st, "oig%d" % i_, [128, 8], F32), m8=sb(st, "om8_%d" % i_, [128, 8], F32),
                               A_b=sb(st, "oAb%d" % i_, [128, 32], BF16)))
            zb = sb(st, "ozb", [128, 1024], BF16)
            op("dve", lambda e: e.memset(zb[:], 0.0), w=["ozb"])
            op("sp", lambda e: e.dma_start(out=H2_d[SO:SO + 128, :], in_=zb[:]), r=["ozb"], w=["H2_d_pad"], dsem=sc.dsem())
            if NG3_DEBUG < 1000:
                op("sp", [lambda e, t=t: e.dma_start(out=H2_d[t * 128:(t + 1) * 128, :], in_=zb[:]) for t in range(NT)], r=["ozb"], w=["H2_d"], dsem=sc.dsem())

            def mix_chain(g):
                k_ = g % 2
                ya_s, yb_s, mg_s, mixT = ya_sL[k_], yb_sL[k_], mg_sL[k_], mixTL[k_]
                yan, ybn, mgn, mxn = "ya_s%d" % k_, "yb_s%d" % k_, "mg_s%d" % k_, "mixT%d" % k_
                op("sp", [lambda e: e.dma_start(out=ya_s[:], in_=yaT_d[:, :, g * 512:(g + 1) * 512].rearrange("c p t -> p c t")),
                          lambda e: e.dma_start(out=yb_s[:], in_=ybT_d[:, :, g * 512:(g + 1) * 512].rearrange("c p t -> p c t")),
                          lambda e: e.dma_start(out=mg_s[:], in_=mgT_d[:, :, g * 512:(g + 1) * 512].rearrange("c p t -> p c t"))],
                   r=["yaT_d", "ybT_d", "mgT_d"], w=[yan, ybn, mgn], dsem=insem[k_])
                for m in range(8):
                    pa, pan = nextbank(); pb, pbn = nextbank()
                    for c in range(8):
                        op("pe", lambda e, c=c, m=m, pa=pa: e.matmul(pa[:], lhsT=WA[:, c, m * 128:(m + 1) * 128], rhs=ya_s[:, c, :], start=(c == 0), stop=(c == 7)),
                           r=[yan, "WA_%d" % c], w=[pan])
                    for c in range(8):
                        op("pe", lambda e, c=c, m=m, pb=pb: e.matmul(pb[:], lhsT=WB[:, c, m * 128:(m + 1) * 128], rhs=yb_s[:, c, :], start=(c == 0), stop=(c == 7)),
                           r=[ybn, "WB_%d" % c], w=[pbn])
                    op("dve", lambda e, m=m, pa=pa: e.tensor_tensor(out=t1[:], in0=pa[:], in1=mg_s[:, m, :], op=ALU.mult), r=[pan, mgn], w=["ot1"])
                    op("dve", lambda e, m=m, pb=pb: e.tensor_tensor(out=t2[:], in0=pb[:], in1=mg_s[:, 8 + m, :], op=ALU.mult), r=[pbn, mgn], w=["ot2"])
                    op("dve", lambda e, m=m: e.tensor_tensor(out=mixT[:, m, :], in0=t1[:], in1=t2[:], op=ALU.add), r=["ot1", "ot2"], w=[mxn])
                    yield

            def tile_chain(g, t, k_):
                mixT = mixTL[g % 2]; mxn = "mixT%d" % (g % 2)
                R_ = RS[k_]; rs, lg, ohg, tmp32, ig, m8, A_b = R_["rs"], R_["lg"], R_["ohg"], R_["tmp32"], R_["ig"], R_["m8"], R_["A_b"]
                rsn, lgn, ohgn, tmpn, ign, m8n, Abn = ("ors%d" % k_, "olg%d" % k_, "ohg%d" % k_, "otmp32_%d" % k_, "oig%d" % k_, "om8_%d" % k_, "oAb%d" % k_)
                h2 = h2L[k_]; h2n = "oh2_%d" % k_; h2T = h2TL[k_]; h2Tn = "oh2T%d" % k_
                ti = g * 4 + t; sl = k_
                op("sp", lambda e: e.dma_start(out=xin[sl][:], in_=I["x_own"][ti * 128:(ti + 1) * 128, :]), w=["oxin%d" % sl], dsem=xsem[sl])
                for nh in range(2):
                    po, pon = nextbank()
                    for c in range(8):
                        op("pe", lambda e, c=c, nh=nh, po=po: e.matmul(po[:], lhsT=mixT[:, c, t * 128:(t + 1) * 128], rhs=WO[:, c, nh * 512:(nh + 1) * 512], start=(c == 0), stop=(c == 7)),
                           r=[mxn, "WO_%d" % c], w=[pon])
                    op("dve", lambda e, nh=nh, po=po: e.tensor_tensor(out=x1[sl][:, nh * 512:(nh + 1) * 512], in0=po[:], in1=xin[sl][:, nh * 512:(nh + 1) * 512], op=ALU.add),
                       r=[pon, "oxin%d" % sl], w=["ox1_%d" % sl])
                    yield
                op("sp", lambda e: e.dma_start(out=X1_d[ti * 128:(ti + 1) * 128, :], in_=x1[sl][:]), r=["ox1_%d" % sl], w=["X1_d"], dsem=x1sem[sl])
                op("act", lambda e: e.activation(out=ojunk[:], in_=x1[sl][:], func=AF.Square, accum_out=rs[:, 0:1]), r=["ox1_%d" % sl], w=["ojunk", rsn])
                op("act", lambda e: e.activation(out=rs[:, 1:2], in_=rs[:, 0:1], func=AF.Ln, scale=float(1.0 / D), bias=float(EPS)), r=[rsn], w=[rsn])
                op("act", lambda e: e.activation(out=rs[:, 1:2], in_=rs[:, 1:2], func=AF.Exp, scale=-0.5), r=[rsn], w=[rsn])
                yield
                op("dve", lambda e: e.scalar_tensor_tensor(out=h2[:], in0=x1[sl][:], scalar=rs[:, 1:2], in1=g2bc[:], op0=ALU.mult, op1=ALU.mult),
                   r=["ox1_%d" % sl, rsn, "g2bc"], w=[h2n])
                yield
                op("act", lambda e: e.activation(out=h2b[sl][:], in_=h2[:], func=AF.Copy), r=[h2n], w=["oh2b%d" % sl])
                op("sp", lambda e: e.dma_start(out=H2_d[ti * 128:(ti + 1) * 128, :], in_=h2b[sl][:]), r=["oh2b%d" % sl], w=["H2_d"], dsem=h2sem[sl])
                yield
                for half in range(2):
                    bank, bn = nextbank()
                    for c4 in range(4):
                        c = half * 4 + c4
                        op("pe", lambda e, c=c, c4=c4, bank=bank: e.transpose(out=bank[:, c4 * 128:(c4 + 1) * 128], in_=h2[:, c * 128:(c + 1) * 128], identity=C["ident"][:]),
                           r=[h2n, "k_ident"], w=[bn])
                    op("act", lambda e, half=half, bank=bank: e.activation(out=h2T[:, half * 4:half * 4 + 4, :], in_=bank[:].rearrange("p (c q) -> p c q", q=128), func=AF.Copy),
                       r=[bn], w=[h2Tn])
                    yield
                pl, pln = nextbank()
                for c in range(8):
                    op("pe", lambda e, c=c: e.matmul(pl[:, 0:36], lhsT=h2T[:, c, :], rhs=WR[:, c, :], start=(c == 0), stop=(c == 7)), r=[h2Tn, "WR"], w=[pln])
                op("dve", lambda e: e.tensor_tensor(out=lg[:], in0=pl[:, 0:36], in1=bbc[:], op=ALU.add), r=[pln, "bbc"], w=[lgn])
                yield
                op("dve", lambda e: e.tensor_reduce(out=rs[:, 2:3], in_=lg[:, 0:4], axis=AX.X, op=ALU.max), r=[lgn], w=[rsn])
                op("dve", lambda e: e.tensor_scalar(out=ohg[:], in0=lg[:, 0:4], scalar1=rs[:, 2:3], scalar2=None, op0=ALU.is_ge), r=[lgn, rsn], w=[ohgn])
                yield
                op("dve", lambda e: e.tensor_scalar(out=rs[:, 3:4], in0=rs[:, 2:3], scalar1=-1.0, scalar2=None, op0=ALU.mult), r=[rsn], w=[rsn])
                op("act", lambda e: e.activation(out=ig[:, 0:4], in_=lg[:, 0:4], func=AF.Exp, bias=rs[:, 3:4], accum_out=rs[:, 4:5]), r=[lgn, rsn], w=[ign, rsn])
                yield
                op("dve", lambda e: e.reciprocal(out=rs[:, 5:6], in_=rs[:, 4:5]), r=[rsn], w=[rsn])
                op("dve", lambda e: e.tensor_tensor(out=tmp32[:].rearrange("p (g x) -> p g x", x=8), in0=lg[:, 4:36].rearrange("p (g x) -> p g x", x=8),
                                                    in1=ohg[:].unsqueeze(2).broadcast_to([128, 4, 8]), op=ALU.mult), r=[lgn, ohgn], w=[tmpn])
                yield
                op("dve", lambda e: e.tensor_reduce(out=ig[:], in_=tmp32[:].rearrange("p (g x) -> p x g", x=8), axis=AX.X, op=ALU.add), r=[tmpn, ign], w=[ign])
                op("dve", lambda e: e.max(out=m8[:], in_=ig[:]), r=[ign], w=[m8n])
                yield
                op("dve", lambda e: e.tensor_tensor(out=rs[:, 6:7], in0=m8[:, 1:2], in1=m8[:, 0:1], op=ALU.subtract), r=[m8n, rsn], w=[rsn])
                op("act", lambda e: e.activation(out=rs[:, 7:8], in_=rs[:, 6:7], func=AF.Exp), r=[rsn], w=[rsn])
                yield
                op("dve", lambda e: e.tensor_scalar(out=rs[:, 8:9], in0=rs[:, 7:8], scalar1=1.0, scalar2=None, op0=ALU.add), r=[rsn], w=[rsn])
                op("dve", lambda e: e.reciprocal(out=rs[:, 8:9], in_=rs[:, 8:9]), r=[rsn], w=[rsn])
                yield
                op("dve", lambda e: e.tensor_tensor(out=gates[:, ti, 0:1], in0=rs[:, 8:9], in1=rs[:, 5:6], op=ALU.mult), r=[rsn], w=["gates"])
                op("dve", lambda e: e.tensor_tensor(out=gates[:, ti, 1:2], in0=gates[:, ti, 0:1], in1=rs[:, 7:8], op=ALU.mult), r=[rsn, "gates"], w=["gates"])
                yield
                for kk, sel in ((0, sel1), (1, sel2)):
                    op("dve", lambda e, kk=kk: e.tensor_scalar(out=tmp32[:], in0=lg[:, 4:36], scalar1=m8[:, kk:kk + 1], scalar2=None, op0=ALU.is_equal), r=[lgn, m8n], w=[tmpn])
                    op("dve", lambda e, sel=sel: e.tensor_tensor(out=sel[:, ti, :].rearrange("p (g x) -> p g x", x=8), in0=tmp32[:].rearrange("p (g x) -> p g x", x=8),
                                                                in1=ohg[:].unsqueeze(2).broadcast_to([128, 4, 8]), op=ALU.mult), r=[tmpn, ohgn], w=["sel%d" % (kk + 1)])
                    yield
                op("dve", lambda e: e.tensor_tensor(out=A_b[:], in0=sel1[:, ti, :], in1=sel2[:, ti, :], op=ALU.add), r=["sel1", "sel2"], w=[Abn])
                pr_, prn_ = nextbank()
                op("pe", lambda e: e.matmul(pr_[:, 0:32], lhsT=CB["triu"][:], rhs=A_b[:], start=True, stop=False), r=[Abn, "kb_triu"], w=[prn_])
                op("pe", lambda e: e.matmul(pr_[:, 0:32], lhsT=CB["ones"][:], rhs=Asum[:], start=False, stop=True), r=["Asum", "kb_ones"], w=[prn_])
                op("act", lambda e: e.activation(out=rank[:, ti, :], in_=pr_[:, 0:32], func=AF.Copy), r=[prn_], w=["rank"])
                op("dve", lambda e: e.tensor_tensor(out=Asum[:], in0=Asum[:], in1=A_b[:], op=ALU.add), r=["Asum", Abn], w=["Asum"])
                yield

            def seq(*gens):
                for g_ in gens:
                    for _ in g_:
                        yield

            def rr_merge3(gens):
                gens = [g_ for g_ in gens if g_ is not None]
                while gens:
                    for g_ in list(gens):
                        try:
                            next(g_)
                        except StopIteration:
                            gens.remove(g_)

            NG3 = min(SO // 512, NG3_DEBUG)
            if NG3 > 0:
                for _ in mix_chain(0): pass
            for g in range(NG3):
                rr_merge3([mix_chain(g + 1) if g + 1 < NG3 else None,
                           seq(tile_chain(g, 0, 0), tile_chain(g, 2, 0)), seq(tile_chain(g, 1, 1), tile_chain(g, 3, 1))])
            sc.barrier()

    if stage >= 5:
        with ExitStack() as st:
            toki = sb(st, "mtoki", [128, NT], I32)
            sti = sb(st, "msti", [128, NSLOT // 128], I32)
            wi13i = sb(st, "mwi13i", [128, NBLK], I32)
            sidxA = sb(st, "msidxA", [128, NBLK * NS_], I32)
            tmp = ExitStack()
            cnt = sb(tmp, "mcnt", [128, 32], F32); nb_i = sb(tmp, "mnbi", [128, 32], I32); padc = sb(tmp, "mpadc", [128, 32], F32)
            pend = sb(tmp, "mpend", [128, 32], F32); pstart = sb(tmp, "mpstart", [128, 32], F32); z32 = sb(tmp, "mz32", [128, 32], F32)
            big = sb(tmp, "mbig", [128, NT, 32], F32); dd = sb(tmp, "mdd", [128, NT], F32)
            tokf = sb(tmp, "mtokf", [128, NT], F32)
            cmpb = sb(tmp, "mcmp", [128, NBLK, 32], F32); be = sb(tmp, "mbe", [128, NBLK], F32)
            wi13 = sb(tmp, "mwi13", [128, NBLK], F32)
            pc_, pcn_ = nextbank()
            op("pe", lambda e: e.matmul(pc_[:, 0:32], lhsT=CB["ones"][:], rhs=Asum[:], start=True, stop=True), r=["Asum", "kb_ones"], w=[pcn_])
            op("dve", lambda e: e.tensor_scalar(out=cnt[:], in0=pc_[:, 0:32], scalar1=float(BLK - 1), scalar2=float(1.0 / BLK), op0=ALU.add, op1=ALU.mult), r=[pcn_], w=["mcnt"])
            op("dve", lambda e: e.tensor_scalar(out=nb_i[:], in0=cnt[:], scalar1=-0.498, scalar2=None, op0=ALU.add), r=["mcnt"], w=["mnbi"])
            op("dve", lambda e: e.tensor_copy(out=padc[:], in_=nb_i[:]), r=["mnbi"], w=["mpadc"])
            op("dve", lambda e: e.tensor_scalar(out=padc[:], in0=padc[:], scalar1=float(BLK), scalar2=None, op0=ALU.mult), r=["mpadc"], w=["mpadc"])
            op("dve", lambda e: e.memset(z32[:], 0.0), w=["mz32"])
            op("dve", lambda e: e.tensor_tensor_scan(out=pend[:], data0=z32[:], data1=padc[:], initial=0.0, op0=ALU.add, op1=ALU.add), r=["mz32", "mpadc"], w=["mpend"])
            op("dve", lambda e: e.tensor_tensor(out=pstart[:], in0=pend[:], in1=padc[:], op=ALU.subtract), r=["mpend", "mpadc"], w=["mpstart"])
            for kk, (sel, di) in enumerate(((sel1, d1i), (sel2, d2i))):
                op("dve", lambda e: e.tensor_tensor(out=big[:], in0=rank[:], in1=pstart[:].unsqueeze(1).broadcast_to([128, NT, 32]), op=ALU.add), r=["rank", "mpstart"], w=["mbig"])
                op("dve", lambda e, sel=sel: e.tensor_tensor(out=big[:], in0=big[:], in1=sel[:], op=ALU.mult), r=["mbig", "sel%d" % (kk + 1)], w=["mbig"])
                op("dve", lambda e: e.tensor_reduce(out=dd[:], in_=big[:], axis=AX.X, op=ALU.add), r=["mbig"], w=["mdd"])
                op("dve", lambda e, di=di: e.tensor_copy(out=di[:], in_=dd[:]), r=["mdd"], w=["d%di" % (kk + 1)])
            for t in range(NT):
                op("dve", lambda e, t=t: e.tensor_scalar(out=tokf[:, t:t + 1], in0=C["piota"][:], scalar1=float(t * 128), scalar2=None, op0=ALU.add), r=["k_piota"], w=["mtokf"])
            op("dve", lambda e: e.tensor_copy(out=toki[:], in_=tokf[:]), r=["mtokf"], w=["mtoki"])
            op("dve", lambda e: e.memset(sti[:], SO), w=["msti"])
            op("sp", lambda e: e.dma_start(out=ST_d.rearrange("(p n) o -> p (n o)", p=128), in_=sti[:]), r=["msti"], w=["ST_d"], dsem=sc.dsem())
            NTd = min(NT, NG3_DEBUG * 4)
            fl = []
            for t in range(NTd):
                for di in (d1i, d2i):
                    fl.append(lambda e, t=t, di=di: e.indirect_dma_start(out=ST_d[:, :], out_offset=bass.IndirectOffsetOnAxis(ap=di[:, t:t + 1], axis=0),
                                                                         in_=toki[:, t:t + 1], in_offset=None))
            op("pool", fl, r=["d1i", "d2i", "mtoki", "ST_d"], w=["ST_d"], dsem=sc.dsem())
            op("dve", lambda e: e.tensor_tensor(out=cmpb[:], in0=pend[:].unsqueeze(1).broadcast_to([128, NBLK, 32]),
                                                in1=C["blkpos"][:].unsqueeze(2).broadcast_to([128, NBLK, 32]), op=ALU.is_le), r=["mpend", "k_blkpos"], w=["mcmp"])
            op("dve", lambda e: e.tensor_reduce(out=be[:], in_=cmpb[:], axis=AX.X, op=ALU.add), r=["mcmp"], w=["mbe"])
            op("dve", lambda e: e.tensor_scalar(out=wi13[:], in0=be[:], scalar1=128.0, scalar2=C["piota"][:, 0:1], op0=ALU.mult, op1=ALU.add), r=["mbe", "k_piota"], w=["mwi13"])
            op("dve", lambda e: e.tensor_copy(out=wi13i[:], in_=wi13[:]), r=["mwi13"], w=["mwi13i"])
            sc.barrier()
            tmp.close()
            w13f = I["w13"][0].rearrange("e (p c) n -> (e p) (c n)", c=8); w2f = I["w2"][0].rearrange("e (p j) n -> (e p) (j n)", j=4)
            op("sp", lambda e: e.dma_start(out=sidxA[:], in_=ST_d.rearrange("(b p s) o -> p b (s o)", p=128, s=NS_)), r=["ST_d"], w=["msidxA"], dsem=sc.dsem())
            wf13L = [sb(st, "mwf13_%d" % i, [128, 8, 1024], F32) for i in range(2)]; wf2L = [sb(st, "mwf2_%d" % i, [128, 4, 1024], F32) for i in range(2)]
            wb13 = sb(st, "mwb13", [128, 8, 1024], BF16); wb2 = sb(st, "mwb2", [128, 4, 1024], BF16)
            wsem13 = [sc.dsem() for _ in range(2)]; wsem2 = [sc.dsem() for _ in range(2)]
            for i_ in range(2):
                op("dve", lambda e, i_=i_: e.memset(wf13L[i_][:], 0.0), w=["mwf13_%d" % i_])
                op("dve", lambda e, i_=i_: e.memset(wf2L[i_][:], 0.0), w=["mwf2_%d" % i_])
            xg = [sb(st, "mxg%d" % i, [128, NS_, 1024], BF16) for i in range(2)]; xgsem = [sc.dsem() for _ in range(2)]
            xgT = sb(st, "mxgT", [128, 8, BLK], BF16)
            sg = sb(st, "msg", [128, BLK], F32); actT = sb(st, "mactT", [128, 4, BLK], BF16)
            yst2 = [sb(st, "myst%d" % i, [128, 1024], F32) for i in range(2)]; ysem2 = [sc.dsem() for _ in range(2)]
            ny = 0
            if NB_DEBUG < 1000:
                zf = sb(st, "mzf", [128, 1024], F32)
                op("dve", lambda e: e.memset(zf[:], 0.0), w=["mzf"])
                op("sp", [lambda e, t=t: e.dma_start(out=Y_d[t * 128:(t + 1) * 128, :], in_=zf[:]) for t in range(NSLOT // 128)], r=["mzf"], w=["Y_d"], dsem=sc.dsem())
            for blk in range(min(NBLK, NB_DEBUG)):
                sl = blk % 2
                op("pool", [lambda e, s_=s_, sl=sl, blk=blk: e.indirect_dma_start(out=xg[sl][:, s_, :], out_offset=None, in_=H2_d[:, :],
                                                                        in_offset=bass.IndirectOffsetOnAxis(ap=sidxA[:, blk * NS_ + s_:blk * NS_ + s_ + 1], axis=0)) for s_ in range(NS_)],
                   r=["msidxA", "H2_d", "H2_d_pad"], w=["mxg%d" % sl], dsem=xgsem[sl])
                wf13 = wf13L[sl]; wf2 = wf2L[sl]; wf13n = "mwf13_%d" % sl; wf2n = "mwf2_%d" % sl
                op("pool", lambda e, blk=blk, wf13=wf13: e.indirect_dma_start(out=wf13[:].rearrange("p c n -> p (c n)"), out_offset=None, in_=w13f,
                                                                             in_offset=bass.IndirectOffsetOnAxis(ap=wi13i[:, blk:blk + 1], axis=0),
                                                                             bounds_check=breg(e, 4095), oob_is_err=False),
                   r=["mwi13i"], w=[wf13n], dsem=wsem13[sl])
                op("pool", lambda e, blk=blk, wf2=wf2: e.indirect_dma_start(out=wf2[:].rearrange("p j n -> p (j n)"), out_offset=None, in_=w2f,
                                                                           in_offset=bass.IndirectOffsetOnAxis(ap=wi13i[:, blk:blk + 1], axis=0),
                                                                           bounds_check=breg(e, 4095), oob_is_err=False),
                   r=["mwi13i"], w=[wf2n], dsem=wsem2[sl])
                for c in range(8):
                    eng = "act" if c % 2 == 0 else "dve"
                    if eng == "act":
                        op("act", lambda e, c=c, wf13=wf13: e.activation(out=wb13[:, c, :], in_=wf13[:, c, :], func=AF.Copy), r=[wf13n], w=["mwb13_%d" % c])
                    else:
                        op("dve", lambda e, c=c, wf13=wf13: e.tensor_copy(out=wb13[:, c, :], in_=wf13[:, c, :]), r=[wf13n], w=["mwb13_%d" % c])
                for j in range(4):
                    if j % 2 == 0:
                        op("act", lambda e, j=j, wf2=wf2: e.activation(out=wb2[:, j, :], in_=wf2[:, j, :], func=AF.Copy), r=[wf2n], w=["mwb2_%d" % j])
                    else:
                        op("dve", lambda e, j=j, wf2=wf2: e.tensor_copy(out=wb2[:, j, :], in_=wf2[:, j, :]), r=[wf2n], w=["mwb2_%d" % j])
                for s_ in range(NS_):
                    for half in range(2):
                        bank = PS[half]; bn = psn[half]
                        bv = bank[:].bitcast(BF16)
                        for c4 in range(4):
                            c = half * 4 + c4
                            op("pe", lambda e, c=c, c4=c4, s_=s_, sl=sl, bv=bv: e.transpose(out=bv[:, c4 * 128:(c4 + 1) * 128], in_=xg[sl][:, s_, :].rearrange("p (q c) -> p c q", c=8)[:, c, :], identity=CB["ident"][:]),
                               r=["mxg%d" % sl, "kb_ident"], w=[bn])
                        op("act", lambda e, half=half, s_=s_, bv=bv: e.activation(out=xgT[:, half * 4:half * 4 + 4, s_ * 128:(s_ + 1) * 128], in_=bv[:, 0:512].rearrange("p (c q) -> p c q", q=128), func=AF.Copy),
                           r=[bn], w=["mxgT"])
                for m in range(4):
                    pg, pgn = nextbank(); pu, pun = nextbank()
                    for c in range(8):
                        op("pe", lambda e, c=c, m=m, pg=pg: e.matmul(pg[:, 0:BLK], lhsT=wb13[:, c, 0:512].rearrange("p (q j) -> p j q", j=4)[:, m, :], rhs=xgT[:, c, :], start=(c == 0), stop=(c == 7)),
                           r=["mxgT", "mwb13_%d" % c], w=[pgn])
                    for c in range(8):
                        op("pe", lambda e, c=c, m=m, pu=pu: e.matmul(pu[:, 0:BLK], lhsT=wb13[:, c, 512:1024].rearrange("p (q j) -> p j q", j=4)[:, m, :], rhs=xgT[:, c, :], start=(c == 0), stop=(c == 7)),
                           r=["mxgT", "mwb13_%d" % c], w=[pun])
                    op("act", lambda e, pg=pg: e.activation(out=sg[:], in_=pg[:, 0:BLK], func=AF.Silu), r=[pgn], w=["msg"])
                    op("dve", lambda e, m=m, pu=pu: e.tensor_tensor(out=actT[:, m, :], in0=sg[:], in1=pu[:, 0:BLK], op=ALU.mult), r=["msg", pun], w=["mactT"])
                for s_ in range(NS_):
                    ys = ny % 2; ny += 1
                    for nh in range(2):
                        py, pyn = nextbank()
                        for j in range(4):
                            op("pe", lambda e, j=j, s_=s_, nh=nh, py=py: e.matmul(py[:], lhsT=actT[:, j, s_ * 128:(s_ + 1) * 128], rhs=wb2[:, j, nh * 512:(nh + 1) * 512], start=(j == 0), stop=(j == 3)),
                               r=["mactT", "mwb2_%d" % j], w=[pyn])
                        op("act", lambda e, nh=nh, ys=ys, py=py: e.activation(out=yst2[ys][:, nh * 512:(nh + 1) * 512], in_=py[:], func=AF.Copy), r=[pyn], w=["myst%d" % ys])
                    op("sp", lambda e, blk=blk, s_=s_, ys=ys: e.dma_start(out=Y_d[blk * BLK:(blk + 1) * BLK, :].rearrange("(p s) d -> p s d", s=NS_)[:, s_, :], in_=yst2[ys][:]),
                       r=["myst%d" % ys], w=["Y_d"], dsem=ysem2[ys])
            y1 = [sb(st, "my1_%d" % i, [128, 1024], F32) for i in range(2)]; y2 = [sb(st, "my2_%d" % i, [128, 1024], F32) for i in range(2)]
            xx = [sb(st, "mxx%d" % i, [128, 1024], F32) for i in range(2)]
            csem = [sc.dsem() for _ in range(2)]; c2sem = [sc.dsem() for _ in range(2)]; osem2 = [sc.dsem() for _ in range(2)]
            for t in range(NTd):
                sl = t % 2
                op("pool", [lambda e, t=t, sl=sl: e.indirect_dma_start(out=y1[sl][:], out_offset=None, in_=Y_d[:, :], in_offset=bass.IndirectOffsetOnAxis(ap=d1i[:, t:t + 1], axis=0)),
                            lambda e, t=t, sl=sl: e.indirect_dma_start(out=y2[sl][:], out_offset=None, in_=Y_d[:, :], in_offset=bass.IndirectOffsetOnAxis(ap=d2i[:, t:t + 1], axis=0))],
                   r=["Y_d", "d1i", "d2i"], w=["my1_%d" % sl, "my2_%d" % sl], dsem=csem[sl])
                op("sp", lambda e, t=t, sl=sl: e.dma_start(out=xx[sl][:], in_=X1_d[t * 128:(t + 1) * 128, :]), r=["X1_d"], w=["mxx%d" % sl], dsem=c2sem[sl])
                op("dve", lambda e, t=t, sl=sl: e.scalar_tensor_tensor(out=xx[sl][:], in0=y1[sl][:], scalar=gates[:, t, 0:1], in1=xx[sl][:], op0=ALU.mult, op1=ALU.add),
                   r=["my1_%d" % sl, "gates", "mxx%d" % sl], w=["mxx%d" % sl])
                op("dve", lambda e, t=t, sl=sl: e.scalar_tensor_tensor(out=xx[sl][:], in0=y2[sl][:], scalar=gates[:, t, 1:2], in1=xx[sl][:], op0=ALU.mult, op1=ALU.add),
                   r=["my2_%d" % sl, "gates", "mxx%d" % sl], w=["mxx%d" % sl])
                op("sp", lambda e, t=t, sl=sl: e.dma_start(out=out[t * 128:(t + 1) * 128, :], in_=xx[sl][:]), r=["mxx%d" % sl], w=["out"], dsem=osem2[sl])
            sc.barrier()

    sc.barrier()
    sc.emit()
    es.close()
    return nc


def make_in_maps(inputs):
    maps = []
    for core in range(8):
        b, par = core // 2, core % 2
        x = np.ascontiguousarray(inputs["x"][b])
        pos = np.ascontiguousarray(inputs["positions"][b]).astype(np.int32)
        own = np.arange(S).reshape(S // 128, 128)[par::2].reshape(-1)
        m = {"x_full": x, "x_own": np.ascontiguousarray(x[own]), "pos_full": pos.reshape(1, S),
             "pos_own": np.ascontiguousarray(pos[own]).reshape(1, SO)}
        for k in PARAMS:
            m[k] = np.ascontiguousarray(inputs[k]).astype(np.float32)
        for k, v in host_consts(par).items():
            m["c_" + k] = v
        maps.append(m)
    return maps


def kernel(**inputs):
    inputs = {k: np.asarray(v) for k, v in inputs.items()}
    nc = build()
    maps = make_in_maps(inputs)
    res = run_bass_kernel_spmd(nc, maps, core_ids=list(range(8)))
    outp = np.zeros((4, S, D), np.float32)
    for core in range(8):
        b, par = core // 2, core % 2
        own = np.arange(S).reshape(S // 128, 128)[par::2].reshape(-1)
        outp[b, own] = res.results[core]["out"]
    return outp
```

```python
import numpy as np
from contextlib import ExitStack
import concourse.bass as bass
import concourse.mybir as mybir
from concourse.bass_utils import run_bass_kernel_spmd

F32 = mybir.dt.float32; BF16 = mybir.dt.bfloat16; I32 = mybir.dt.int32
ALU = mybir.AluOpType; AF = mybir.ActivationFunctionType; AX = mybir.AxisListType

D = 1024; S = 8192; SO = 4096; NCOL = 6216
EPS = 1e-6
BIG = 30000.0
TWO_PI = 6.283185307179586
PI_HI = 6.28125
PI_LO = TWO_PI - PI_HI
NBIS = 16
BLK = 512
NS_ = BLK // 128
NBLK = (2 * SO) // BLK + 32
NSLOT = NBLK * BLK
TOPK = 256
NG_DEBUG = 1000
CUT_DEBUG = 99
NQ_DEBUG = 1000
NG2_DEBUG = 1000
NG3_DEBUG = 1000
NB_DEBUG = 1000
HE_CUT = 99


class Sched:
    def __init__(s, nc, es):
        s.nc = nc; s.es = es
        s.sems = {}; s.cnt = {}
        s.ops = {e: [] for e in ("pe", "act", "dve", "pool", "sp")}
        s.waited = {e: {} for e in s.ops}
        s.lw = {}; s.lr = {}
        for e in ("pe", "act", "dve", "pool"):
            s.newsem("E_" + e)
        s.nd = 0

    def newsem(s, name):
        s.sems[name] = s.es.enter_context(s.nc.semaphore(name)); s.cnt[name] = 0
        return name

    def dsem(s):
        s.nd += 1
        return s.newsem("D%d" % s.nd)

    def op(s, eng, fn, r=(), w=(), dsem=None):
        deps = {}
        def add(t):
            if t is None: return
            k, v, e = t
            if deps.get(k, (0, None))[0] < v: deps[k] = (v, e)
        for x in r:
            add(s.lw.get(x))
            if x.startswith("ps"):
                for k, (v, e) in s.lr.get(x, {}).items():
                    if e != eng: add((k, v, e))
        for x in w:
            add(s.lw.get(x))
            for k, (v, e) in s.lr.get(x, {}).items():
                add((k, v, e))
        fns = fn if isinstance(fn, list) else [fn]
        if dsem is None:
            sk = "E_" + eng; s.cnt[sk] += 1; inc = 1
        else:
            sk = dsem; s.cnt[sk] += 16 * len(fns); inc = 16
        tok = (sk, s.cnt[sk], eng)
        wd = s.waited[eng]; waits = []
        for k, (v, e) in deps.items():
            if eng == "pe" and k == "E_pe": continue
            if wd.get(k, 0) >= v: continue
            waits.append((k, v)); wd[k] = v
        for i, f in enumerate(fns):
            s.ops[eng].append((f, waits if i == 0 else [], sk, inc))
        for x in w:
            s.lw[x] = tok; s.lr[x] = {}
        for x in r:
            d = s.lr.setdefault(x, {})
            if d.get(sk, (0, None))[0] < tok[1]: d[sk] = (tok[1], eng)
        return tok

    def barrier(s):
        for eng in s.ops:
            wd = s.waited[eng]; waits = []
            for k, c in s.cnt.items():
                if c > 0 and wd.get(k, 0) < c and not (eng == "pe" and k == "E_pe"):
                    waits.append((k, c)); wd[k] = c
            if waits:
                s.ops[eng].append((None, waits, None, 0))

    def emit(s):
        nc = s.nc
        def replay(eng, e):
            for fn, waits, sk, inc in s.ops[eng]:
                for k, v in waits:
                    e.wait_ge(s.sems[k], v)
                if fn is not None:
                    fn(e).then_inc(s.sems[sk], inc)
        with nc.Block() as block:
            @block.tensor
            def _(e): replay("pe", e)
            @block.scalar
            def _(e): replay("act", e)
            @block.vector
            def _(e): replay("dve", e)
            @block.gpsimd
            def _(e): replay("pool", e)
            @block.sync
            def _(e): replay("sp", e)


def host_consts(par):
    c = {}
    c["ident"] = np.eye(128, dtype=np.float32)
    p = np.arange(128)
    c["triu"] = (p[:, None] < p[None, :]).astype(np.float32)
    c["ones"] = np.ones((128, 128), np.float32)
    rt = np.zeros((128, 128), np.float32)
    for d in range(16):
        rt[d + 16, d] = -1.0; rt[d, d + 16] = 1.0
    c["rtm"] = rt
    ri = np.zeros((128, 128), np.float32)
    for base in (0, 64):
        for d in range(8):
            ri[base + d + 8, base + d] = -1.0; ri[base + d, base + d + 8] = 1.0
    c["rti"] = ri
    c["blkones"] = (p[:, None] // 64 == p[None, :] // 64).astype(np.float32)
    invm = np.zeros((128, 1), np.float32)
    f = (np.float32(500000.0) ** (-np.arange(0, 32, 2, dtype=np.float32) / np.float32(32))).astype(np.float32)
    for d in range(32): invm[d, 0] = f[d % 16]
    invi = np.zeros((128, 1), np.float32)
    fi = (np.float32(500000.0) ** (-np.arange(0, 16, 2, dtype=np.float32) / np.float32(16))).astype(np.float32)
    for base in (0, 64):
        for d in range(16): invi[base + d, 0] = fi[d % 8]
    c["invf"] = np.concatenate([invm, invi], axis=1)
    sel = np.zeros((128, 2), np.float32); sel[:, par] = 1.0
    c["sel"] = sel
    q = np.arange(128); kk = np.arange(256)
    m = np.zeros((128, 256), np.float32)
    if par == 0:
        adm = np.zeros((128, 256), bool)
        adm[:, :128] = (kk[None, :128] // 64) <= (q[:, None] // 64)
    else:
        adm = np.ones((128, 256), bool)
        adm[:, 128:] = (kk[None, :128] // 64) <= (q[:, None] // 64)
    m[~adm] = -BIG
    c["amask"] = m
    c["piota"] = np.arange(128, dtype=np.float32).reshape(128, 1)
    c["blkpos"] = np.tile((np.arange(NBLK, dtype=np.float32) * BLK)[None, :], (128, 1))
    return c


CONST_SHAPES = {"ident": [128, 128], "triu": [128, 128], "ones": [128, 128], "rtm": [128, 128], "rti": [128, 128],
                "blkones": [128, 128], "invf": [128, 2], "sel": [128, 2], "amask": [128, 256], "piota": [128, 1],
                "blkpos": [128, NBLK]}

PARAMS = {"norm1_g": [1, 1024], "w_in": [1, 1024, NCOL], "conv_w": [1, 4, 1024], "conv_b": [1, 1024],
          "lru_w_r": [1, 8, 128, 128], "lru_b_r": [1, 8, 128], "lru_w_i": [1, 8, 128, 128], "lru_b_i": [1, 8, 128],
          "lru_a_param": [1, 1024], "q_norm_g": [1, 128], "k_norm_g": [1, 128], "idx_k_norm_g": [1, 64],
          "w_lru_out": [1, 1024, 1024], "w_attn_out": [1, 1024, 1024], "w_o": [1, 1024, 1024], "norm2_g": [1, 1024],
          "w_grp": [1, 1024, 4], "b_grp": [1, 4], "w_exp": [1, 1024, 32], "b_exp": [1, 32],
          "w13": [1, 32, 1024, 1024], "w2": [1, 32, 512, 1024]}


def build(stage=99, dbg=False):
    nc = bass.Bass("TRN2", target_bir_lowering=False)
    es = ExitStack()
    sc = Sched(nc, es)
    op = sc.op
    I = {}
    I["x_full"] = nc.dram_tensor("x_full", [S, D], F32, kind="ExternalInput").ap()
    I["x_own"] = nc.dram_tensor("x_own", [SO, D], F32, kind="ExternalInput").ap()
    I["pos_full"] = nc.dram_tensor("pos_full", [1, S], I32, kind="ExternalInput").ap()
    I["pos_own"] = nc.dram_tensor("pos_own", [1, SO], I32, kind="ExternalInput").ap()
    for k, shp in PARAMS.items():
        I[k] = nc.dram_tensor(k, shp, F32, kind="ExternalInput").ap()
    for k, shp in CONST_SHAPES.items():
        I["c_" + k] = nc.dram_tensor("c_" + k, shp, F32, kind="ExternalInput").ap()
    out = nc.dram_tensor("out", [SO, D], F32, kind="ExternalOutput").ap()
    def scratch(name, shp, dt):
        if dbg and name in dbg:
            return nc.dram_tensor(name, shp, dt, kind="ExternalOutput").ap()
        return nc.dram_tensor(name, shp, dt).ap()
    hT_d = scratch("hT_d", [8, 128, S], BF16)
    kT_d = scratch("kT_d", [2, 128, S], BF16)
    kiT_d = scratch("kiT_d", [128, S], BF16)
    V_d = scratch("V_d", [S, 256], BF16)
    qT_d = scratch("qT_d", [8, 128, SO], BF16)
    qiT_d = scratch("qiT_d", [4, 128, SO], BF16)
    yaT_d = scratch("yaT_d", [8, 128, SO], BF16)
    ybT_d = scratch("ybT_d", [8, 128, SO], BF16)
    mgT_d = scratch("mgT_d", [16, 128, SO], BF16)
    X1_d = scratch("X1_d", [SO, D], F32)
    H2_d = scratch("H2_d", [SO + 128, D], BF16)
    Y_d = scratch("Y_d", [NSLOT, D], BF16)
    ST_d = scratch("ST_d", [NSLOT, 1], I32)
    D_ = {}

    _bregs = {}
    def breg(e, val):
        if val not in _bregs:
            _bregs[val] = e.to_reg(val)
        return _bregs[val]

    def sb(stack, name, shp, dt=F32):
        return stack.enter_context(nc.sbuf_tensor(name, shp, dt))

    PS = [es.enter_context(nc.psum_tensor("ps%d" % i, [128, 512], F32)) for i in range(8)]
    psn = ["ps%d" % i for i in range(8)]
    rot = {"i": 0}
    def nextbank(lo=2, hi=8):
        i = lo + rot["i"] % (hi - lo); rot["i"] += 1
        return PS[i], psn[i]

    C = {}
    cs = sc.dsem()
    TMPC = ("triu", "ones", "rtm", "rti", "blkones")
    CB = {}
    for k in ("ident", "triu", "ones", "rtm", "rti", "blkones"):
        CB[k] = sb(es, "kb_" + k, [128, 128], BF16)
    wq = sb(es, "wq", [128, 32, 8], F32)
    for k, shp in CONST_SHAPES.items():
        if k not in TMPC:
            C[k] = sb(es, "k_" + k, shp, F32)
    with ExitStack() as tst:
        for k in TMPC:
            C[k] = sb(tst, "k_" + k, CONST_SHAPES[k], F32)
        op("sp", [lambda e, k=k: e.dma_start(out=C[k][:], in_=I["c_" + k]) for k in CONST_SHAPES], w=["k_" + k for k in CONST_SHAPES], dsem=cs)
        for k in ("ident", "triu", "ones", "rtm", "rti", "blkones"):
            op("dve", lambda e, k=k: e.tensor_copy(out=CB[k][:], in_=C[k][:]), r=["k_" + k], w=["kb_" + k])
        sc.barrier()

    def dma_T(dst, src_row, n, name=None, sem=None):
        fn = lambda e: e.dma_start(out=dst, in_=src_row.rearrange("(c p) -> p c", p=128), allow_slow_non_contiguous=True)
        if sem is not None:
            op("sp", fn, w=[name], dsem=sem)
        return fn

    def rope_tables(stk, pos_ap, g, ntok, pfx):
        T = {}
        posi = sb(stk, pfx + "posi", [128, ntok], I32)
        posf = sb(stk, pfx + "posf", [128, ntok], F32)
        ang = sb(stk, pfx + "ang", [128, ntok], F32)
        kq = sb(stk, pfx + "kq", [128, ntok], I32)
        kf = sb(stk, pfx + "kf", [128, ntok], F32)
        for nm in ("cm", "sm", "ci", "si"):
            T[nm] = sb(stk, pfx + nm, [128, ntok], F32)
        return dict(T=T, posi=posi, posf=posf, ang=ang, kq=kq, kf=kf, pfx=pfx, sem=sc.dsem(), pos=pos_ap, ntok=ntok)

    def rope_fill(RT_, g):
        T = RT_["T"]; pfx = RT_["pfx"]; ntok = RT_["ntok"]
        posi, posf, ang, kq, kf = RT_["posi"], RT_["posf"], RT_["ang"], RT_["kq"], RT_["kf"]
        src = RT_["pos"][0, g * ntok:(g + 1) * ntok].partition_broadcast(128)
        op("sp", lambda e: e.dma_start(out=posi[:], in_=src), w=[pfx + "posi"], dsem=RT_["sem"])
        op("dve", lambda e: e.tensor_copy(out=posf[:], in_=posi[:]), r=[pfx + "posi"], w=[pfx + "posf"])
        for col, (sn, cn) in enumerate((("sm", "cm"), ("si", "ci"))):
            for nm, shift in ((sn, 0.0), (cn, np.pi / 2)):
                t = T[nm]; tn = pfx + nm
                op("dve", lambda e, col=col, shift=shift, t=t: e.tensor_scalar(out=t[:], in0=posf[:], scalar1=C["invf"][:, col:col + 1],
                                                                              scalar2=float(shift), op0=ALU.mult, op1=ALU.add),
                   r=[pfx + "posf", "k_invf"], w=[tn])
                op("dve", lambda e, t=t: e.tensor_scalar(out=kq[:], in0=t[:], scalar1=float(1.0 / TWO_PI), scalar2=None, op0=ALU.mult),
                   r=[tn], w=[pfx + "kq"])
                op("dve", lambda e: e.tensor_copy(out=kf[:], in_=kq[:]), r=[pfx + "kq"], w=[pfx + "kf"])
                yield
                op("dve", lambda e, t=t: e.scalar_tensor_tensor(out=t[:], in0=kf[:], scalar=float(-PI_HI), in1=t[:], op0=ALU.mult, op1=ALU.add),
                   r=[pfx + "kf", tn], w=[tn])
                op("dve", lambda e, t=t: e.scalar_tensor_tensor(out=t[:], in0=kf[:], scalar=float(-PI_LO), in1=t[:], op0=ALU.mult, op1=ALU.add),
                   r=[pfx + "kf", tn], w=[tn])
                op("dve", lambda e, t=t: e.tensor_scalar(out=t[:], in0=t[:], scalar1=3.1415925, scalar2=-3.1415925, op0=ALU.min, op1=ALU.max),
                   r=[tn], w=[tn])
                yield
        for nm in ("sm", "cm", "si", "ci"):
            op("act", lambda e, t=T[nm]: e.activation(out=t[:], in_=t[:], func=AF.Sin), r=[pfx + nm], w=[pfx + nm])
        yield

    def make_xT(stk, pfx, nslot=2):
        R_ = dict(pfx=pfx, n=0, nslot=nslot)
        R_["xin"] = [sb(stk, pfx + "xin%d" % i, [128, 4, D], BF16) for i in range(nslot)]
        R_["xn"] = [sb(stk, pfx + "xn%d" % i, [128, 4, D], BF16) for i in range(nslot)]
        R_["junk"] = sb(stk, pfx + "junk", [128, D], BF16)
        R_["ss"] = sb(stk, pfx + "ss", [128, 8], F32)
        R_["sems"] = [sc.dsem() for _ in range(nslot)]
        return R_

    def xT_group(R_, x_ap, g, dst, dstname):
        pfx = R_["pfx"]; sl = R_["n"] % R_["nslot"]; R_["n"] += 1
        xin = R_["xin"][sl]; xn = R_["xn"][sl]; ss = R_["ss"]; junk = R_["junk"]
        xinn = pfx + "xin%d" % sl; xnn = pfx + "xn%d" % sl
        src = x_ap[g * 512:(g + 1) * 512, :].rearrange("(t p) d -> p t d", p=128)
        op("pool", [lambda e, t=t: e.dma_start(out=xin[:, t, :], in_=src[:, t, :]) for t in range(4)],
           w=[xinn + "_%d" % t for t in range(4)], dsem=R_["sems"][sl])
        for t in range(4):
            op("act", lambda e, t=t: e.activation(out=junk[:], in_=xin[:, t, :], func=AF.Square, accum_out=ss[:, t:t + 1]),
               r=[xinn + "_%d" % t], w=[pfx + "junk", pfx + "ss%d" % t])
            op("act", lambda e, t=t: e.activation(out=ss[:, 4 + t:5 + t], in_=ss[:, t:t + 1], func=AF.Ln, scale=float(1.0 / D), bias=float(EPS)),
               r=[pfx + "ss%d" % t], w=[pfx + "sr%d" % t])
            op("act", lambda e, t=t: e.activation(out=ss[:, 4 + t:5 + t], in_=ss[:, 4 + t:5 + t], func=AF.Exp, scale=-0.5), r=[pfx + "sr%d" % t], w=[pfx + "sr%d" % t])
            op("dve", lambda e, t=t: e.tensor_scalar(out=xn[:, t, :], in0=xin[:, t, :], scalar1=ss[:, 4 + t:5 + t], scalar2=None, op0=ALU.mult),
               r=[xinn + "_%d" % t, pfx + "sr%d" % t], w=[xnn + "_%d" % t])
            yield
        for cp in range(4):
            bank = PS[cp % 2]; bn = psn[cp % 2]
            bv = bank[:].bitcast(BF16)
            for cc in range(2):
                c = cp * 2 + cc
                for t in range(4):
                    op("pe", lambda e, c=c, t=t, cc=cc, bv=bv: e.transpose(out=bv[:, cc * 512 + t * 128: cc * 512 + (t + 1) * 128],
                                                                      in_=xn[:, t, c * 128:(c + 1) * 128], identity=CB["ident"][:]),
                       r=[xnn + "_%d" % t, "kb_ident"], w=[bn])
            eng = "act" if cp % 2 == 0 else "dve"
            if eng == "act":
                op("act", lambda e, cp=cp, bv=bv: e.activation(out=dst[:, 2 * cp:2 * cp + 2, :], in_=bv.rearrange("p (c n) -> p c n", c=2), func=AF.Copy),
                   r=[bn], w=[dstname])
            else:
                op("dve", lambda e, cp=cp, bv=bv: e.tensor_copy(out=dst[:, 2 * cp:2 * cp + 2, :], in_=bv.rearrange("p (c n) -> p c n", c=2)),
                   r=[bn], w=[dstname])
            yield

    def load_w_cols(dst, dstname, w2d, c0, ncols, gsb, gname, sem, dcol=0):
        src = w2d.rearrange("(c p) n -> p c n", p=128)
        op("pool", [lambda e, c=c: e.dma_start(out=dst[:, c, dcol:dcol + ncols], in_=src[:, c, c0:c0 + ncols]) for c in range(8)],
           w=[dstname + "_%d" % c for c in range(8)], dsem=sem)
        if gsb is not None:
            for c in range(8):
                op("dve", lambda e, c=c: e.tensor_scalar(out=dst[:, c, dcol:dcol + ncols], in0=dst[:, c, dcol:dcol + ncols],
                                                         scalar1=gsb[:, c:c + 1], scalar2=None, op0=ALU.mult),
                   r=[dstname + "_%d" % c, gname], w=[dstname + "_%d" % c])

    def wdeps(name):
        return [name + "_%d" % c for c in range(8)]

    g1 = sb(es, "g1", [128, 8], F32)
    dma_T(g1[:], I["norm1_g"][0], 8, "g1", sc.dsem())

    def head_epilogue(stk_t, pb, pbn, gcol, gname, ones_lhsT, ones_name, rt_lhsT, rt_name, Ct, Cn, St, Sn, dst, dstnames, norm=True, tp="he"):
        t0, sq, rtt, tnb, o1, o2 = stk_t
        if norm:
            op("act", lambda e: e.activation(out=sq[:], in_=pb[:], func=AF.Square), r=[pbn], w=[tp + "sq"])
            op("dve", lambda e: e.tensor_scalar(out=t0[:], in0=pb[:], scalar1=gcol, scalar2=None, op0=ALU.mult), r=[pbn, gname, tp + "sq"], w=[tp + "t0"])
            yield
            p2, p2n = nextbank()
            op("pe", lambda e: e.matmul(p2[:], lhsT=ones_lhsT, rhs=sq[:], start=True, stop=True), r=[tp + "sq", ones_name], w=[p2n])
            nrm = float(1.0 / 128) if ones_name == "kb_ones" else float(1.0 / 64)
            op("act", lambda e: e.activation(out=rtt[:], in_=p2[:], func=AF.Ln, scale=nrm, bias=float(EPS)), r=[p2n], w=[tp + "rt"])
            yield
            op("act", lambda e: e.activation(out=rtt[:], in_=rtt[:], func=AF.Exp, scale=-0.5), r=[tp + "rt"], w=[tp + "rt"])
            op("dve", lambda e: e.tensor_tensor(out=tnb[:], in0=t0[:], in1=rtt[:], op=ALU.mult), r=[tp + "t0", tp + "rt"], w=[tp + "tnb"])
        else:
            op("act", lambda e: e.activation(out=tnb[:], in_=pb[:], func=AF.Copy), r=[pbn], w=[tp + "tnb"])
        yield
        op("dve", lambda e: e.tensor_tensor(out=o1[:], in0=tnb[:], in1=Ct[:], op=ALU.mult), r=[tp + "tnb", Cn], w=[tp + "o1"])
        yield
        p3, p3n = nextbank()
        op("pe", lambda e: e.matmul(p3[:], lhsT=rt_lhsT, rhs=tnb[:], start=True, stop=True), r=[tp + "tnb", rt_name], w=[p3n])
        op("dve", lambda e: e.tensor_tensor(out=o2[:], in0=p3[:], in1=St[:], op=ALU.mult), r=[p3n, Sn], w=[tp + "o2"])
        op("dve", lambda e: e.tensor_tensor(out=dst, in0=o1[:], in1=o2[:], op=ALU.add), r=[tp + "o1", tp + "o2"], w=dstnames)

    def he_tiles(stk, tp="he"):
        return (sb(stk, tp + "t0", [128, 512], F32), sb(stk, tp + "sq", [128, 512], BF16), sb(stk, tp + "rt", [128, 512], F32),
                sb(stk, tp + "tnb", [128, 512], BF16), sb(stk, tp + "o1", [128, 512], F32), sb(stk, tp + "o2", [128, 512], F32))

    if stage >= 1:
        with ExitStack() as st:
            W1 = sb(st, "W1", [128, 8, 1664], BF16)
            ws = sc.dsem()
            w_in2 = I["w_in"][0]
            load_w_cols(W1, "W1a", w_in2, 0, 1024, g1, "g1", sc.dsem(), 0)
            load_w_cols(W1, "W1b", w_in2, 3072, 512, g1, "g1", sc.dsem(), 1024)
            load_w_cols(W1, "W1c", w_in2, 4096, 64, g1, "g1", sc.dsem(), 1536)
            load_w_cols(W1, "W1d", w_in2, 4096, 64, g1, "g1", sc.dsem(), 1600)
            W1names = wdeps("W1a") + wdeps("W1b") + wdeps("W1c") + wdeps("W1d")
            psem = sc.dsem()
            cw = sb(st, "cw", [128, 4, 8], F32); cb = sb(st, "cb", [128, 8], F32)
            br = sb(st, "br", [128, 8], F32); bi = sb(st, "bi", [128, 8], F32); lap = sb(st, "lap", [128, 8], F32)
            nsp = sb(st, "nsp", [128, 8], F32)
            fl = [dma_T(cw[:, k, :], I["conv_w"][0, k], 8) for k in range(4)]
            fl.append(dma_T(cb[:], I["conv_b"][0], 8))
            fl.append(dma_T(br[:], I["lru_b_r"][0].rearrange("n d -> (n d)"), 8))
            fl.append(dma_T(bi[:], I["lru_b_i"][0].rearrange("n d -> (n d)"), 8))
            fl.append(dma_T(lap[:], I["lru_a_param"][0], 8))
            op("sp", fl, w=["cw0", "cw1", "cw2", "cw3", "cb", "br", "bi", "lap"], dsem=psem)
            op("act", lambda e: e.activation(out=nsp[:], in_=lap[:], func=AF.Exp, scale=-1.0), r=["lap"], w=["nsp"])
            op("act", lambda e: e.activation(out=nsp[:], in_=nsp[:], func=AF.Ln, bias=1.0), r=["nsp"], w=["nsp"])
            op("dve", lambda e: e.tensor_scalar(out=nsp[:], in0=nsp[:], scalar1=-8.0, scalar2=None, op0=ALU.mult), r=["nsp"], w=["nsp"])
            wr = sb(st, "wr", [128, 8, 128], BF16); wi = sb(st, "wi", [128, 8, 128], BF16)
            op("pool", [lambda e: e.dma_start(out=wr[:], in_=I["lru_w_r"][0].rearrange("n c d -> c n d")),
                        lambda e: e.dma_start(out=wi[:], in_=I["lru_w_i"][0].rearrange("n c d -> c n d"))], w=["wr", "wi"], dsem=sc.dsem())
            gk = sb(st, "gk", [128, 2], F32)
            op("sp", [lambda e: e.dma_start(out=gk[:, 0:1], in_=I["k_norm_g"][0].rearrange("(p o) -> p o", o=1)),
                      lambda e: e.dma_start(out=gk[0:64, 1:2], in_=I["idx_k_norm_g"][0].rearrange("(p o) -> p o", o=1)),
                      lambda e: e.dma_start(out=gk[64:128, 1:2], in_=I["idx_k_norm_g"][0].rearrange("(p o) -> p o", o=1))], w=["gk"], dsem=sc.dsem())
            XR = make_xT(st, "p1")
            xT = [sb(st, "p1xT%d" % i, [128, 8, 512], BF16) for i in range(2)]
            RP = rope_tables(st, I["pos_full"], 0, 512, "r1")
            HT = he_tiles(st)
            lxs = sb(st, "lxs", [128, 8, 515], F32)
            op("dve", lambda e: e.memset(lxs[:, :, 0:3], 0.0), w=["lxs%d" % n for n in range(8)])
            state = sb(st, "lstate", [128, 8], F32)
            op("dve", lambda e: e.memset(state[:], 0.0), w=["lstate%d" % n for n in range(8)])
            LS = [{nm: sb(st, "L%d" % k_ + nm, [128, 512], F32) for nm in ("xa", "r", "i", "a", "a2", "u", "h")} for k_ in range(3)]
            xabS = [sb(st, "L%dxab" % k_, [128, 512], BF16) for k_ in range(3)]
            hst = [sb(st, "hst%d" % i, [128, 8, 512], BF16) for i in range(2)]
            kst = [sb(st, "kst%d" % i, [128, 3, 512], BF16) for i in range(2)]
            vst = [sb(st, "vst%d" % i, [128, 4, 256], BF16) for i in range(2)]
            hsem = [sc.dsem() for _ in range(2)]; ksem = [sc.dsem() for _ in range(2)]; vsem = [sc.dsem() for _ in range(2)]

            def rr_merge(gens):
                gens = [g_ for g_ in gens if g_ is not None]
                while gens:
                    for g_ in list(gens):
                        try:
                            next(g_)
                        except StopIteration:
                            gens.remove(g_)

            def lru_chain(n, k_, sl, xTg, xTn):
                L = LS[k_]; xab = xabS[k_]; P_ = "L%d" % k_
                pb, pbn = nextbank()
                for c in range(8):
                    op("pe", lambda e, c=c: e.matmul(pb[:], lhsT=W1[:, c, n * 128:(n + 1) * 128], rhs=xTg[:, c, :], start=(c == 0), stop=(c == 7)),
                       r=[xTn, "W1a_%d" % c], w=[pbn])
                ln = "lxs%d" % n
                op("act", lambda e: e.activation(out=lxs[:, n, 3:515], in_=pb[:], func=AF.Copy), r=[pbn], w=[ln])
                yield
                op("dve", lambda e: e.tensor_scalar(out=L["xa"][:], in0=lxs[:, n, 3:515], scalar1=cw[:, 3, n:n + 1], scalar2=cb[:, n:n + 1],
                                                    op0=ALU.mult, op1=ALU.add), r=[ln, "cw3", "cb"], w=[P_ + "xa"])
                for k in range(3):
                    op("dve", lambda e, k=k: e.scalar_tensor_tensor(out=L["xa"][:], in0=lxs[:, n, k:k + 512], scalar=cw[:, k, n:n + 1], in1=L["xa"][:],
                                                                    op0=ALU.mult, op1=ALU.add), r=[ln, "cw%d" % k, P_ + "xa"], w=[P_ + "xa"])
                    yield
                op("dve", lambda e: e.tensor_copy(out=lxs[:, n, 0:3], in_=lxs[:, n, 512:515]), r=[ln], w=[ln])
                op("act", lambda e: e.activation(out=xab[:], in_=L["xa"][:], func=AF.Copy), r=[P_ + "xa"], w=[P_ + "xab"])
                yield
                pr, prn = nextbank(); pi_, pin = nextbank()
                op("pe", lambda e: e.matmul(pr[:], lhsT=wr[:, n, :], rhs=xab[:], start=True, stop=True), r=[P_ + "xab", "wr"], w=[prn])
                op("pe", lambda e: e.matmul(pi_[:], lhsT=wi[:, n, :], rhs=xab[:], start=True, stop=True), r=[P_ + "xab", "wi"], w=[pin])
                op("act", lambda e: e.activation(out=L["r"][:], in_=pr[:], func=AF.Sigmoid, bias=br[:, n:n + 1]), r=[prn, "br"], w=[P_ + "r"])
                op("act", lambda e: e.activation(out=L["i"][:], in_=pi_[:], func=AF.Sigmoid, bias=bi[:, n:n + 1]), r=[pin, "bi"], w=[P_ + "i"])
                yield
                op("act", lambda e: e.activation(out=L["a"][:], in_=L["r"][:], func=AF.Exp, scale=nsp[:, n:n + 1]), r=[P_ + "r", "nsp"], w=[P_ + "a"])
                yield
                op("act", lambda e: e.activation(out=L["a2"][:], in_=L["a"][:], func=AF.Square), r=[P_ + "a"], w=[P_ + "a2"])
                yield
                op("dve", lambda e: e.tensor_scalar(out=L["a2"][:], in0=L["a2"][:], scalar1=-1.0, scalar2=1.0000001, op0=ALU.mult, op1=ALU.add), r=[P_ + "a2"], w=[P_ + "a2"])
                yield
                op("act", lambda e: e.activation(out=L["a2"][:], in_=L["a2"][:], func=AF.Ln), r=[P_ + "a2"], w=[P_ + "a2"])
                yield
                op("act", lambda e: e.activation(out=L["a2"][:], in_=L["a2"][:], func=AF.Exp, scale=0.5), r=[P_ + "a2"], w=[P_ + "a2"])
                yield
                op("dve", lambda e: e.tensor_tensor(out=L["u"][:], in0=L["a2"][:], in1=L["i"][:], op=ALU.mult), r=[P_ + "a2", P_ + "i"], w=[P_ + "u"])
                yield
                op("dve", lambda e: e.tensor_tensor(out=L["u"][:], in0=L["u"][:], in1=L["xa"][:], op=ALU.mult), r=[P_ + "u", P_ + "xa"], w=[P_ + "u"])
                yield
                op("dve", lambda e: e.tensor_tensor_scan(out=L["h"][:], data0=L["a"][:], data1=L["u"][:], initial=state[:, n:n + 1],
                                                         op0=ALU.mult, op1=ALU.add), r=[P_ + "a", P_ + "u", "lstate%d" % n], w=[P_ + "h"])
                op("dve", lambda e: e.tensor_copy(out=state[:, n:n + 1], in_=L["h"][:, 511:512]), r=[P_ + "h"], w=["lstate%d" % n])
                yield
                op("act", lambda e: e.activation(out=hst[sl][:, n, :], in_=L["h"][:], func=AF.Copy), r=[P_ + "h"], w=["hst%d" % sl])
                yield

            def rope_chain(g):
                for _ in rope_fill(RP, g):
                    yield

            def khead_chain(j, sl, xTg, xTn):
                pb, pbn = nextbank()
                c0 = 1024 + j * 128 if j < 2 else 1536
                wn = "W1b_%d" if j < 2 else "W1c_%d"
                for c in range(8):
                    op("pe", lambda e, c=c: e.matmul(pb[:], lhsT=W1[:, c, c0:c0 + 128], rhs=xTg[:, c, :], start=(c == 0), stop=(c == 7)),
                       r=[xTn, wn % c, "W1d_%d" % c], w=[pbn])
                if j < 2:
                    gen = head_epilogue(HT, pb, pbn, gk[:, 0:1], "gk", CB["ones"][:], "kb_ones", CB["rtm"][:], "kb_rtm",
                                        RP["T"]["cm"], "r1cm", RP["T"]["sm"], "r1sm", kst[sl][:, j, :], ["kst%d" % sl])
                else:
                    gen = head_epilogue(HT, pb, pbn, gk[:, 1:2], "gk", CB["blkones"][:], "kb_blkones", CB["rti"][:], "kb_rti",
                                        RP["T"]["ci"], "r1ci", RP["T"]["si"], "r1si", kst[sl][:, j, :], ["kst%d" % sl])
                for _ in gen:
                    yield

            def v_chain(sl, xTg, xTn):
                for t in range(4):
                    pb, pbn = nextbank()
                    for c in range(8):
                        op("pe", lambda e, c=c, t=t, pb=pb: e.matmul(pb[:, 0:256], lhsT=xTg[:, c, t * 128:(t + 1) * 128], rhs=W1[:, c, 1280:1536], start=(c == 0), stop=(c == 7)),
                           r=[xTn, "W1b_%d" % c], w=[pbn])
                    op("act", lambda e, t=t, pb=pb: e.activation(out=vst[sl][:, t, :], in_=pb[:, 0:256], func=AF.Copy), r=[pbn], w=["vst%d" % sl])
                    yield

            NG1 = min(S // 512, NG_DEBUG)
            if NG1 > 0:
                for _ in xT_group(XR, I["x_full"], 0, xT[0], "p1xT0"): pass
            for g in range(NG1):
                sl = g % 2
                xTg = xT[sl]; xTn = "p1xT%d" % sl
                nxt = xT_group(XR, I["x_full"], g + 1, xT[1 - sl], "p1xT%d" % (1 - sl)) if g + 1 < NG1 else None
                rr_merge([lru_chain(0, 0, sl, xTg, xTn), lru_chain(1, 1, sl, xTg, xTn), lru_chain(2, 2, sl, xTg, xTn), rope_chain(g)])
                rr_merge([lru_chain(3, 0, sl, xTg, xTn), lru_chain(4, 1, sl, xTg, xTn), lru_chain(5, 2, sl, xTg, xTn), khead_chain(0, sl, xTg, xTn)])
                rr_merge([lru_chain(6, 0, sl, xTg, xTn), lru_chain(7, 1, sl, xTg, xTn), khead_chain(1, sl, xTg, xTn), nxt])
                rr_merge([khead_chain(2, sl, xTg, xTn), v_chain(sl, xTg, xTn)])
                op("sp", lambda e, g=g, sl=sl: e.dma_start(out=hT_d[:, :, g * 512:(g + 1) * 512].rearrange("c p t -> p c t"), in_=hst[sl][:]),
                   r=["hst%d" % sl], w=["hT_d"], dsem=hsem[sl])
                op("sp", [lambda e, g=g, sl=sl: e.dma_start(out=kT_d[:, :, g * 512:(g + 1) * 512].rearrange("c p t -> p c t"), in_=kst[sl][:, 0:2, :]),
                          lambda e, g=g, sl=sl: e.dma_start(out=kiT_d[:, g * 512:(g + 1) * 512], in_=kst[sl][:, 2, :])],
                   r=["kst%d" % sl], w=["kT_d", "kiT_d"], dsem=ksem[sl])
                op("sp", lambda e, g=g, sl=sl: e.dma_start(out=V_d[g * 512:(g + 1) * 512, :].rearrange("(t p) d -> p t d", p=128), in_=vst[sl][:]),
                   r=["vst%d" % sl], w=["V_d"], dsem=vsem[sl])
            sc.barrier()

    if stage >= 2:
        with ExitStack() as st:
            W2 = sb(st, "W2", [128, 8, 4616], BF16)
            w_in2 = I["w_in"][0]
            load_w_cols(W2, "W2a", w_in2, 1024, 2048, g1, "g1", sc.dsem(), 0)
            load_w_cols(W2, "W2b", w_in2, 3584, 512, g1, "g1", sc.dsem(), 2048)
            load_w_cols(W2, "W2c", w_in2, 4160, 2056, g1, "g1", sc.dsem(), 2560)
            gq = sb(st, "gq", [128, 1], F32)
            op("sp", lambda e: e.dma_start(out=gq[:], in_=I["q_norm_g"][0].rearrange("(p o) -> p o", o=1)), w=["gq"], dsem=sc.dsem())
            XR2 = make_xT(st, "p2", nslot=1)
            xT2 = [sb(st, "p2xT%d" % i, [128, 8, 512], BF16) for i in range(2)]
            RP2 = rope_tables(st, I["pos_own"], 0, 512, "r2")
            HT2 = [he_tiles(st, "h2"), he_tiles(st, "h3")]
            hh = [sb(st, "hh%d" % i, [128, 1024], BF16) for i in range(2)]
            hhsem = [sc.dsem() for _ in range(2)]
            GS = [{nm: sb(st, "G%d" % k_ + nm, [128, 512], F32) for nm in ("z2", "t", "sg", "hs")} for k_ in range(2)]
            yst = [sb(st, "yst0", [128, 8, 512], BF16)] * 2
            qst = [sb(st, "qst0", [128, 8, 512], BF16)] * 2
            qist = [sb(st, "qist0", [128, 4, 512], BF16)] * 2
            mst = [sb(st, "mst%d" % i, [128, 8, 512], BF16) for i in range(2)]
            ysem = [sc.dsem()] * 2; qsem = [sc.dsem()] * 2
            qisem = [sc.dsem()] * 2; msem = [sc.dsem() for _ in range(2)]

            def rr_merge2(gens):
                gens = [g_ for g_ in gens if g_ is not None]
                while gens:
                    for g_ in list(gens):
                        try:
                            next(g_)
                        except StopIteration:
                            gens.remove(g_)

            def gate_chain(n, k_, g, xTg, xTn):
                G = GS[k_]; P_ = "G%d" % k_
                op("sp", lambda e: e.dma_start(out=hh[k_][:], in_=hT_d[n, :, g * 1024:(g + 1) * 1024]), r=["hT_d"], w=["hh%d" % k_], dsem=hhsem[k_])
                pb, pbn = nextbank()
                for c in range(8):
                    op("pe", lambda e, c=c: e.matmul(pb[:], lhsT=W2[:, c, n * 128:(n + 1) * 128], rhs=xTg[:, c, :], start=(c == 0), stop=(c == 7)),
                       r=[xTn, "W2a_%d" % c], w=[pbn])
                op("act", lambda e: e.activation(out=G["z2"][:], in_=pb[:], func=AF.Copy), r=[pbn], w=[P_ + "z2"])
                yield
                op("act", lambda e: e.activation(out=G["t"][:], in_=G["z2"][:], func=AF.Square), r=[P_ + "z2"], w=[P_ + "t"])
                yield
                op("dve", lambda e: e.tensor_scalar(out=G["t"][:], in0=G["t"][:], scalar1=0.044715, scalar2=1.0, op0=ALU.mult, op1=ALU.add), r=[P_ + "t"], w=[P_ + "t"])
                yield
                op("dve", lambda e: e.tensor_tensor(out=G["t"][:], in0=G["t"][:], in1=G["z2"][:], op=ALU.mult), r=[P_ + "t", P_ + "z2"], w=[P_ + "t"])
                yield
                op("act", lambda e: e.activation(out=G["sg"][:], in_=G["t"][:], func=AF.Sigmoid, scale=1.5957691216), r=[P_ + "t"], w=[P_ + "sg"])
                yield
                op("dve", lambda e: e.tensor_tensor(out=G["sg"][:], in0=G["sg"][:], in1=G["z2"][:], op=ALU.mult), r=[P_ + "sg", P_ + "z2"], w=[P_ + "sg"])
                yield
                hv = hh[k_][:].rearrange("p (t two q) -> p t two q", two=2, q=128)
                hsv = G["hs"][:].rearrange("p (t q) -> p t q", q=128)
                op("dve", lambda e: e.tensor_scalar(out=hsv, in0=hv[:, :, 0, :], scalar1=C["sel"][:, 0:1], scalar2=None, op0=ALU.mult),
                   r=["hh%d" % k_, "k_sel"], w=[P_ + "hs"])
                yield
                op("dve", lambda e: e.scalar_tensor_tensor(out=hsv, in0=hv[:, :, 1, :], scalar=C["sel"][:, 1:2], in1=hsv, op0=ALU.mult, op1=ALU.add),
                   r=["hh%d" % k_, "k_sel", P_ + "hs"], w=[P_ + "hs"])
                yield
                op("dve", lambda e: e.tensor_tensor(out=yst[0][:, n, :], in0=G["hs"][:], in1=G["sg"][:], op=ALU.mult), r=[P_ + "hs", P_ + "sg"], w=["yst0"])
                yield

            def qhead_chain(hd, k_, xTg, xTn):
                pb, pbn = nextbank()
                for c in range(8):
                    op("pe", lambda e, c=c: e.matmul(pb[:], lhsT=W2[:, c, 1024 + hd * 128:1024 + (hd + 1) * 128], rhs=xTg[:, c, :], start=(c == 0), stop=(c == 7)),
                       r=[xTn, "W2a_%d" % c], w=[pbn])
                for _ in head_epilogue(HT2[k_], pb, pbn, gq[:, 0:1], "gq", CB["ones"][:], "kb_ones", CB["rtm"][:], "kb_rtm",
                                       RP2["T"]["cm"], "r2cm", RP2["T"]["sm"], "r2sm", qst[0][:, hd, :], ["qst0"], tp="h%d" % (2 + k_)):
                    yield

            def qidx_chain(hp, k_, xTg, xTn):
                pb, pbn = nextbank()
                for c in range(8):
                    op("pe", lambda e, c=c: e.matmul(pb[:], lhsT=W2[:, c, 2048 + hp * 128:2048 + (hp + 1) * 128], rhs=xTg[:, c, :], start=(c == 0), stop=(c == 7)),
                       r=[xTn, "W2b_%d" % c], w=[pbn])
                for _ in head_epilogue(HT2[k_], pb, pbn, None, None, None, None, CB["rti"][:], "kb_rti",
                                       RP2["T"]["ci"], "r2ci", RP2["T"]["si"], "r2si", qist[0][:, hp, :], ["qist0"], norm=False, tp="h%d" % (2 + k_)):
                    yield

            def widx_chain(g, xTg, xTn):
                for t in range(4):
                    pb, pbn = nextbank()
                    for c in range(8):
                        op("pe", lambda e, c=c, t=t, pb=pb: e.matmul(pb[:, 0:8], lhsT=xTg[:, c, t * 128:(t + 1) * 128], rhs=W2[:, c, 2560:2568], start=(c == 0), stop=(c == 7)),
                           r=[xTn, "W2c_%d" % c], w=[pbn])
                    op("act", lambda e, t=t, pb=pb: e.activation(out=wq[:, g * 4 + t, :], in_=pb[:, 0:8], func=AF.Copy, scale=float((8 ** -0.5) * (64 ** -0.5))),
                       r=[pbn], w=["wq"])
                    yield

            def mg_chain(g, mh, m8s, xTg, xTn, last):
                for m8 in m8s:
                    m = mh * 8 + m8
                    pb, pbn = nextbank()
                    for c in range(8):
                        op("pe", lambda e, c=c, m=m, pb=pb: e.matmul(pb[:], lhsT=W2[:, c, 2568 + m * 128:2568 + (m + 1) * 128], rhs=xTg[:, c, :], start=(c == 0), stop=(c == 7)),
                           r=[xTn, "W2c_%d" % c], w=[pbn])
                    op("act", lambda e, m8=m8, pb=pb: e.activation(out=mst[mh][:, m8, :], in_=pb[:], func=AF.Sigmoid), r=[pbn], w=["mst%d" % mh])
                    yield
                if last:
                    op("sp", lambda e: e.dma_start(out=mgT_d[mh * 8:(mh + 1) * 8, :, g * 512:(g + 1) * 512].rearrange("c p t -> p c t"), in_=mst[mh][:]),
                       r=["mst%d" % mh], w=["mgT_d"], dsem=msem[mh])

            def rope2_chain(g):
                for _ in rope_fill(RP2, g):
                    yield

            NG2 = min(SO // 512, NG2_DEBUG)
            if NG2 > 0:
                for _ in xT_group(XR2, I["x_own"], 0, xT2[0], "p2xT0"): pass
            for g in range(NG2):
                sl = g % 2
                xTg = xT2[sl]; xTn = "p2xT%d" % sl
                nxt = xT_group(XR2, I["x_own"], g + 1, xT2[1 - sl], "p2xT%d" % (1 - sl)) if g + 1 < NG2 else None
                rr_merge2([gate_chain(0, 0, g, xTg, xTn), gate_chain(1, 1, g, xTg, xTn), rope2_chain(g), mg_chain(g, 0, range(0, 4), xTg, xTn, False)])
                rr_merge2([gate_chain(2, 0, g, xTg, xTn), gate_chain(3, 1, g, xTg, xTn), qhead_chain(0, 0, xTg, xTn), qhead_chain(1, 1, xTg, xTn),
                           mg_chain(g, 0, range(4, 8), xTg, xTn, True)])
                rr_merge2([gate_chain(4, 0, g, xTg, xTn), gate_chain(5, 1, g, xTg, xTn), qhead_chain(2, 0, xTg, xTn), qhead_chain(3, 1, xTg, xTn),
                           mg_chain(g, 1, range(0, 4), xTg, xTn, False)])
                rr_merge2([gate_chain(6, 0, g, xTg, xTn), gate_chain(7, 1, g, xTg, xTn), qhead_chain(4, 0, xTg, xTn), qhead_chain(5, 1, xTg, xTn),
                           mg_chain(g, 1, range(4, 8), xTg, xTn, True)])
                op("sp", lambda e, g=g: e.dma_start(out=yaT_d[:, :, g * 512:(g + 1) * 512].rearrange("c p t -> p c t"), in_=yst[0][:]),
                   r=["yst0"], w=["yaT_d"], dsem=ysem[0])
                rr_merge2([qhead_chain(6, 0, xTg, xTn), qhead_chain(7, 1, xTg, xTn), widx_chain(g, xTg, xTn), nxt])
                op("sp", lambda e, g=g: e.dma_start(out=qT_d[:, :, g * 512:(g + 1) * 512].rearrange("c p t -> p c t"), in_=qst[0][:]),
                   r=["qst0"], w=["qT_d"], dsem=qsem[0])
                rr_merge2([qidx_chain(0, 0, xTg, xTn), qidx_chain(1, 1, xTg, xTn)])
                rr_merge2([qidx_chain(2, 0, xTg, xTn), qidx_chain(3, 1, xTg, xTn)])
                op("sp", lambda e, g=g: e.dma_start(out=qiT_d[:, :, g * 512:(g + 1) * 512].rearrange("c p t -> p c t"), in_=qist[0][:]),
                   r=["qist0"], w=["qiT_d"], dsem=qisem[0])
            sc.barrier()

    if stage >= 3:
        with ExitStack() as st:
            kT = sb(st, "kT", [128, 2, S], BF16); kiT = sb(st, "kiT", [128, S], BF16); Vs = sb(st, "Vs", [128, 64, 256], BF16)
            lsem = sc.dsem()
            fl = []
            NQ4 = min(4, (min(NG_DEBUG * 512, S) + 2047) // 2048)
            for j in range(2):
                for q4 in range(NQ4):
                    fl.append(lambda e, j=j, q4=q4: e.dma_start(out=kT[:, j, q4 * 2048:(q4 + 1) * 2048], in_=kT_d[j, :, q4 * 2048:(q4 + 1) * 2048]))
            for q4 in range(NQ4):
                fl.append(lambda e, q4=q4: e.dma_start(out=kiT[:, q4 * 2048:(q4 + 1) * 2048], in_=kiT_d[:, q4 * 2048:(q4 + 1) * 2048]))
            for q4 in range(NQ4):
                fl.append(lambda e, q4=q4: e.dma_start(out=Vs[:, q4 * 16:(q4 + 1) * 16, :], in_=V_d[q4 * 2048:(q4 + 1) * 2048, :].rearrange("(t p) d -> p t d", p=128)))
            op("sp", fl, r=["kT_d", "kiT_d", "V_d"], w=["kT", "kiT", "Vs"], dsem=lsem)
            scoresL = [sb(st, "scores%d" % i, [128, S], F32) for i in range(2)]
            junk = sb(st, "ajunk", [128, S], mybir.dt.uint8)
            maskb = [sb(st, "maskb%d" % i, [128, S], BF16) for i in range(2)]
            iselb = sb(st, "iselb", [128, 4, 128], BF16)
            for h4 in range(4):
                op("dve", lambda e, h4=h4: e.tensor_scalar(out=iselb[:, h4, :], in0=CB["ident"][:], scalar1=float(BIG), scalar2=None, op0=ALU.mult),
                   r=["kb_ident"], w=["iselb"])
            qTb = [sb(st, "qTb%d" % i, [128, 8, 128], BF16) for i in range(2)]
            qiTb = [sb(st, "qiTb%d" % i, [128, 4, 128], BF16) for i in range(2)]
            qsm = [sc.dsem() for _ in range(2)]; qsm2 = [sc.dsem() for _ in range(2)]
            Rt = [sb(st, "Rt%d" % i, [128, 512], BF16) for i in range(2)]
            diagw = sb(st, "diagw", [128, 8, 128], BF16)
            Et = [sb(st, "Et%d" % i, [128, 512], BF16) for i in range(3)]
            bs = sb(st, "bs", [128, 8], F32)
            Wk = sb(st, "Wk", [128, NBIS], F32)
            p2k = sb(st, "p2k", [128, NBIS], F32)
            for k in range(NBIS):
                op("dve", lambda e, k=k: e.memset(p2k[:, k:k + 1], float(0.5 ** (k + 1))), w=["p2k"])
            rden = sb(st, "rden", [128, 512], F32)
            ybst = [sb(st, "ybst0", [128, 8, 128], BF16)] * 2
            ybsem = [sc.dsem()] * 2
            SCALE = float(128 ** -0.5)
            cnts = {"R": 0, "E": 0, "z": 0}

            def gen_I(i):
                sl = i % 2
                nkt = 2 * i + 2; Lk = nkt * 128
                scores = scoresL[sl]; scn = "scores%d" % sl
                op("sp", lambda e: e.dma_start(out=qiTb[sl][:], in_=qiT_d[:, :, i * 128:(i + 1) * 128].rearrange("c p t -> p c t")),
                   r=["qiT_d"], w=["qiTb%d" % sl], dsem=qsm[sl])
                for h in range(8):
                    op("dve", lambda e, h=h: e.tensor_scalar(out=diagw[:, h, :], in0=CB["ident"][:], scalar1=wq[:, i, h:h + 1], scalar2=None, op0=ALU.mult),
                       r=["kb_ident", "wq"], w=["diagw"])
                yield
                for kg in range((nkt + 3) // 4):
                    W_ = min(4, nkt - 4 * kg) * 128
                    psc, pscn = PS[6 + kg % 2], psn[6 + kg % 2]
                    pend_ = None
                    for h in range(9):
                        if h < 8:
                            zi = 4 + cnts["z"] % 2; cnts["z"] += 1
                            pz, pzn = PS[zi], psn[zi]
                            pb0 = 64 * (h % 2)
                            op("pe", lambda e, h=h, kg=kg, W_=W_, pz=pz, pb0=pb0: e.matmul(pz[:, 0:W_], lhsT=qiTb[sl][pb0:pb0 + 64, h // 2, :],
                                                                                       rhs=kiT[pb0:pb0 + 64, kg * 512:kg * 512 + W_], start=True, stop=True),
                               r=["qiTb%d" % sl, "kiT"], w=[pzn])
                            rt_ = cnts["R"] % 2; cnts["R"] += 1
                            op("act", lambda e, W_=W_, pz=pz, rt_=rt_: e.activation(out=Rt[rt_][:, 0:W_], in_=pz[:, 0:W_], func=AF.Relu), r=[pzn], w=["Rt%d" % rt_])
                        if pend_ is not None:
                            ph, prt = pend_
                            op("pe", lambda e, ph=ph, W_=W_, psc=psc, prt=prt: e.matmul(psc[:, 0:W_], lhsT=diagw[:, ph, :], rhs=Rt[prt][:, 0:W_], start=(ph == 0), stop=(ph == 7)),
                               r=["Rt%d" % prt, "diagw"], w=[pscn])
                        pend_ = (h, rt_) if h < 8 else None
                    op("act", lambda e, kg=kg, W_=W_, psc=psc: e.activation(out=scores[:, kg * 512:kg * 512 + W_], in_=psc[:, 0:W_], func=AF.Copy), r=[pscn], w=[scn])
                    yield

            def gen_T(i):
                sl = i % 2
                nkt = 2 * i + 2; Lk = nkt * 128
                scores = scoresL[sl]; scn = "scores%d" % sl
                op("dve", lambda e: e.tensor_tensor(out=scores[:, Lk - 256:Lk], in0=scores[:, Lk - 256:Lk], in1=C["amask"][:], op=ALU.add),
                   r=[scn, "k_amask"], w=[scn])
                if i == 0:
                    op("dve", lambda e: e.memset(bs[:, 0:1], float(-BIG / 2)), w=["bs"])
                else:
                    op("dve", lambda e: e.tensor_reduce(out=bs[:, 1:2], in_=scores[:, 0:Lk], axis=AX.X, op=ALU.max), r=[scn], w=["bs"])
                    op("dve", lambda e: e.tensor_reduce(out=bs[:, 0:1], in_=scores[:, 0:Lk - 256], axis=AX.X, op=ALU.min), r=[scn, "bs"], w=["bs"])
                    op("dve", lambda e: e.scalar_tensor_tensor(out=bs[:, 1:2], in0=bs[:, 1:2], scalar=1.0, in1=bs[:, 0:1], op0=ALU.add, op1=ALU.subtract), r=["bs"], w=["bs"])
                    op("dve", lambda e: e.tensor_scalar(out=Wk[:], in0=p2k[:], scalar1=bs[:, 1:2], scalar2=None, op0=ALU.mult), r=["bs", "p2k"], w=["Wk"])
                    yield
                    for k in range(NBIS):
                        op("dve", lambda e, k=k: e.tensor_tensor(out=bs[:, 2:3], in0=bs[:, 0:1], in1=Wk[:, k:k + 1], op=ALU.add), r=["bs", "Wk"], w=["bs"])
                        op("dve", lambda e: e.tensor_scalar(out=junk[:, 0:Lk], in0=scores[:, 0:Lk], scalar1=bs[:, 2:3], scalar2=None, op0=ALU.is_ge, op1=ALU.add,
                                                            accum_out=bs[:, 3:4]), r=[scn, "bs"], w=["bs", "ajunk"])
                        op("dve", lambda e, k=k: e.scalar_tensor_tensor(out=bs[:, 4:5], in0=bs[:, 3:4], scalar=float(TOPK - 0.5), in1=Wk[:, k:k + 1], op0=ALU.is_ge, op1=ALU.mult),
                           r=["bs", "Wk"], w=["bs"])
                        op("dve", lambda e: e.tensor_tensor(out=bs[:, 0:1], in0=bs[:, 0:1], in1=bs[:, 4:5], op=ALU.add), r=["bs"], w=["bs"])
                        yield
                op("dve", lambda e: e.tensor_scalar(out=maskb[sl][:, 0:Lk], in0=scores[:, 0:Lk], scalar1=bs[:, 0:1], scalar2=1.0, op0=ALU.is_ge, op1=ALU.subtract),
                   r=[scn, "bs"], w=["maskb%d" % sl])
                yield

            def gen_A(i):
                sl = i % 2
                nkt = 2 * i + 2
                mb = maskb[sl]; mbn = "maskb%d" % sl
                op("sp", lambda e: e.dma_start(out=qTb[sl][:], in_=qT_d[:, :, i * 128:(i + 1) * 128].rearrange("c p t -> p c t")),
                   r=["qT_d"], w=["qTb%d" % sl], dsem=qsm2[sl])
                for j in range(2):
                    prev = None
                    for kt in range(nkt + 1):
                        if kt < nkt:
                            si = cnts["E"] % 2
                            ps_, psn_ = PS[si], psn[si]
                            op("pe", lambda e, kt=kt, ps_=ps_, j=j: e.matmul(ps_[:], lhsT=kT[:, j, kt * 128:(kt + 1) * 128], rhs=qTb[sl][:, 4 * j:4 * j + 4, :],
                                                                           start=True, stop=False), r=["kT", "qTb%d" % sl], w=[psn_])
                            op("pe", lambda e, kt=kt, ps_=ps_: e.matmul(ps_[:], lhsT=mb[:, kt * 128:(kt + 1) * 128], rhs=iselb[:].rearrange("p h q -> p (h q)"),
                                                                      start=False, stop=True), r=[mbn, "iselb"], w=[psn_])
                            et = cnts["E"] % 3; cnts["E"] += 1
                            op("act", lambda e, ps_=ps_, et=et: e.activation(out=Et[et][:], in_=ps_[:], func=AF.Exp, scale=SCALE), r=[psn_], w=["Et%d" % et])
                        if prev is not None:
                            pkt, pet = prev
                            op("pe", lambda e, pkt=pkt, pet=pet, j=j: e.matmul(PS[2][:], lhsT=Vs[:, pkt, j * 128:(j + 1) * 128], rhs=Et[pet][:], start=(pkt == 0), stop=(pkt == nkt - 1)),
                               r=["Vs", "Et%d" % pet], w=[psn[2]])
                            op("pe", lambda e, pkt=pkt, pet=pet: e.matmul(PS[3][:], lhsT=CB["ones"][:], rhs=Et[pet][:], start=(pkt == 0), stop=(pkt == nkt - 1)),
                               r=["kb_ones", "Et%d" % pet], w=[psn[3]])
                        prev = (kt, et) if kt < nkt else None
                        yield
                    op("act", lambda e: e.activation(out=rden[:], in_=PS[3][:], func=AF.Ln), r=[psn[3]], w=["rden"])
                    op("act", lambda e: e.activation(out=rden[:], in_=rden[:], func=AF.Exp, scale=-1.0), r=["rden"], w=["rden"])
                    op("dve", lambda e, j=j: e.tensor_tensor(out=ybst[sl][:, 4 * j:4 * j + 4, :], in0=PS[2][:].rearrange("p (h q) -> p h q", q=128),
                                                            in1=rden[:].rearrange("p (h q) -> p h q", q=128), op=ALU.mult),
                       r=[psn[2], "rden"], w=["ybst0"])
                op("sp", lambda e: e.dma_start(out=ybT_d[:, :, i * 128:(i + 1) * 128].rearrange("c p t -> p c t"), in_=ybst[sl][:]),
                   r=["ybst0"], w=["ybT_d"], dsem=ybsem[sl])
                yield

            def n_I(i):
                return 1 + (2 * i + 2 + 3) // 4

            def n_T(i):
                return (0 if i == 0 else 1 + NBIS) + 1

            def n_A(i):
                return 2 * (2 * i + 3) + 1

            def merge(gens):
                st_ = [[g, n, 0, False] for g, n in gens if g is not None]
                while any(not x[3] for x in st_):
                    best = None
                    for x in st_:
                        if x[3]: continue
                        frac = x[2] / float(x[1])
                        if best is None or frac < best[0]: best = (frac, x)
                    x = best[1]
                    try:
                        next(x[0]); x[2] += 1
                    except StopIteration:
                        x[3] = True

            NQ = min(SO // 128, NQ_DEBUG)
            for s_ in range(-1, NQ + 1):
                gl = []
                if 0 <= s_ - 1 < NQ: gl.append((gen_A(s_ - 1), n_A(s_ - 1)))
                if 0 <= s_ < NQ: gl.append((gen_T(s_), n_T(s_)))
                if 0 <= s_ + 1 < NQ: gl.append((gen_I(s_ + 1), n_I(s_ + 1)))
                merge(gl)
            sc.barrier()

    NT = SO // 128
    if stage >= 4:
        NTd = min(NT, NG3_DEBUG * 4)
        rt_ = es.enter_context(ExitStack())
        sel1 = sb(rt_, "sel1", [128, NT, 32], F32); sel2 = sb(rt_, "sel2", [128, NT, 32], F32)
        rank = sb(rt_, "rank", [128, NT, 32], F32); gates = sb(rt_, "gates", [128, NT, 2], F32)
        Asum = sb(rt_, "Asum", [128, 32], BF16)
        d1i = sb(rt_, "d1i", [128, NT], I32); d2i = sb(rt_, "d2i", [128, NT], I32)
        op("dve", lambda e: e.memset(Asum[:], 0.0), w=["Asum"])
        op("dve", lambda e: e.memset(sel1[:], 0.0), w=["sel1"])
        op("dve", lambda e: e.memset(sel2[:], 0.0), w=["sel2"])
        op("dve", lambda e: e.memset(rank[:], 0.0), w=["rank"])
        with ExitStack() as st:
            WA = sb(st, "WA", [128, 8, 1024], BF16); WB = sb(st, "WB", [128, 8, 1024], BF16); WO = sb(st, "WO", [128, 8, 1024], BF16)
            load_w_cols(WA, "WA", I["w_lru_out"][0], 0, 1024, None, None, sc.dsem())
            load_w_cols(WB, "WB", I["w_attn_out"][0], 0, 1024, None, None, sc.dsem())
            load_w_cols(WO, "WO", I["w_o"][0], 0, 1024, None, None, sc.dsem())
            WR = sb(st, "WR", [128, 8, 36], F32)
            op("sp", [lambda e: e.dma_start(out=WR[:, :, 0:4], in_=I["w_grp"][0].rearrange("(c p) n -> p c n", p=128)),
                      lambda e: e.dma_start(out=WR[:, :, 4:36], in_=I["w_exp"][0].rearrange("(c p) n -> p c n", p=128))], w=["WR"], dsem=sc.dsem())
            g2bc = sb(st, "g2bc", [128, 1024], F32); bbc = sb(st, "bbc", [128, 36], F32)
            op("sp", [lambda e: e.dma_start(out=g2bc[:], in_=I["norm2_g"][0].partition_broadcast(128)),
                      lambda e: e.dma_start(out=bbc[:, 0:4], in_=I["b_grp"][0].partition_broadcast(128)),
                      lambda e: e.dma_start(out=bbc[:, 4:36], in_=I["b_exp"][0].partition_broadcast(128))], w=["g2bc", "bbc"], dsem=sc.dsem())
            ya_sL = [sb(st, "ya_s%d" % i, [128, 8, 512], BF16) for i in range(2)]; yb_sL = [sb(st, "yb_s%d" % i, [128, 8, 512], BF16) for i in range(2)]
            mg_sL = [sb(st, "mg_s%d" % i, [128, 16, 512], BF16) for i in range(2)]
            insem = [sc.dsem() for _ in range(2)]
            mixTL = [sb(st, "mixT%d" % i, [128, 8, 512], BF16) for i in range(2)]
            t1 = sb(st, "ot1", [128, 512], F32); t2 = sb(st, "ot2", [128, 512], F32)
            xin = [sb(st, "oxin%d" % i, [128, 1024], F32) for i in range(2)]; xsem = [sc.dsem() for _ in range(2)]
            x1 = [sb(st, "ox1_%d" % i, [128, 1024], F32) for i in range(2)]; x1sem = [sc.dsem() for _ in range(2)]
            h2L = [sb(st, "oh2_%d" % i, [128, 1024], F32) for i in range(2)]
            h2b = [sb(st, "oh2b%d" % i, [128, 1024], BF16) for i in range(2)]; h2sem = [sc.dsem() for _ in range(2)]
            h2TL = [sb(st, "oh2T%d" % i, [128, 8, 128], F32) for i in range(2)]
            ojunk = sb(st, "ojunk", [128, 1024], BF16)
            RS = []
            for i_ in range(2):
                RS.append(dict(rs=sb(st, "ors%d" % i_, [128, 16], F32), lg=sb(st, "olg%d" % i_, [128, 36], F32), ohg=sb(st, "ohg%d" % i_, [128, 4], F32),
                               tmp32=sb(st, "otmp32_%d" % i_, [128, 32], F32), ig=sb(st, "oig%d" % i_, [128, 8], F32), m8=sb(st, "om8_%d" % i_, [128, 8], F32),
                               A_b=sb(st, "oAb%d" % i_, [128, 32], BF16)))
            zb = sb(st, "ozb", [128, 1024], BF16)
            op("dve", lambda e: e.memset(zb[:], 0.0), w=["ozb"])
            op("sp", lambda e: e.dma_start(out=H2_d[SO:SO + 128, :], in_=zb[:]), r=["ozb"], w=["H2_d_pad"], dsem=sc.dsem())
            if NG3_DEBUG < 1000:
                op("sp", [lambda e, t=t: e.dma_start(out=H2_d[t * 128:(t + 1) * 128, :], in_=zb[:]) for t in range(NT)], r=["ozb"], w=["H2_d"], dsem=sc.dsem())

            def mix_chain(g):
                k_ = g % 2
                ya_s, yb_s, mg_s, mixT = ya_sL[k_], yb_sL[k_], mg_sL[k_], mixTL[k_]
                yan, ybn, mgn, mxn = "ya_s%d" % k_, "yb_s%d" % k_, "mg_s%d" % k_, "mixT%d" % k_
                op("sp", [lambda e: e.dma_start(out=ya_s[:], in_=yaT_d[:, :, g * 512:(g + 1) * 512].rearrange("c p t -> p c t")),
                          lambda e: e.dma_start(out=yb_s[:], in_=ybT_d[:, :, g * 512:(g + 1) * 512].rearrange("c p t -> p c t")),
                          lambda e: e.dma_start(out=mg_s[:], in_=mgT_d[:, :, g * 512:(g + 1) * 512].rearrange("c p t -> p c t"))],
                   r=["yaT_d", "ybT_d", "mgT_d"], w=[yan, ybn, mgn], dsem=insem[k_])
                for m in range(8):
                    pa, pan = nextbank(); pb, pbn = nextbank()
                    for c in range(8):
                        op("pe", lambda e, c=c, m=m, pa=pa: e.matmul(pa[:], lhsT=WA[:, c, m * 128:(m + 1) * 128], rhs=ya_s[:, c, :], start=(c == 0), stop=(c == 7)),
                           r=[yan, "WA_%d" % c], w=[pan])
                    for c in range(8):
                        op("pe", lambda e, c=c, m=m, pb=pb: e.matmul(pb[:], lhsT=WB[:, c, m * 128:(m + 1) * 128], rhs=yb_s[:, c, :], start=(c == 0), stop=(c == 7)),
                           r=[ybn, "WB_%d" % c], w=[pbn])
                    op("dve", lambda e, m=m, pa=pa: e.tensor_tensor(out=t1[:], in0=pa[:], in1=mg_s[:, m, :], op=ALU.mult), r=[pan, mgn], w=["ot1"])
                    op("dve", lambda e, m=m, pb=pb: e.tensor_tensor(out=t2[:], in0=pb[:], in1=mg_s[:, 8 + m, :], op=ALU.mult), r=[pbn, mgn], w=["ot2"])
                    op("dve", lambda e, m=m: e.tensor_tensor(out=mixT[:, m, :], in0=t1[:], in1=t2[:], op=ALU.add), r=["ot1", "ot2"], w=[mxn])
                    yield

            def tile_chain(g, t, k_):
                mixT = mixTL[g % 2]; mxn = "mixT%d" % (g % 2)
                R_ = RS[k_]; rs, lg, ohg, tmp32, ig, m8, A_b = R_["rs"], R_["lg"], R_["ohg"], R_["tmp32"], R_["ig"], R_["m8"], R_["A_b"]
                rsn, lgn, ohgn, tmpn, ign, m8n, Abn = ("ors%d" % k_, "olg%d" % k_, "ohg%d" % k_, "otmp32_%d" % k_, "oig%d" % k_, "om8_%d" % k_, "oAb%d" % k_)
                h2 = h2L[k_]; h2n = "oh2_%d" % k_; h2T = h2TL[k_]; h2Tn = "oh2T%d" % k_
                ti = g * 4 + t; sl = k_
                op("sp", lambda e: e.dma_start(out=xin[sl][:], in_=I["x_own"][ti * 128:(ti + 1) * 128, :]), w=["oxin%d" % sl], dsem=xsem[sl])
                for nh in range(2):
                    po, pon = nextbank()
                    for c in range(8):
                        op("pe", lambda e, c=c, nh=nh, po=po: e.matmul(po[:], lhsT=mixT[:, c, t * 128:(t + 1) * 128], rhs=WO[:, c, nh * 512:(nh + 1) * 512], start=(c == 0), stop=(c == 7)),
                           r=[mxn, "WO_%d" % c], w=[pon])
                    op("dve", lambda e, nh=nh, po=po: e.tensor_tensor(out=x1[sl][:, nh * 512:(nh + 1) * 512], in0=po[:], in1=xin[sl][:, nh * 512:(nh + 1) * 512], op=ALU.add),
                       r=[pon, "oxin%d" % sl], w=["ox1_%d" % sl])
                    yield
                op("sp", lambda e: e.dma_start(out=X1_d[ti * 128:(ti + 1) * 128, :], in_=x1[sl][:]), r=["ox1_%d" % sl], w=["X1_d"], dsem=x1sem[sl])
                op("act", lambda e: e.activation(out=ojunk[:], in_=x1[sl][:], func=AF.Square, accum_out=rs[:, 0:1]), r=["ox1_%d" % sl], w=["ojunk", rsn])
                op("act", lambda e: e.activation(out=rs[:, 1:2], in_=rs[:, 0:1], func=AF.Ln, scale=float(1.0 / D), bias=float(EPS)), r=[rsn], w=[rsn])
                op("act", lambda e: e.activation(out=rs[:, 1:2], in_=rs[:, 1:2], func=AF.Exp, scale=-0.5), r=[rsn], w=[rsn])
                yield
                op("dve", lambda e: e.scalar_tensor_tensor(out=h2[:], in0=x1[sl][:], scalar=rs[:, 1:2], in1=g2bc[:], op0=ALU.mult, op1=ALU.mult),
                   r=["ox1_%d" % sl, rsn, "g2bc"], w=[h2n])
                yield
                op("act", lambda e: e.activation(out=h2b[sl][:], in_=h2[:], func=AF.Copy), r=[h2n], w=["oh2b%d" % sl])
                op("sp", lambda e: e.dma_start(out=H2_d[ti * 128:(ti + 1) * 128, :], in_=h2b[sl][:]), r=["oh2b%d" % sl], w=["H2_d"], dsem=h2sem[sl])
                yield
                for half in range(2):
                    bank, bn = nextbank()
                    for c4 in range(4):
                        c = half * 4 + c4
                        op("pe", lambda e, c=c, c4=c4, bank=bank: e.transpose(out=bank[:, c4 * 128:(c4 + 1) * 128], in_=h2[:, c * 128:(c + 1) * 128], identity=C["ident"][:]),
                           r=[h2n, "k_ident"], w=[bn])
                    op("act", lambda e, half=half, bank=bank: e.activation(out=h2T[:, half * 4:half * 4 + 4, :], in_=bank[:].rearrange("p (c q) -> p c q", q=128), func=AF.Copy),
                       r=[bn], w=[h2Tn])
                    yield
                pl, pln = nextbank()
                for c in range(8):
                    op("pe", lambda e, c=c: e.matmul(pl[:, 0:36], lhsT=h2T[:, c, :], rhs=WR[:, c, :], start=(c == 0), stop=(c == 7)), r=[h2Tn, "WR"], w=[pln])
                op("dve", lambda e: e.tensor_tensor(out=lg[:], in0=pl[:, 0:36], in1=bbc[:], op=ALU.add), r=[pln, "bbc"], w=[lgn])
                yield
                op("dve", lambda e: e.tensor_reduce(out=rs[:, 2:3], in_=lg[:, 0:4], axis=AX.X, op=ALU.max), r=[lgn], w=[rsn])
                op("dve", lambda e: e.tensor_scalar(out=ohg[:], in0=lg[:, 0:4], scalar1=rs[:, 2:3], scalar2=None, op0=ALU.is_ge), r=[lgn, rsn], w=[ohgn])
                yield
                op("dve", lambda e: e.tensor_scalar(out=rs[:, 3:4], in0=rs[:, 2:3], scalar1=-1.0, scalar2=None, op0=ALU.mult), r=[rsn], w=[rsn])
                op("act", lambda e: e.activation(out=ig[:, 0:4], in_=lg[:, 0:4], func=AF.Exp, bias=rs[:, 3:4], accum_out=rs[:, 4:5]), r=[lgn, rsn], w=[ign, rsn])
                yield
                op("dve", lambda e: e.reciprocal(out=rs[:, 5:6], in_=rs[:, 4:5]), r=[rsn], w=[rsn])
                op("dve", lambda e: e.tensor_tensor(out=tmp32[:].rearrange("p (g x) -> p g x", x=8), in0=lg[:, 4:36].rearrange("p (g x) -> p g x", x=8),
                                                    in1=ohg[:].unsqueeze(2).broadcast_to([128, 4, 8]), op=ALU.mult), r=[lgn, ohgn], w=[tmpn])
                yield
                op("dve", lambda e: e.tensor_reduce(out=ig[:], in_=tmp32[:].rearrange("p (g x) -> p x g", x=8), axis=AX.X, op=ALU.add), r=[tmpn, ign], w=[ign])
                op("dve", lambda e: e.max(out=m8[:], in_=ig[:]), r=[ign], w=[m8n])
                yield
                op("dve", lambda e: e.tensor_tensor(out=rs[:, 6:7], in0=m8[:, 1:2], in1=m8[:, 0:1], op=ALU.subtract), r=[m8n, rsn], w=[rsn])
                op("act", lambda e: e.activation(out=rs[:, 7:8], in_=rs[:, 6:7], func=AF.Exp), r=[rsn], w=[rsn])
                yield
                op("dve", lambda e: e.tensor_scalar(out=rs[:, 8:9], in0=rs[:, 7:8], scalar1=1.0, scalar2=None, op0=ALU.add), r=[rsn], w=[rsn])
                op("dve", lambda e: e.reciprocal(out=rs[:, 8:9], in_=rs[:, 8:9]), r=[rsn], w=[rsn])
                yield
                op("dve", lambda e: e.tensor_tensor(out=gates[:, ti, 0:1], in0=rs[:, 8:9], in1=rs[:, 5:6], op=ALU.mult), r=[rsn], w=["gates"])
                op("dve", lambda e: e.tensor_tensor(out=gates[:, ti, 1:2], in0=gates[:, ti, 0:1], in1=rs[:, 7:8], op=ALU.mult), r=[rsn, "gates"], w=["gates"])
                yield
                for kk, sel in ((0, sel1), (1, sel2)):
                    op("dve", lambda e, kk=kk: e.tensor_scalar(out=tmp32[:], in0=lg[:, 4:36], scalar1=m8[:, kk:kk + 1], scalar2=None, op0=ALU.is_equal), r=[lgn, m8n], w=[tmpn])
                    op("dve", lambda e, sel=sel: e.tensor_tensor(out=sel[:, ti, :].rearrange("p (g x) -> p g x", x=8), in0=tmp32[:].rearrange("p (g x) -> p g x", x=8),
                                                                in1=ohg[:].unsqueeze(2).broadcast_to([128, 4, 8]), op=ALU.mult), r=[tmpn, ohgn], w=["sel%d" % (kk + 1)])
                    yield
                op("dve", lambda e: e.tensor_tensor(out=A_b[:], in0=sel1[:, ti, :], in1=sel2[:, ti, :], op=ALU.add), r=["sel1", "sel2"], w=[Abn])
                pr_, prn_ = nextbank()
                op("pe", lambda e: e.matmul(pr_[:, 0:32], lhsT=CB["triu"][:], rhs=A_b[:], start=True, stop=False), r=[Abn, "kb_triu"], w=[prn_])
                op("pe", lambda e: e.matmul(pr_[:, 0:32], lhsT=CB["ones"][:], rhs=Asum[:], start=False, stop=True), r=["Asum", "kb_ones"], w=[prn_])
                op("act", lambda e: e.activation(out=rank[:, ti, :], in_=pr_[:, 0:32], func=AF.Copy), r=[prn_], w=["rank"])
                op("dve", lambda e: e.tensor_tensor(out=Asum[:], in0=Asum[:], in1=A_b[:], op=ALU.add), r=["Asum", Abn], w=["Asum"])
                yield

            def seq(*gens):
                for g_ in gens:
                    for _ in g_:
                        yield

            def rr_merge3(gens):
                gens = [g_ for g_ in gens if g_ is not None]
                while gens:
                    for g_ in list(gens):
                        try:
                            next(g_)
                        except StopIteration:
                            gens.remove(g_)

            NG3 = min(SO // 512, NG3_DEBUG)
            if NG3 > 0:
                for _ in mix_chain(0): pass
            for g in range(NG3):
                rr_merge3([mix_chain(g + 1) if g + 1 < NG3 else None,
                           seq(tile_chain(g, 0, 0), tile_chain(g, 2, 0)), seq(tile_chain(g, 1, 1), tile_chain(g, 3, 1))])
            sc.barrier()

    if stage >= 5:
        with ExitStack() as st:
            toki = sb(st, "mtoki", [128, NT], I32)
            sti = sb(st, "msti", [128, NSLOT // 128], I32)
            wi13i = sb(st, "mwi13i", [128, NBLK], I32)
            sidxA = sb(st, "msidxA", [128, NBLK * NS_], I32)
            tmp = ExitStack()
            cnt = sb(tmp, "mcnt", [128, 32], F32); nb_i = sb(tmp, "mnbi", [128, 32], I32); padc = sb(tmp, "mpadc", [128, 32], F32)
            pend = sb(tmp, "mpend", [128, 32], F32); pstart = sb(tmp, "mpstart", [128, 32], F32); z32 = sb(tmp, "mz32", [128, 32], F32)
            big = sb(tmp, "mbig", [128, NT, 32], F32); dd = sb(tmp, "mdd", [128, NT], F32)
            tokf = sb(tmp, "mtokf", [128, NT], F32)
            cmpb = sb(tmp, "mcmp", [128, NBLK, 32], F32); be = sb(tmp, "mbe", [128, NBLK], F32)
            wi13 = sb(tmp, "mwi13", [128, NBLK], F32)
            pc_, pcn_ = nextbank()
            op("pe", lambda e: e.matmul(pc_[:, 0:32], lhsT=CB["ones"][:], rhs=Asum[:], start=True, stop=True), r=["Asum", "kb_ones"], w=[pcn_])
            op("dve", lambda e: e.tensor_scalar(out=cnt[:], in0=pc_[:, 0:32], scalar1=float(BLK - 1), scalar2=float(1.0 / BLK), op0=ALU.add, op1=ALU.mult), r=[pcn_], w=["mcnt"])
            op("dve", lambda e: e.tensor_scalar(out=nb_i[:], in0=cnt[:], scalar1=-0.498, scalar2=None, op0=ALU.add), r=["mcnt"], w=["mnbi"])
            op("dve", lambda e: e.tensor_copy(out=padc[:], in_=nb_i[:]), r=["mnbi"], w=["mpadc"])
            op("dve", lambda e: e.tensor_scalar(out=padc[:], in0=padc[:], scalar1=float(BLK), scalar2=None, op0=ALU.mult), r=["mpadc"], w=["mpadc"])
            op("dve", lambda e: e.memset(z32[:], 0.0), w=["mz32"])
            op("dve", lambda e: e.tensor_tensor_scan(out=pend[:], data0=z32[:], data1=padc[:], initial=0.0, op0=ALU.add, op1=ALU.add), r=["mz32", "mpadc"], w=["mpend"])
            op("dve", lambda e: e.tensor_tensor(out=pstart[:], in0=pend[:], in1=padc[:], op=ALU.subtract), r=["mpend", "mpadc"], w=["mpstart"])
            for kk, (sel, di) in enumerate(((sel1, d1i), (sel2, d2i))):
                op("dve", lambda e: e.tensor_tensor(out=big[:], in0=rank[:], in1=pstart[:].unsqueeze(1).broadcast_to([128, NT, 32]), op=ALU.add), r=["rank", "mpstart"], w=["mbig"])
                op("dve", lambda e, sel=sel: e.tensor_tensor(out=big[:], in0=big[:], in1=sel[:], op=ALU.mult), r=["mbig", "sel%d" % (kk + 1)], w=["mbig"])
                op("dve", lambda e: e.tensor_reduce(out=dd[:], in_=big[:], axis=AX.X, op=ALU.add), r=["mbig"], w=["mdd"])
                op("dve", lambda e, di=di: e.tensor_copy(out=di[:], in_=dd[:]), r=["mdd"], w=["d%di" % (kk + 1)])
            for t in range(NT):
                op("dve", lambda e, t=t: e.tensor_scalar(out=tokf[:, t:t + 1], in0=C["piota"][:], scalar1=float(t * 128), scalar2=None, op0=ALU.add), r=["k_piota"], w=["mtokf"])
            op("dve", lambda e: e.tensor_copy(out=toki[:], in_=tokf[:]), r=["mtokf"], w=["mtoki"])
            op("dve", lambda e: e.memset(sti[:], SO), w=["msti"])
            op("sp", lambda e: e.dma_start(out=ST_d.rearrange("(p n) o -> p (n o)", p=128), in_=sti[:]), r=["msti"], w=["ST_d"], dsem=sc.dsem())
            NTd = min(NT, NG3_DEBUG * 4)
            fl = []
            for t in range(NTd):
                for di in (d1i, d2i):
                    fl.append(lambda e, t=t, di=di: e.indirect_dma_start(out=ST_d[:, :], out_offset=bass.IndirectOffsetOnAxis(ap=di[:, t:t + 1], axis=0),
                                                                         in_=toki[:, t:t + 1], in_offset=None))
            op("pool", fl, r=["d1i", "d2i", "mtoki", "ST_d"], w=["ST_d"], dsem=sc.dsem())
            op("dve", lambda e: e.tensor_tensor(out=cmpb[:], in0=pend[:].unsqueeze(1).broadcast_to([128, NBLK, 32]),
                                                in1=C["blkpos"][:].unsqueeze(2).broadcast_to([128, NBLK, 32]), op=ALU.is_le), r=["mpend", "k_blkpos"], w=["mcmp"])
            op("dve", lambda e: e.tensor_reduce(out=be[:], in_=cmpb[:], axis=AX.X, op=ALU.add), r=["mcmp"], w=["mbe"])
            op("dve", lambda e: e.tensor_scalar(out=wi13[:], in0=be[:], scalar1=128.0, scalar2=C["piota"][:, 0:1], op0=ALU.mult, op1=ALU.add), r=["mbe", "k_piota"], w=["mwi13"])
            op("dve", lambda e: e.tensor_copy(out=wi13i[:], in_=wi13[:]), r=["mwi13"], w=["mwi13i"])
            sc.barrier()
            tmp.close()
            w13f = I["w13"][0].rearrange("e (p c) n -> (e p) (c n)", c=8); w2f = I["w2"][0].rearrange("e (p j) n -> (e p) (j n)", j=4)
            op("sp", lambda e: e.dma_start(out=sidxA[:], in_=ST_d.rearrange("(b p s) o -> p b (s o)", p=128, s=NS_)), r=["ST_d"], w=["msidxA"], dsem=sc.dsem())
            bst = ExitStack()
            wf13L = [sb(bst, "mwf13_%d" % i, [128, 8, 1024], F32) for i in range(2)]; wf2L = [sb(bst, "mwf2_%d" % i, [128, 4, 1024], F32) for i in range(2)]
            wb13L = [sb(bst, "mwb13_%d" % i, [128, 8, 1024], BF16) for i in range(2)]; wb2L = [sb(bst, "mwb2_%d" % i, [128, 4, 1024], BF16) for i in range(2)]
            wsem13 = [sc.dsem() for _ in range(2)]; wsem2 = [sc.dsem() for _ in range(2)]
            for i_ in range(2):
                op("dve", lambda e, i_=i_: e.memset(wf13L[i_][:], 0.0), w=["mwf13_%d" % i_])
                op("dve", lambda e, i_=i_: e.memset(wf2L[i_][:], 0.0), w=["mwf2_%d" % i_])
            xg = [sb(bst, "mxg%d" % i, [128, NS_, 1024], BF16) for i in range(2)]; xgsem = [sc.dsem() for _ in range(2)]
            xgT = sb(bst, "mxgT", [128, 8, BLK], BF16)
            sg = sb(bst, "msg", [128, BLK], F32); actT = sb(bst, "mactT", [128, 4, BLK], BF16)
            yst2 = [sb(bst, "myst%d" % i, [128, 1024], BF16) for i in range(2)]; ysem2 = [sc.dsem() for _ in range(2)]
            ny = 0
            if NB_DEBUG < 1000:
                zf = sb(bst, "mzf", [128, 1024], BF16)
                op("dve", lambda e: e.memset(zf[:], 0.0), w=["mzf"])
                op("sp", [lambda e, t=t: e.dma_start(out=Y_d[t * 128:(t + 1) * 128, :], in_=zf[:]) for t in range(NSLOT // 128)], r=["mzf"], w=["Y_d"], dsem=sc.dsem())
            for blk in range(min(NBLK, NB_DEBUG)):
                sl = blk % 2
                op("pool", [lambda e, s_=s_, sl=sl, blk=blk: e.indirect_dma_start(out=xg[sl][:, s_, :], out_offset=None, in_=H2_d[:, :],
                                                                        in_offset=bass.IndirectOffsetOnAxis(ap=sidxA[:, blk * NS_ + s_:blk * NS_ + s_ + 1], axis=0)) for s_ in range(NS_)],
                   r=["msidxA", "H2_d", "H2_d_pad"], w=["mxg%d" % sl], dsem=xgsem[sl])
                wf13 = wf13L[sl]; wf2 = wf2L[sl]; wf13n = "mwf13_%d" % sl; wf2n = "mwf2_%d" % sl
                wb13 = wb13L[sl]; wb2 = wb2L[sl]; wbn = "mwb13s%d_" % sl; wb2n = "mwb2s%d_" % sl
                op("pool", lambda e, blk=blk, wf13=wf13: e.indirect_dma_start(out=wf13[:].rearrange("p c n -> p (c n)"), out_offset=None, in_=w13f,
                                                                             in_offset=bass.IndirectOffsetOnAxis(ap=wi13i[:, blk:blk + 1], axis=0),
                                                                             bounds_check=breg(e, 4095), oob_is_err=False),
                   r=["mwi13i"], w=[wf13n], dsem=wsem13[sl])
                op("pool", lambda e, blk=blk, wf2=wf2: e.indirect_dma_start(out=wf2[:].rearrange("p j n -> p (j n)"), out_offset=None, in_=w2f,
                                                                           in_offset=bass.IndirectOffsetOnAxis(ap=wi13i[:, blk:blk + 1], axis=0),
                                                                           bounds_check=breg(e, 4095), oob_is_err=False),
                   r=["mwi13i"], w=[wf2n], dsem=wsem2[sl])
                for c in range(8):
                    eng = "act" if c % 2 == 0 else "dve"
                    if eng == "act":
                        op("act", lambda e, c=c, wf13=wf13, wb13=wb13: e.activation(out=wb13[:, c, :], in_=wf13[:, c, :], func=AF.Copy), r=[wf13n], w=[wbn + "%d" % c])
                    else:
                        op("dve", lambda e, c=c, wf13=wf13, wb13=wb13: e.tensor_copy(out=wb13[:, c, :], in_=wf13[:, c, :]), r=[wf13n], w=[wbn + "%d" % c])
                for j in range(4):
                    if j % 2 == 0:
                        op("act", lambda e, j=j, wf2=wf2, wb2=wb2: e.activation(out=wb2[:, j, :], in_=wf2[:, j, :], func=AF.Copy), r=[wf2n], w=[wb2n + "%d" % j])
                    else:
                        op("dve", lambda e, j=j, wf2=wf2, wb2=wb2: e.tensor_copy(out=wb2[:, j, :], in_=wf2[:, j, :]), r=[wf2n], w=[wb2n + "%d" % j])
                for s_ in range(NS_):
                    for half in range(2):
                        bank = PS[half]; bn = psn[half]
                        bv = bank[:].bitcast(BF16)
                        for c4 in range(4):
                            c = half * 4 + c4
                            op("pe", lambda e, c=c, c4=c4, s_=s_, sl=sl, bv=bv: e.transpose(out=bv[:, c4 * 128:(c4 + 1) * 128], in_=xg[sl][:, s_, :].rearrange("p (q c) -> p c q", c=8)[:, c, :], identity=CB["ident"][:]),
                               r=["mxg%d" % sl, "kb_ident"], w=[bn])
                        op("act", lambda e, half=half, s_=s_, bv=bv: e.activation(out=xgT[:, half * 4:half * 4 + 4, s_ * 128:(s_ + 1) * 128], in_=bv[:, 0:512].rearrange("p (c q) -> p c q", q=128), func=AF.Copy),
                           r=[bn], w=["mxgT"])
                for m in range(4):
                    pg, pgn = nextbank(); pu, pun = nextbank()
                    for c in range(8):
                        op("pe", lambda e, c=c, m=m, pg=pg, wb13=wb13: e.matmul(pg[:, 0:BLK], lhsT=wb13[:, c, 0:512].rearrange("p (q j) -> p j q", j=4)[:, m, :], rhs=xgT[:, c, :], start=(c == 0), stop=(c == 7)),
                           r=["mxgT", wbn + "%d" % c], w=[pgn])
                    for c in range(8):
                        op("pe", lambda e, c=c, m=m, pu=pu, wb13=wb13: e.matmul(pu[:, 0:BLK], lhsT=wb13[:, c, 512:1024].rearrange("p (q j) -> p j q", j=4)[:, m, :], rhs=xgT[:, c, :], start=(c == 0), stop=(c == 7)),
                           r=["mxgT", wbn + "%d" % c], w=[pun])
                    op("act", lambda e, pg=pg: e.activation(out=sg[:], in_=pg[:, 0:BLK], func=AF.Silu), r=[pgn], w=["msg"])
                    op("dve", lambda e, m=m, pu=pu: e.tensor_tensor(out=actT[:, m, :], in0=sg[:], in1=pu[:, 0:BLK], op=ALU.mult), r=["msg", pun], w=["mactT"])
                for s_ in range(NS_):
                    ys = ny % 2; ny += 1
                    for nh in range(2):
                        py, pyn = nextbank()
                        for j in range(4):
                            op("pe", lambda e, j=j, s_=s_, nh=nh, py=py, wb2=wb2: e.matmul(py[:], lhsT=actT[:, j, s_ * 128:(s_ + 1) * 128], rhs=wb2[:, j, nh * 512:(nh + 1) * 512], start=(j == 0), stop=(j == 3)),
                               r=["mactT", wb2n + "%d" % j], w=[pyn])
                        op("act", lambda e, nh=nh, ys=ys, py=py: e.activation(out=yst2[ys][:, nh * 512:(nh + 1) * 512], in_=py[:], func=AF.Copy), r=[pyn], w=["myst%d" % ys])
                    op("sp", lambda e, blk=blk, s_=s_, ys=ys: e.dma_start(out=Y_d[blk * BLK:(blk + 1) * BLK, :].rearrange("(p s) d -> p s d", s=NS_)[:, s_, :], in_=yst2[ys][:]),
                       r=["myst%d" % ys], w=["Y_d"], dsem=ysem2[ys])
            sc.barrier()
            bst.close()
            y1 = [sb(st, "my1_%d" % i, [128, 1024], BF16) for i in range(2)]; y2 = [sb(st, "my2_%d" % i, [128, 1024], BF16) for i in range(2)]
            xx = [sb(st, "mxx%d" % i, [128, 1024], F32) for i in range(2)]
            csem = [sc.dsem() for _ in range(2)]; c2sem = [sc.dsem() for _ in range(2)]; osem2 = [sc.dsem() for _ in range(2)]
            for t in range(NTd):
                sl = t % 2
                op("pool", [lambda e, t=t, sl=sl: e.indirect_dma_start(out=y1[sl][:], out_offset=None, in_=Y_d[:, :], in_offset=bass.IndirectOffsetOnAxis(ap=d1i[:, t:t + 1], axis=0)),
                            lambda e, t=t, sl=sl: e.indirect_dma_start(out=y2[sl][:], out_offset=None, in_=Y_d[:, :], in_offset=bass.IndirectOffsetOnAxis(ap=d2i[:, t:t + 1], axis=0))],
                   r=["Y_d", "d1i", "d2i"], w=["my1_%d" % sl, "my2_%d" % sl], dsem=csem[sl])
                op("sp", lambda e, t=t, sl=sl: e.dma_start(out=xx[sl][:], in_=X1_d[t * 128:(t + 1) * 128, :]), r=["X1_d"], w=["mxx%d" % sl], dsem=c2sem[sl])
                op("dve", lambda e, t=t, sl=sl: e.scalar_tensor_tensor(out=xx[sl][:], in0=y1[sl][:], scalar=gates[:, t, 0:1], in1=xx[sl][:], op0=ALU.mult, op1=ALU.add),
                   r=["my1_%d" % sl, "gates", "mxx%d" % sl], w=["mxx%d" % sl])
                op("dve", lambda e, t=t, sl=sl: e.scalar_tensor_tensor(out=xx[sl][:], in0=y2[sl][:], scalar=gates[:, t, 1:2], in1=xx[sl][:], op0=ALU.mult, op1=ALU.add),
                   r=["my2_%d" % sl, "gates", "mxx%d" % sl], w=["mxx%d" % sl])
                op("sp", lambda e, t=t, sl=sl: e.dma_start(out=out[t * 128:(t + 1) * 128, :], in_=xx[sl][:]), r=["mxx%d" % sl], w=["out"], dsem=osem2[sl])
            sc.barrier()

    sc.barrier()
    sc.emit()
    es.close()
    return nc


def make_in_maps(inputs):
    maps = []
    for core in range(8):
        b, par = core // 2, core % 2
        x = np.ascontiguousarray(inputs["x"][b])
        pos = np.ascontiguousarray(inputs["positions"][b]).astype(np.int32)
        own = np.arange(S).reshape(S // 128, 128)[par::2].reshape(-1)
        m = {"x_full": x, "x_own": np.ascontiguousarray(x[own]), "pos_full": pos.reshape(1, S),
             "pos_own": np.ascontiguousarray(pos[own]).reshape(1, SO)}
        for k in PARAMS:
            m[k] = np.ascontiguousarray(inputs[k]).astype(np.float32)
        for k, v in host_consts(par).items():
            m["c_" + k] = v
        maps.append(m)
    return maps


def kernel(**inputs):
    inputs = {k: np.asarray(v) for k, v in inputs.items()}
    nc = build()
    maps = make_in_maps(inputs)
    res = run_bass_kernel_spmd(nc, maps, core_ids=list(range(8)))
    outp = np.zeros((4, S, D), np.float32)
    for core in range(8):
        b, par = core // 2, core % 2
        own = np.arange(S).reshape(S // 128, 128)[par::2].reshape(-1)
        outp[b, own] = res.results[core]["out"]
    return outp
```

```python
import numpy as np
from contextlib import ExitStack
import concourse.bass as bass
import concourse.mybir as mybir
from concourse.bass_utils import run_bass_kernel_spmd

F32 = mybir.dt.float32; BF16 = mybir.dt.bfloat16; I32 = mybir.dt.int32
ALU = mybir.AluOpType; AF = mybir.ActivationFunctionType; AX = mybir.AxisListType

D = 1024; S = 8192; SO = 4096; NCOL = 6216
EPS = 1e-6
BIG = 30000.0
TWO_PI = 6.283185307179586
PI_HI = 6.28125
PI_LO = TWO_PI - PI_HI
NBIS = 16
BLK = 512
NS_ = BLK // 128
NBLK = (2 * SO) // BLK + 32
NSLOT = NBLK * BLK
TOPK = 256
NG_DEBUG = 1000
CUT_DEBUG = 99
NQ_DEBUG = 1000
NG2_DEBUG = 1000
NG3_DEBUG = 1000
NB_DEBUG = 1000
HE_CUT = 99


class Sched:
    def __init__(s, nc, es):
        s.nc = nc; s.es = es
        s.sems = {}; s.cnt = {}
        s.ops = {e: [] for e in ("pe", "act", "dve", "pool", "sp")}
        s.waited = {e: {} for e in s.ops}
        s.lw = {}; s.lr = {}
        for e in ("pe", "act", "dve", "pool"):
            s.newsem("E_" + e)
        s.nd = 0

    def newsem(s, name):
        s.sems[name] = s.es.enter_context(s.nc.semaphore(name)); s.cnt[name] = 0
        return name

    def dsem(s):
        s.nd += 1
        return s.newsem("D%d" % s.nd)

    def op(s, eng, fn, r=(), w=(), dsem=None):
        deps = {}
        def add(t):
            if t is None: return
            k, v, e = t
            if deps.get(k, (0, None))[0] < v: deps[k] = (v, e)
        for x in r:
            add(s.lw.get(x))
            if x.startswith("ps"):
                for k, (v, e) in s.lr.get(x, {}).items():
                    if e != eng: add((k, v, e))
        for x in w:
            add(s.lw.get(x))
            for k, (v, e) in s.lr.get(x, {}).items():
                add((k, v, e))
        fns = fn if isinstance(fn, list) else [fn]
        if dsem is None:
            sk = "E_" + eng; s.cnt[sk] += 1; inc = 1
        else:
            sk = dsem; s.cnt[sk] += 16 * len(fns); inc = 16
        tok = (sk, s.cnt[sk], eng)
        wd = s.waited[eng]; waits = []
        for k, (v, e) in deps.items():
            if eng == "pe" and k == "E_pe": continue
            if wd.get(k, 0) >= v: continue
            waits.append((k, v)); wd[k] = v
        for i, f in enumerate(fns):
            s.ops[eng].append((f, waits if i == 0 else [], sk, inc))
        for x in w:
            s.lw[x] = tok; s.lr[x] = {}
        for x in r:
            d = s.lr.setdefault(x, {})
            if d.get(sk, (0, None))[0] < tok[1]: d[sk] = (tok[1], eng)
        return tok

    def barrier(s):
        for eng in s.ops:
            wd = s.waited[eng]; waits = []
            for k, c in s.cnt.items():
                if c > 0 and wd.get(k, 0) < c and not (eng == "pe" and k == "E_pe"):
                    waits.append((k, c)); wd[k] = c
            if waits:
                s.ops[eng].append((None, waits, None, 0))

    def emit(s):
        nc = s.nc
        def replay(eng, e):
            for fn, waits, sk, inc in s.ops[eng]:
                for k, v in waits:
                    e.wait_ge(s.sems[k], v)
                if fn is not None:
                    fn(e).then_inc(s.sems[sk], inc)
        with nc.Block() as block:
            @block.tensor
            def _(e): replay("pe", e)
            @block.scalar
            def _(e): replay("act", e)
            @block.vector
            def _(e): replay("dve", e)
            @block.gpsimd
            def _(e): replay("pool", e)
            @block.sync
            def _(e): replay("sp", e)


def host_consts(par):
    c = {}
    c["ident"] = np.eye(128, dtype=np.float32)
    p = np.arange(128)
    c["triu"] = (p[:, None] < p[None, :]).astype(np.float32)
    c["ones"] = np.ones((128, 128), np.float32)
    rt = np.zeros((128, 128), np.float32)
    for d in range(16):
        rt[d + 16, d] = -1.0; rt[d, d + 16] = 1.0
    c["rtm"] = rt
    ri = np.zeros((128, 128), np.float32)
    for base in (0, 64):
        for d in range(8):
            ri[base + d + 8, base + d] = -1.0; ri[base + d, base + d + 8] = 1.0
    c["rti"] = ri
    c["blkones"] = (p[:, None] // 64 == p[None, :] // 64).astype(np.float32)
    invm = np.zeros((128, 1), np.float32)
    f = (np.float32(500000.0) ** (-np.arange(0, 32, 2, dtype=np.float32) / np.float32(32))).astype(np.float32)
    for d in range(32): invm[d, 0] = f[d % 16]
    invi = np.zeros((128, 1), np.float32)
    fi = (np.float32(500000.0) ** (-np.arange(0, 16, 2, dtype=np.float32) / np.float32(16))).astype(np.float32)
    for base in (0, 64):
        for d in range(16): invi[base + d, 0] = fi[d % 8]
    c["invf"] = np.concatenate([invm, invi], axis=1)
    sel = np.zeros((128, 2), np.float32); sel[:, par] = 1.0
    c["sel"] = sel
    q = np.arange(128); kk = np.arange(256)
    m = np.zeros((128, 256), np.float32)
    if par == 0:
        adm = np.zeros((128, 256), bool)
        adm[:, :128] = (kk[None, :128] // 64) <= (q[:, None] // 64)
    else:
        adm = np.ones((128, 256), bool)
        adm[:, 128:] = (kk[None, :128] // 64) <= (q[:, None] // 64)
    m[~adm] = -BIG
    c["amask"] = m
    c["piota"] = np.arange(128, dtype=np.float32).reshape(128, 1)
    c["blkpos"] = np.tile((np.arange(NBLK, dtype=np.float32) * BLK)[None, :], (128, 1))
    return c


CONST_SHAPES = {"ident": [128, 128], "triu": [128, 128], "ones": [128, 128], "rtm": [128, 128], "rti": [128, 128],
                "blkones": [128, 128], "invf": [128, 2], "sel": [128, 2], "amask": [128, 256], "piota": [128, 1],
                "blkpos": [128, NBLK]}

PARAMS = {"norm1_g": [1, 1024], "w_in": [1, 1024, NCOL], "conv_w": [1, 4, 1024], "conv_b": [1, 1024],
          "lru_w_r": [1, 8, 128, 128], "lru_b_r": [1, 8, 128], "lru_w_i": [1, 8, 128, 128], "lru_b_i": [1, 8, 128],
          "lru_a_param": [1, 1024], "q_norm_g": [1, 128], "k_norm_g": [1, 128], "idx_k_norm_g": [1, 64],
          "w_lru_out": [1, 1024, 1024], "w_attn_out": [1, 1024, 1024], "w_o": [1, 1024, 1024], "norm2_g": [1, 1024],
          "w_grp": [1, 1024, 4], "b_grp": [1, 4], "w_exp": [1, 1024, 32], "b_exp": [1, 32],
          "w13": [1, 32, 1024, 1024], "w2": [1, 32, 512, 1024]}


def build(stage=99, dbg=False):
    nc = bass.Bass("TRN2", target_bir_lowering=False)
    es = ExitStack()
    sc = Sched(nc, es)
    op = sc.op
    I = {}
    I["x_full"] = nc.dram_tensor("x_full", [S, D], F32, kind="ExternalInput").ap()
    I["x_own"] = nc.dram_tensor("x_own", [SO, D], F32, kind="ExternalInput").ap()
    I["pos_full"] = nc.dram_tensor("pos_full", [1, S], I32, kind="ExternalInput").ap()
    I["pos_own"] = nc.dram_tensor("pos_own", [1, SO], I32, kind="ExternalInput").ap()
    for k, shp in PARAMS.items():
        I[k] = nc.dram_tensor(k, shp, F32, kind="ExternalInput").ap()
    for k, shp in CONST_SHAPES.items():
        I["c_" + k] = nc.dram_tensor("c_" + k, shp, F32, kind="ExternalInput").ap()
    out = nc.dram_tensor("out", [SO, D], F32, kind="ExternalOutput").ap()
    def scratch(name, shp, dt):
        if dbg and name in dbg:
            return nc.dram_tensor(name, shp, dt, kind="ExternalOutput").ap()
        return nc.dram_tensor(name, shp, dt).ap()
    hT_d = scratch("hT_d", [8, 128, S], BF16)
    kT_d = scratch("kT_d", [2, 128, S], BF16)
    kiT_d = scratch("kiT_d", [128, S], BF16)
    V_d = scratch("V_d", [S, 256], BF16)
    qT_d = scratch("qT_d", [8, 128, SO], BF16)
    qiT_d = scratch("qiT_d", [4, 128, SO], BF16)
    yaT_d = scratch("yaT_d", [8, 128, SO], BF16)
    ybT_d = scratch("ybT_d", [8, 128, SO], BF16)
    mgT_d = scratch("mgT_d", [16, 128, SO], BF16)
    X1_d = scratch("X1_d", [SO, D], F32)
    H2_d = scratch("H2_d", [SO + 128, D], BF16)
    Y_d = scratch("Y_d", [NSLOT, D], BF16)
    ST_d = scratch("ST_d", [NSLOT, 1], I32)
    D_ = {}

    _bregs = {}
    def breg(e, val):
        if val not in _bregs:
            _bregs[val] = e.to_reg(val)
        return _bregs[val]

    def sb(stack, name, shp, dt=F32):
        return stack.enter_context(nc.sbuf_tensor(name, shp, dt))

    PS = [es.enter_context(nc.psum_tensor("ps%d" % i, [128, 512], F32)) for i in range(8)]
    psn = ["ps%d" % i for i in range(8)]
    rot = {"i": 0}
    def nextbank(lo=2, hi=8):
        i = lo + rot["i"] % (hi - lo); rot["i"] += 1
        return PS[i], psn[i]

    C = {}
    cs = sc.dsem()
    TMPC = ("triu", "ones", "rtm", "rti", "blkones")
    CB = {}
    for k in ("ident", "triu", "ones", "rtm", "rti", "blkones"):
        CB[k] = sb(es, "kb_" + k, [128, 128], BF16)
    wq = sb(es, "wq", [128, 32, 8], F32)
    for k, shp in CONST_SHAPES.items():
        if k not in TMPC:
            C[k] = sb(es, "k_" + k, shp, F32)
    with ExitStack() as tst:
        for k in TMPC:
            C[k] = sb(tst, "k_" + k, CONST_SHAPES[k], F32)
        op("sp", [lambda e, k=k: e.dma_start(out=C[k][:], in_=I["c_" + k]) for k in CONST_SHAPES], w=["k_" + k for k in CONST_SHAPES], dsem=cs)
        for k in ("ident", "triu", "ones", "rtm", "rti", "blkones"):
            op("dve", lambda e, k=k: e.tensor_copy(out=CB[k][:], in_=C[k][:]), r=["k_" + k], w=["kb_" + k])
        sc.barrier()

    def dma_T(dst, src_row, n, name=None, sem=None):
        fn = lambda e: e.dma_start(out=dst, in_=src_row.rearrange("(c p) -> p c", p=128), allow_slow_non_contiguous=True)
        if sem is not None:
            op("sp", fn, w=[name], dsem=sem)
        return fn

    def rope_tables(stk, pos_ap, g, ntok, pfx):
        T = {}
        posi = sb(stk, pfx + "posi", [128, ntok], I32)
        posf = sb(stk, pfx + "posf", [128, ntok], F32)
        ang = sb(stk, pfx + "ang", [128, ntok], F32)
        kq = sb(stk, pfx + "kq", [128, ntok], I32)
        kf = sb(stk, pfx + "kf", [128, ntok], F32)
        for nm in ("cm", "sm", "ci", "si"):
            T[nm] = sb(stk, pfx + nm, [128, ntok], F32)
        return dict(T=T, posi=posi, posf=posf, ang=ang, kq=kq, kf=kf, pfx=pfx, sem=sc.dsem(), pos=pos_ap, ntok=ntok)

    def rope_fill(RT_, g):
        T = RT_["T"]; pfx = RT_["pfx"]; ntok = RT_["ntok"]
        posi, posf, ang, kq, kf = RT_["posi"], RT_["posf"], RT_["ang"], RT_["kq"], RT_["kf"]
        src = RT_["pos"][0, g * ntok:(g + 1) * ntok].partition_broadcast(128)
        op("sp", lambda e: e.dma_start(out=posi[:], in_=src), w=[pfx + "posi"], dsem=RT_["sem"])
        op("dve", lambda e: e.tensor_copy(out=posf[:], in_=posi[:]), r=[pfx + "posi"], w=[pfx + "posf"])
        for col, (sn, cn) in enumerate((("sm", "cm"), ("si", "ci"))):
            for nm, shift in ((sn, 0.0), (cn, np.pi / 2)):
                t = T[nm]; tn = pfx + nm
                op("dve", lambda e, col=col, shift=shift, t=t: e.tensor_scalar(out=t[:], in0=posf[:], scalar1=C["invf"][:, col:col + 1],
                                                                              scalar2=float(shift), op0=ALU.mult, op1=ALU.add),
                   r=[pfx + "posf", "k_invf"], w=[tn])
                op("dve", lambda e, t=t: e.tensor_scalar(out=kq[:], in0=t[:], scalar1=float(1.0 / TWO_PI), scalar2=None, op0=ALU.mult),
                   r=[tn], w=[pfx + "kq"])
                op("dve", lambda e: e.tensor_copy(out=kf[:], in_=kq[:]), r=[pfx + "kq"], w=[pfx + "kf"])
                yield
                op("dve", lambda e, t=t: e.scalar_tensor_tensor(out=t[:], in0=kf[:], scalar=float(-PI_HI), in1=t[:], op0=ALU.mult, op1=ALU.add),
                   r=[pfx + "kf", tn], w=[tn])
                op("dve", lambda e, t=t: e.scalar_tensor_tensor(out=t[:], in0=kf[:], scalar=float(-PI_LO), in1=t[:], op0=ALU.mult, op1=ALU.add),
                   r=[pfx + "kf", tn], w=[tn])
                op("dve", lambda e, t=t: e.tensor_scalar(out=t[:], in0=t[:], scalar1=3.1415925, scalar2=-3.1415925, op0=ALU.min, op1=ALU.max),
                   r=[tn], w=[tn])
                yield
        for nm in ("sm", "cm", "si", "ci"):
            op("act", lambda e, t=T[nm]: e.activation(out=t[:], in_=t[:], func=AF.Sin), r=[pfx + nm], w=[pfx + nm])
        yield

    def make_xT(stk, pfx, nslot=2):
        R_ = dict(pfx=pfx, n=0, nslot=nslot)
        R_["xin"] = [sb(stk, pfx + "xin%d" % i, [128, 4, D], BF16) for i in range(nslot)]
        R_["xn"] = [sb(stk, pfx + "xn%d" % i, [128, 4, D], BF16) for i in range(nslot)]
        R_["junk"] = sb(stk, pfx + "junk", [128, D], BF16)
        R_["ss"] = sb(stk, pfx + "ss", [128, 8], F32)
        R_["sems"] = [sc.dsem() for _ in range(nslot)]
        return R_

    def xT_group(R_, x_ap, g, dst, dstname):
        pfx = R_["pfx"]; sl = R_["n"] % R_["nslot"]; R_["n"] += 1
        xin = R_["xin"][sl]; xn = R_["xn"][sl]; ss = R_["ss"]; junk = R_["junk"]
        xinn = pfx + "xin%d" % sl; xnn = pfx + "xn%d" % sl
        src = x_ap[g * 512:(g + 1) * 512, :].rearrange("(t p) d -> p t d", p=128)
        op("pool", [lambda e, t=t: e.dma_start(out=xin[:, t, :], in_=src[:, t, :]) for t in range(4)],
           w=[xinn + "_%d" % t for t in range(4)], dsem=R_["sems"][sl])
        for t in range(4):
            op("act", lambda e, t=t: e.activation(out=junk[:], in_=xin[:, t, :], func=AF.Square, accum_out=ss[:, t:t + 1]),
               r=[xinn + "_%d" % t], w=[pfx + "junk", pfx + "ss%d" % t])
            op("act", lambda e, t=t: e.activation(out=ss[:, 4 + t:5 + t], in_=ss[:, t:t + 1], func=AF.Ln, scale=float(1.0 / D), bias=float(EPS)),
               r=[pfx + "ss%d" % t], w=[pfx + "sr%d" % t])
            op("act", lambda e, t=t: e.activation(out=ss[:, 4 + t:5 + t], in_=ss[:, 4 + t:5 + t], func=AF.Exp, scale=-0.5), r=[pfx + "sr%d" % t], w=[pfx + "sr%d" % t])
            op("dve", lambda e, t=t: e.tensor_scalar(out=xn[:, t, :], in0=xin[:, t, :], scalar1=ss[:, 4 + t:5 + t], scalar2=None, op0=ALU.mult),
               r=[xinn + "_%d" % t, pfx + "sr%d" % t], w=[xnn + "_%d" % t])
            yield
        for cp in range(4):
            bank = PS[cp % 2]; bn = psn[cp % 2]
            bv = bank[:].bitcast(BF16)
            for cc in range(2):
                c = cp * 2 + cc
                for t in range(4):
                    op("pe", lambda e, c=c, t=t, cc=cc, bv=bv: e.transpose(out=bv[:, cc * 512 + t * 128: cc * 512 + (t + 1) * 128],
                                                                      in_=xn[:, t, c * 128:(c + 1) * 128], identity=CB["ident"][:]),
                       r=[xnn + "_%d" % t, "kb_ident"], w=[bn])
            eng = "act" if cp % 2 == 0 else "dve"
            if eng == "act":
                op("act", lambda e, cp=cp, bv=bv: e.activation(out=dst[:, 2 * cp:2 * cp + 2, :], in_=bv.rearrange("p (c n) -> p c n", c=2), func=AF.Copy),
                   r=[bn], w=[dstname])
            else:
                op("dve", lambda e, cp=cp, bv=bv: e.tensor_copy(out=dst[:, 2 * cp:2 * cp + 2, :], in_=bv.rearrange("p (c n) -> p c n", c=2)),
                   r=[bn], w=[dstname])
            yield

    def load_w_cols(dst, dstname, w2d, c0, ncols, gsb, gname, sem, dcol=0):
        src = w2d.rearrange("(c p) n -> p c n", p=128)
        op("pool", [lambda e, c=c: e.dma_start(out=dst[:, c, dcol:dcol + ncols], in_=src[:, c, c0:c0 + ncols]) for c in range(8)],
           w=[dstname + "_%d" % c for c in range(8)], dsem=sem)
        if gsb is not None:
            for c in range(8):
                op("dve", lambda e, c=c: e.tensor_scalar(out=dst[:, c, dcol:dcol + ncols], in0=dst[:, c, dcol:dcol + ncols],
                                                         scalar1=gsb[:, c:c + 1], scalar2=None, op0=ALU.mult),
                   r=[dstname + "_%d" % c, gname], w=[dstname + "_%d" % c])

    def wdeps(name):
        return [name + "_%d" % c for c in range(8)]

    g1 = sb(es, "g1", [128, 8], F32)
    dma_T(g1[:], I["norm1_g"][0], 8, "g1", sc.dsem())

    def head_epilogue(stk_t, pb, pbn, gcol, gname, ones_lhsT, ones_name, rt_lhsT, rt_name, Ct, Cn, St, Sn, dst, dstnames, norm=True, tp="he"):
        t0, sq, rtt, tnb, o1, o2 = stk_t
        if norm:
            op("act", lambda e: e.activation(out=sq[:], in_=pb[:], func=AF.Square), r=[pbn], w=[tp + "sq"])
            op("dve", lambda e: e.tensor_scalar(out=t0[:], in0=pb[:], scalar1=gcol, scalar2=None, op0=ALU.mult), r=[pbn, gname, tp + "sq"], w=[tp + "t0"])
            yield
            p2, p2n = nextbank()
            op("pe", lambda e: e.matmul(p2[:], lhsT=ones_lhsT, rhs=sq[:], start=True, stop=True), r=[tp + "sq", ones_name], w=[p2n])
            nrm = float(1.0 / 128) if ones_name == "kb_ones" else float(1.0 / 64)
            op("act", lambda e: e.activation(out=rtt[:], in_=p2[:], func=AF.Ln, scale=nrm, bias=float(EPS)), r=[p2n], w=[tp + "rt"])
            yield
            op("act", lambda e: e.activation(out=rtt[:], in_=rtt[:], func=AF.Exp, scale=-0.5), r=[tp + "rt"], w=[tp + "rt"])
            op("dve", lambda e: e.tensor_tensor(out=tnb[:], in0=t0[:], in1=rtt[:], op=ALU.mult), r=[tp + "t0", tp + "rt"], w=[tp + "tnb"])
        else:
            op("act", lambda e: e.activation(out=tnb[:], in_=pb[:], func=AF.Copy), r=[pbn], w=[tp + "tnb"])
        yield
        op("dve", lambda e: e.tensor_tensor(out=o1[:], in0=tnb[:], in1=Ct[:], op=ALU.mult), r=[tp + "tnb", Cn], w=[tp + "o1"])
        yield
        p3, p3n = nextbank()
        op("pe", lambda e: e.matmul(p3[:], lhsT=rt_lhsT, rhs=tnb[:], start=True, stop=True), r=[tp + "tnb", rt_name], w=[p3n])
        op("dve", lambda e: e.tensor_tensor(out=o2[:], in0=p3[:], in1=St[:], op=ALU.mult), r=[p3n, Sn], w=[tp + "o2"])
        op("dve", lambda e: e.tensor_tensor(out=dst, in0=o1[:], in1=o2[:], op=ALU.add), r=[tp + "o1", tp + "o2"], w=dstnames)

    def he_tiles(stk, tp="he"):
        return (sb(stk, tp + "t0", [128, 512], F32), sb(stk, tp + "sq", [128, 512], BF16), sb(stk, tp + "rt", [128, 512], F32),
                sb(stk, tp + "tnb", [128, 512], BF16), sb(stk, tp + "o1", [128, 512], F32), sb(stk, tp + "o2", [128, 512], F32))

    if stage >= 1:
        with ExitStack() as st:
            W1 = sb(st, "W1", [128, 8, 1664], BF16)
            ws = sc.dsem()
            w_in2 = I["w_in"][0]
            load_w_cols(W1, "W1a", w_in2, 0, 1024, g1, "g1", sc.dsem(), 0)
            load_w_cols(W1, "W1b", w_in2, 3072, 512, g1, "g1", sc.dsem(), 1024)
            load_w_cols(W1, "W1c", w_in2, 4096, 64, g1, "g1", sc.dsem(), 1536)
            load_w_cols(W1, "W1d", w_in2, 4096, 64, g1, "g1", sc.dsem(), 1600)
            W1names = wdeps("W1a") + wdeps("W1b") + wdeps("W1c") + wdeps("W1d")
            psem = sc.dsem()
            cw = sb(st, "cw", [128, 4, 8], F32); cb = sb(st, "cb", [128, 8], F32)
            br = sb(st, "br", [128, 8], F32); bi = sb(st, "bi", [128, 8], F32); lap = sb(st, "lap", [128, 8], F32)
            nsp = sb(st, "nsp", [128, 8], F32)
            fl = [dma_T(cw[:, k, :], I["conv_w"][0, k], 8) for k in range(4)]
            fl.append(dma_T(cb[:], I["conv_b"][0], 8))
            fl.append(dma_T(br[:], I["lru_b_r"][0].rearrange("n d -> (n d)"), 8))
            fl.append(dma_T(bi[:], I["lru_b_i"][0].rearrange("n d -> (n d)"), 8))
            fl.append(dma_T(lap[:], I["lru_a_param"][0], 8))
            op("sp", fl, w=["cw0", "cw1", "cw2", "cw3", "cb", "br", "bi", "lap"], dsem=psem)
            op("act", lambda e: e.activation(out=nsp[:], in_=lap[:], func=AF.Exp, scale=-1.0), r=["lap"], w=["nsp"])
            op("act", lambda e: e.activation(out=nsp[:], in_=nsp[:], func=AF.Ln, bias=1.0), r=["nsp"], w=["nsp"])
            op("dve", lambda e: e.tensor_scalar(out=nsp[:], in0=nsp[:], scalar1=-8.0, scalar2=None, op0=ALU.mult), r=["nsp"], w=["nsp"])
            wr = sb(st, "wr", [128, 8, 128], BF16); wi = sb(st, "wi", [128, 8, 128], BF16)
            op("pool", [lambda e: e.dma_start(out=wr[:], in_=I["lru_w_r"][0].rearrange("n c d -> c n d")),
                        lambda e: e.dma_start(out=wi[:], in_=I["lru_w_i"][0].rearrange("n c d -> c n d"))], w=["wr", "wi"], dsem=sc.dsem())
            gk = sb(st, "gk", [128, 2], F32)
            op("sp", [lambda e: e.dma_start(out=gk[:, 0:1], in_=I["k_norm_g"][0].rearrange("(p o) -> p o", o=1)),
                      lambda e: e.dma_start(out=gk[0:64, 1:2], in_=I["idx_k_norm_g"][0].rearrange("(p o) -> p o", o=1)),
                      lambda e: e.dma_start(out=gk[64:128, 1:2], in_=I["idx_k_norm_g"][0].rearrange("(p o) -> p o", o=1))], w=["gk"], dsem=sc.dsem())
            XR = make_xT(st, "p1")
            xT = [sb(st, "p1xT%d" % i, [128, 8, 512], BF16) for i in range(2)]
            RP = rope_tables(st, I["pos_full"], 0, 512, "r1")
            HT = he_tiles(st)
            lxs = sb(st, "lxs", [128, 8, 515], F32)
            op("dve", lambda e: e.memset(lxs[:, :, 0:3], 0.0), w=["lxs%d" % n for n in range(8)])
            state = sb(st, "lstate", [128, 8], F32)
            op("dve", lambda e: e.memset(state[:], 0.0), w=["lstate%d" % n for n in range(8)])
            LS = [{nm: sb(st, "L%d" % k_ + nm, [128, 512], F32) for nm in ("xa", "r", "i", "a", "a2", "u", "h")} for k_ in range(3)]
            xabS = [sb(st, "L%dxab" % k_, [128, 512], BF16) for k_ in range(3)]
            hst = [sb(st, "hst%d" % i, [128, 8, 512], BF16) for i in range(2)]
            kst = [sb(st, "kst%d" % i, [128, 3, 512], BF16) for i in range(2)]
            vst = [sb(st, "vst%d" % i, [128, 4, 256], BF16) for i in range(2)]
            hsem = [sc.dsem() for _ in range(2)]; ksem = [sc.dsem() for _ in range(2)]; vsem = [sc.dsem() for _ in range(2)]

            def rr_merge(gens):
                gens = [g_ for g_ in gens if g_ is not None]
                while gens:
                    for g_ in list(gens):
                        try:
                            next(g_)
                        except StopIteration:
                            gens.remove(g_)

            def lru_chain(n, k_, sl, xTg, xTn):
                L = LS[k_]; xab = xabS[k_]; P_ = "L%d" % k_
                pb, pbn = nextbank()
                for c in range(8):
                    op("pe", lambda e, c=c: e.matmul(pb[:], lhsT=W1[:, c, n * 128:(n + 1) * 128], rhs=xTg[:, c, :], start=(c == 0), stop=(c == 7)),
                       r=[xTn, "W1a_%d" % c], w=[pbn])
                ln = "lxs%d" % n
                op("act", lambda e: e.activation(out=lxs[:, n, 3:515], in_=pb[:], func=AF.Copy), r=[pbn], w=[ln])
                yield
                op("dve", lambda e: e.tensor_scalar(out=L["xa"][:], in0=lxs[:, n, 3:515], scalar1=cw[:, 3, n:n + 1], scalar2=cb[:, n:n + 1],
                                                    op0=ALU.mult, op1=ALU.add), r=[ln, "cw3", "cb"], w=[P_ + "xa"])
                for k in range(3):
                    op("dve", lambda e, k=k: e.scalar_tensor_tensor(out=L["xa"][:], in0=lxs[:, n, k:k + 512], scalar=cw[:, k, n:n + 1], in1=L["xa"][:],
                                                                    op0=ALU.mult, op1=ALU.add), r=[ln, "cw%d" % k, P_ + "xa"], w=[P_ + "xa"])
                    yield
                op("dve", lambda e: e.tensor_copy(out=lxs[:, n, 0:3], in_=lxs[:, n, 512:515]), r=[ln], w=[ln])
                op("act", lambda e: e.activation(out=xab[:], in_=L["xa"][:], func=AF.Copy), r=[P_ + "xa"], w=[P_ + "xab"])
                yield
                pr, prn = nextbank(); pi_, pin = nextbank()
                op("pe", lambda e: e.matmul(pr[:], lhsT=wr[:, n, :], rhs=xab[:], start=True, stop=True), r=[P_ + "xab", "wr"], w=[prn])
                op("pe", lambda e: e.matmul(pi_[:], lhsT=wi[:, n, :], rhs=xab[:], start=True, stop=True), r=[P_ + "xab", "wi"], w=[pin])
                op("act", lambda e: e.activation(out=L["r"][:], in_=pr[:], func=AF.Sigmoid, bias=br[:, n:n + 1]), r=[prn, "br"], w=[P_ + "r"])
                op("act", lambda e: e.activation(out=L["i"][:], in_=pi_[:], func=AF.Sigmoid, bias=bi[:, n:n + 1]), r=[pin, "bi"], w=[P_ + "i"])
                yield
                op("act", lambda e: e.activation(out=L["a"][:], in_=L["r"][:], func=AF.Exp, scale=nsp[:, n:n + 1]), r=[P_ + "r", "nsp"], w=[P_ + "a"])
                yield
                op("act", lambda e: e.activation(out=L["a2"][:], in_=L["a"][:], func=AF.Square), r=[P_ + "a"], w=[P_ + "a2"])
                yield
                op("dve", lambda e: e.tensor_scalar(out=L["a2"][:], in0=L["a2"][:], scalar1=-1.0, scalar2=1.0000001, op0=ALU.mult, op1=ALU.add), r=[P_ + "a2"], w=[P_ + "a2"])
                yield
                op("act", lambda e: e.activation(out=L["a2"][:], in_=L["a2"][:], func=AF.Ln), r=[P_ + "a2"], w=[P_ + "a2"])
                yield
                op("act", lambda e: e.activation(out=L["a2"][:], in_=L["a2"][:], func=AF.Exp, scale=0.5), r=[P_ + "a2"], w=[P_ + "a2"])
                yield
                op("dve", lambda e: e.tensor_tensor(out=L["u"][:], in0=L["a2"][:], in1=L["i"][:], op=ALU.mult), r=[P_ + "a2", P_ + "i"], w=[P_ + "u"])
                yield
                op("dve", lambda e: e.tensor_tensor(out=L["u"][:], in0=L["u"][:], in1=L["xa"][:], op=ALU.mult), r=[P_ + "u", P_ + "xa"], w=[P_ + "u"])
                yield
                op("dve", lambda e: e.tensor_tensor_scan(out=L["h"][:], data0=L["a"][:], data1=L["u"][:], initial=state[:, n:n + 1],
                                                         op0=ALU.mult, op1=ALU.add), r=[P_ + "a", P_ + "u", "lstate%d" % n], w=[P_ + "h"])
                op("dve", lambda e: e.tensor_copy(out=state[:, n:n + 1], in_=L["h"][:, 511:512]), r=[P_ + "h"], w=["lstate%d" % n])
                yield
                op("act", lambda e: e.activation(out=hst[sl][:, n, :], in_=L["h"][:], func=AF.Copy), r=[P_ + "h"], w=["hst%d" % sl])
                yield

            def rope_chain(g):
                for _ in rope_fill(RP, g):
                    yield

            def khead_chain(j, sl, xTg, xTn):
                pb, pbn = nextbank()
                c0 = 1024 + j * 128 if j < 2 else 1536
                wn = "W1b_%d" if j < 2 else "W1c_%d"
                for c in range(8):
                    op("pe", lambda e, c=c: e.matmul(pb[:], lhsT=W1[:, c, c0:c0 + 128], rhs=xTg[:, c, :], start=(c == 0), stop=(c == 7)),
                       r=[xTn, wn % c, "W1d_%d" % c], w=[pbn])
                if j < 2:
                    gen = head_epilogue(HT, pb, pbn, gk[:, 0:1], "gk", CB["ones"][:], "kb_ones", CB["rtm"][:], "kb_rtm",
                                        RP["T"]["cm"], "r1cm", RP["T"]["sm"], "r1sm", kst[sl][:, j, :], ["kst%d" % sl])
                else:
                    gen = head_epilogue(HT, pb, pbn, gk[:, 1:2], "gk", CB["blkones"][:], "kb_blkones", CB["rti"][:], "kb_rti",
                                        RP["T"]["ci"], "r1ci", RP["T"]["si"], "r1si", kst[sl][:, j, :], ["kst%d" % sl])
                for _ in gen:
                    yield

            def v_chain(sl, xTg, xTn):
                for t in range(4):
                    pb, pbn = nextbank()
                    for c in range(8):
                        op("pe", lambda e, c=c, t=t, pb=pb: e.matmul(pb[:, 0:256], lhsT=xTg[:, c, t * 128:(t + 1) * 128], rhs=W1[:, c, 1280:1536], start=(c == 0), stop=(c == 7)),
                           r=[xTn, "W1b_%d" % c], w=[pbn])
                    op("act", lambda e, t=t, pb=pb: e.activation(out=vst[sl][:, t, :], in_=pb[:, 0:256], func=AF.Copy), r=[pbn], w=["vst%d" % sl])
                    yield

            NG1 = min(S // 512, NG_DEBUG)
            if NG1 > 0:
                for _ in xT_group(XR, I["x_full"], 0, xT[0], "p1xT0"): pass
            for g in range(NG1):
                sl = g % 2
                xTg = xT[sl]; xTn = "p1xT%d" % sl
                nxt = xT_group(XR, I["x_full"], g + 1, xT[1 - sl], "p1xT%d" % (1 - sl)) if g + 1 < NG1 else None
                rr_merge([lru_chain(0, 0, sl, xTg, xTn), lru_chain(1, 1, sl, xTg, xTn), lru_chain(2, 2, sl, xTg, xTn), rope_chain(g)])
                rr_merge([lru_chain(3, 0, sl, xTg, xTn), lru_chain(4, 1, sl, xTg, xTn), lru_chain(5, 2, sl, xTg, xTn), khead_chain(0, sl, xTg, xTn)])
                rr_merge([lru_chain(6, 0, sl, xTg, xTn), lru_chain(7, 1, sl, xTg, xTn), khead_chain(1, sl, xTg, xTn), nxt])
                rr_merge([khead_chain(2, sl, xTg, xTn), v_chain(sl, xTg, xTn)])
                op("sp", lambda e, g=g, sl=sl: e.dma_start(out=hT_d[:, :, g * 512:(g + 1) * 512].rearrange("c p t -> p c t"), in_=hst[sl][:]),
                   r=["hst%d" % sl], w=["hT_d"], dsem=hsem[sl])
                op("sp", [lambda e, g=g, sl=sl: e.dma_start(out=kT_d[:, :, g * 512:(g + 1) * 512].rearrange("c p t -> p c t"), in_=kst[sl][:, 0:2, :]),
                          lambda e, g=g, sl=sl: e.dma_start(out=kiT_d[:, g * 512:(g + 1) * 512], in_=kst[sl][:, 2, :])],
                   r=["kst%d" % sl], w=["kT_d", "kiT_d"], dsem=ksem[sl])
                op("sp", lambda e, g=g, sl=sl: e.dma_start(out=V_d[g * 512:(g + 1) * 512, :].rearrange("(t p) d -> p t d", p=128), in_=vst[sl][:]),
                   r=["vst%d" % sl], w=["V_d"], dsem=vsem[sl])
            sc.barrier()

    if stage >= 2:
        with ExitStack() as st:
            W2 = sb(st, "W2", [128, 8, 4616], BF16)
            w_in2 = I["w_in"][0]
            load_w_cols(W2, "W2a", w_in2, 1024, 2048, g1, "g1", sc.dsem(), 0)
            load_w_cols(W2, "W2b", w_in2, 3584, 512, g1, "g1", sc.dsem(), 2048)
            load_w_cols(W2, "W2c", w_in2, 4160, 2056, g1, "g1", sc.dsem(), 2560)
            gq = sb(st, "gq", [128, 1], F32)
            op("sp", lambda e: e.dma_start(out=gq[:], in_=I["q_norm_g"][0].rearrange("(p o) -> p o", o=1)), w=["gq"], dsem=sc.dsem())
            XR2 = make_xT(st, "p2", nslot=1)
            xT2 = [sb(st, "p2xT%d" % i, [128, 8, 512], BF16) for i in range(2)]
            RP2 = rope_tables(st, I["pos_own"], 0, 512, "r2")
            HT2 = [he_tiles(st, "h2"), he_tiles(st, "h3")]
            hh = [sb(st, "hh%d" % i, [128, 1024], BF16) for i in range(2)]
            hhsem = [sc.dsem() for _ in range(2)]
            GS = [{nm: sb(st, "G%d" % k_ + nm, [128, 512], F32) for nm in ("z2", "t", "sg", "hs")} for k_ in range(2)]
            yst = [sb(st, "yst0", [128, 8, 512], BF16)] * 2
            qst = [sb(st, "qst0", [128, 8, 512], BF16)] * 2
            qist = [sb(st, "qist0", [128, 4, 512], BF16)] * 2
            mst = [sb(st, "mst%d" % i, [128, 8, 512], BF16) for i in range(2)]
            ysem = [sc.dsem()] * 2; qsem = [sc.dsem()] * 2
            qisem = [sc.dsem()] * 2; msem = [sc.dsem() for _ in range(2)]

            def rr_merge2(gens):
                gens = [g_ for g_ in gens if g_ is not None]
                while gens:
                    for g_ in list(gens):
                        try:
                            next(g_)
                        except StopIteration:
                            gens.remove(g_)

            def gate_chain(n, k_, g, xTg, xTn):
                G = GS[k_]; P_ = "G%d" % k_
                op("sp", lambda e: e.dma_start(out=hh[k_][:], in_=hT_d[n, :, g * 1024:(g + 1) * 1024]), r=["hT_d"], w=["hh%d" % k_], dsem=hhsem[k_])
                pb, pbn = nextbank()
                for c in range(8):
                    op("pe", lambda e, c=c: e.matmul(pb[:], lhsT=W2[:, c, n * 128:(n + 1) * 128], rhs=xTg[:, c, :], start=(c == 0), stop=(c == 7)),
                       r=[xTn, "W2a_%d" % c], w=[pbn])
                op("act", lambda e: e.activation(out=G["z2"][:], in_=pb[:], func=AF.Copy), r=[pbn], w=[P_ + "z2"])
                yield
                op("act", lambda e: e.activation(out=G["t"][:], in_=G["z2"][:], func=AF.Square), r=[P_ + "z2"], w=[P_ + "t"])
                yield
                op("dve", lambda e: e.tensor_scalar(out=G["t"][:], in0=G["t"][:], scalar1=0.044715, scalar2=1.0, op0=ALU.mult, op1=ALU.add), r=[P_ + "t"], w=[P_ + "t"])
                yield
                op("dve", lambda e: e.tensor_tensor(out=G["t"][:], in0=G["t"][:], in1=G["z2"][:], op=ALU.mult), r=[P_ + "t", P_ + "z2"], w=[P_ + "t"])
                yield
                op("act", lambda e: e.activation(out=G["sg"][:], in_=G["t"][:], func=AF.Sigmoid, scale=1.5957691216), r=[P_ + "t"], w=[P_ + "sg"])
                yield
                op("dve", lambda e: e.tensor_tensor(out=G["sg"][:], in0=G["sg"][:], in1=G["z2"][:], op=ALU.mult), r=[P_ + "sg", P_ + "z2"], w=[P_ + "sg"])
                yield
                hv = hh[k_][:].rearrange("p (t two q) -> p t two q", two=2, q=128)
                hsv = G["hs"][:].rearrange("p (t q) -> p t q", q=128)
                op("dve", lambda e: e.tensor_scalar(out=hsv, in0=hv[:, :, 0, :], scalar1=C["sel"][:, 0:1], scalar2=None, op0=ALU.mult),
                   r=["hh%d" % k_, "k_sel"], w=[P_ + "hs"])
                yield
                op("dve", lambda e: e.scalar_tensor_tensor(out=hsv, in0=hv[:, :, 1, :], scalar=C["sel"][:, 1:2], in1=hsv, op0=ALU.mult, op1=ALU.add),
                   r=["hh%d" % k_, "k_sel", P_ + "hs"], w=[P_ + "hs"])
                yield
                op("dve", lambda e: e.tensor_tensor(out=yst[0][:, n, :], in0=G["hs"][:], in1=G["sg"][:], op=ALU.mult), r=[P_ + "hs", P_ + "sg"], w=["yst0"])
                yield

            def qhead_chain(hd, k_, xTg, xTn):
                pb, pbn = nextbank()
                for c in range(8):
                    op("pe", lambda e, c=c: e.matmul(pb[:], lhsT=W2[:, c, 1024 + hd * 128:1024 + (hd + 1) * 128], rhs=xTg[:, c, :], start=(c == 0), stop=(c == 7)),
                       r=[xTn, "W2a_%d" % c], w=[pbn])
                for _ in head_epilogue(HT2[k_], pb, pbn, gq[:, 0:1], "gq", CB["ones"][:], "kb_ones", CB["rtm"][:], "kb_rtm",
                                       RP2["T"]["cm"], "r2cm", RP2["T"]["sm"], "r2sm", qst[0][:, hd, :], ["qst0"], tp="h%d" % (2 + k_)):
                    yield

            def qidx_chain(hp, k_, xTg, xTn):
                pb, pbn = nextbank()
                for c in range(8):
                    op("pe", lambda e, c=c: e.matmul(pb[:], lhsT=W2[:, c, 2048 + hp * 128:2048 + (hp + 1) * 128], rhs=xTg[:, c, :], start=(c == 0), stop=(c == 7)),
                       r=[xTn, "W2b_%d" % c], w=[pbn])
                for _ in head_epilogue(HT2[k_], pb, pbn, None, None, None, None, CB["rti"][:], "kb_rti",
                                       RP2["T"]["ci"], "r2ci", RP2["T"]["si"], "r2si", qist[0][:, hp, :], ["qist0"], norm=False, tp="h%d" % (2 + k_)):
                    yield

            def widx_chain(g, xTg, xTn):
                for t in range(4):
                    pb, pbn = nextbank()
                    for c in range(8):
                        op("pe", lambda e, c=c, t=t, pb=pb: e.matmul(pb[:, 0:8], lhsT=xTg[:, c, t * 128:(t + 1) * 128], rhs=W2[:, c, 2560:2568], start=(c == 0), stop=(c == 7)),
                           r=[xTn, "W2c_%d" % c], w=[pbn])
                    op("act", lambda e, t=t, pb=pb: e.activation(out=wq[:, g * 4 + t, :], in_=pb[:, 0:8], func=AF.Copy, scale=float((8 ** -0.5) * (64 ** -0.5))),
                       r=[pbn], w=["wq"])
                    yield

            def mg_chain(g, mh, m8s, xTg, xTn, last):
                for m8 in m8s:
                    m = mh * 8 + m8
                    pb, pbn = nextbank()
                    for c in range(8):
                        op("pe", lambda e, c=c, m=m, pb=pb: e.matmul(pb[:], lhsT=W2[:, c, 2568 + m * 128:2568 + (m + 1) * 128], rhs=xTg[:, c, :], start=(c == 0), stop=(c == 7)),
                           r=[xTn, "W2c_%d" % c], w=[pbn])
                    op("act", lambda e, m8=m8, pb=pb: e.activation(out=mst[mh][:, m8, :], in_=pb[:], func=AF.Sigmoid), r=[pbn], w=["mst%d" % mh])
                    yield
                if last:
                    op("sp", lambda e: e.dma_start(out=mgT_d[mh * 8:(mh + 1) * 8, :, g * 512:(g + 1) * 512].rearrange("c p t -> p c t"), in_=mst[mh][:]),
                       r=["mst%d" % mh], w=["mgT_d"], dsem=msem[mh])

            def rope2_chain(g):
                for _ in rope_fill(RP2, g):
                    yield

            NG2 = min(SO // 512, NG2_DEBUG)
            if NG2 > 0:
                for _ in xT_group(XR2, I["x_own"], 0, xT2[0], "p2xT0"): pass
            for g in range(NG2):
                sl = g % 2
                xTg = xT2[sl]; xTn = "p2xT%d" % sl
                nxt = xT_group(XR2, I["x_own"], g + 1, xT2[1 - sl], "p2xT%d" % (1 - sl)) if g + 1 < NG2 else None
                rr_merge2([gate_chain(0, 0, g, xTg, xTn), gate_chain(1, 1, g, xTg, xTn), rope2_chain(g), mg_chain(g, 0, range(0, 4), xTg, xTn, False)])
                rr_merge2([gate_chain(2, 0, g, xTg, xTn), gate_chain(3, 1, g, xTg, xTn), qhead_chain(0, 0, xTg, xTn), qhead_chain(1, 1, xTg, xTn),
                           mg_chain(g, 0, range(4, 8), xTg, xTn, True)])
                rr_merge2([gate_chain(4, 0, g, xTg, xTn), gate_chain(5, 1, g, xTg, xTn), qhead_chain(2, 0, xTg, xTn), qhead_chain(3, 1, xTg, xTn),
                           mg_chain(g, 1, range(0, 4), xTg, xTn, False)])
                rr_merge2([gate_chain(6, 0, g, xTg, xTn), gate_chain(7, 1, g, xTg, xTn), qhead_chain(4, 0, xTg, xTn), qhead_chain(5, 1, xTg, xTn),
                           mg_chain(g, 1, range(4, 8), xTg, xTn, True)])
                op("sp", lambda e, g=g: e.dma_start(out=yaT_d[:, :, g * 512:(g + 1) * 512].rearrange("c p t -> p c t"), in_=yst[0][:]),
                   r=["yst0"], w=["yaT_d"], dsem=ysem[0])
                rr_merge2([qhead_chain(6, 0, xTg, xTn), qhead_chain(7, 1, xTg, xTn), widx_chain(g, xTg, xTn), nxt])
                op("sp", lambda e, g=g: e.dma_start(out=qT_d[:, :, g * 512:(g + 1) * 512].rearrange("c p t -> p c t"), in_=qst[0][:]),
                   r=["qst0"], w=["qT_d"], dsem=qsem[0])
                rr_merge2([qidx_chain(0, 0, xTg, xTn), qidx_chain(1, 1, xTg, xTn)])
                rr_merge2([qidx_chain(2, 0, xTg, xTn), qidx_chain(3, 1, xTg, xTn)])
                op("sp", lambda e, g=g: e.dma_start(out=qiT_d[:, :, g * 512:(g + 1) * 512].rearrange("c p t -> p c t"), in_=qist[0][:]),
                   r=["qist0"], w=["qiT_d"], dsem=qisem[0])
            sc.barrier()

    if stage >= 3:
        with ExitStack() as st:
            kT = sb(st, "kT", [128, 2, S], BF16); kiT = sb(st, "kiT", [128, S], BF16); Vs = sb(st, "Vs", [128, 64, 256], BF16)
            lsem = sc.dsem()
            fl = []
            NQ4 = min(4, (min(NG_DEBUG * 512, S) + 2047) // 2048)
            for j in range(2):
                for q4 in range(NQ4):
                    fl.append(lambda e, j=j, q4=q4: e.dma_start(out=kT[:, j, q4 * 2048:(q4 + 1) * 2048], in_=kT_d[j, :, q4 * 2048:(q4 + 1) * 2048]))
            for q4 in range(NQ4):
                fl.append(lambda e, q4=q4: e.dma_start(out=kiT[:, q4 * 2048:(q4 + 1) * 2048], in_=kiT_d[:, q4 * 2048:(q4 + 1) * 2048]))
            for q4 in range(NQ4):
                fl.append(lambda e, q4=q4: e.dma_start(out=Vs[:, q4 * 16:(q4 + 1) * 16, :], in_=V_d[q4 * 2048:(q4 + 1) * 2048, :].rearrange("(t p) d -> p t d", p=128)))
            op("sp", fl, r=["kT_d", "kiT_d", "V_d"], w=["kT", "kiT", "Vs"], dsem=lsem)
            scoresL = [sb(st, "scores%d" % i, [128, S], F32) for i in range(2)]
            junk = sb(st, "ajunk", [128, S], mybir.dt.uint8)
            maskb = [sb(st, "maskb%d" % i, [128, S], BF16) for i in range(2)]
            iselb = sb(st, "iselb", [128, 4, 128], BF16)
            for h4 in range(4):
                op("dve", lambda e, h4=h4: e.tensor_scalar(out=iselb[:, h4, :], in0=CB["ident"][:], scalar1=float(BIG), scalar2=None, op0=ALU.mult),
                   r=["kb_ident"], w=["iselb"])
            qTb = [sb(st, "qTb%d" % i, [128, 8, 128], BF16) for i in range(2)]
            qiTb = [sb(st, "qiTb%d" % i, [128, 4, 128], BF16) for i in range(2)]
            qsm = [sc.dsem() for _ in range(2)]; qsm2 = [sc.dsem() for _ in range(2)]
            Rt = [sb(st, "Rt%d" % i, [128, 512], BF16) for i in range(2)]
            diagw = sb(st, "diagw", [128, 8, 128], BF16)
            Et = [sb(st, "Et%d" % i, [128, 512], BF16) for i in range(3)]
            bs = sb(st, "bs", [128, 8], F32)
            Wk = sb(st, "Wk", [128, NBIS], F32)
            p2k = sb(st, "p2k", [128, NBIS], F32)
            for k in range(NBIS):
                op("dve", lambda e, k=k: e.memset(p2k[:, k:k + 1], float(0.5 ** (k + 1))), w=["p2k"])
            rden = sb(st, "rden", [128, 512], F32)
            ybst = [sb(st, "ybst0", [128, 8, 128], BF16)] * 2
            ybsem = [sc.dsem()] * 2
            SCALE = float(128 ** -0.5)
            cnts = {"R": 0, "E": 0, "z": 0}

            def gen_I(i):
                sl = i % 2
                nkt = 2 * i + 2; Lk = nkt * 128
                scores = scoresL[sl]; scn = "scores%d" % sl
                op("sp", lambda e: e.dma_start(out=qiTb[sl][:], in_=qiT_d[:, :, i * 128:(i + 1) * 128].rearrange("c p t -> p c t")),
                   r=["qiT_d"], w=["qiTb%d" % sl], dsem=qsm[sl])
                for h in range(8):
                    op("dve", lambda e, h=h: e.tensor_scalar(out=diagw[:, h, :], in0=CB["ident"][:], scalar1=wq[:, i, h:h + 1], scalar2=None, op0=ALU.mult),
                       r=["kb_ident", "wq"], w=["diagw"])
                yield
                for kg in range((nkt + 3) // 4):
                    W_ = min(4, nkt - 4 * kg) * 128
                    psc, pscn = PS[6 + kg % 2], psn[6 + kg % 2]
                    pend_ = None
                    for h in range(9):
                        if h < 8:
                            zi = 4 + cnts["z"] % 2; cnts["z"] += 1
                            pz, pzn = PS[zi], psn[zi]
                            pb0 = 64 * (h % 2)
                            op("pe", lambda e, h=h, kg=kg, W_=W_, pz=pz, pb0=pb0: e.matmul(pz[:, 0:W_], lhsT=qiTb[sl][pb0:pb0 + 64, h // 2, :],
                                                                                       rhs=kiT[pb0:pb0 + 64, kg * 512:kg * 512 + W_], start=True, stop=True),
                               r=["qiTb%d" % sl, "kiT"], w=[pzn])
                            rt_ = cnts["R"] % 2; cnts["R"] += 1
                            op("act", lambda e, W_=W_, pz=pz, rt_=rt_: e.activation(out=Rt[rt_][:, 0:W_], in_=pz[:, 0:W_], func=AF.Relu), r=[pzn], w=["Rt%d" % rt_])
                        if pend_ is not None:
                            ph, prt = pend_
                            op("pe", lambda e, ph=ph, W_=W_, psc=psc, prt=prt: e.matmul(psc[:, 0:W_], lhsT=diagw[:, ph, :], rhs=Rt[prt][:, 0:W_], start=(ph == 0), stop=(ph == 7)),
                               r=["Rt%d" % prt, "diagw"], w=[pscn])
                        pend_ = (h, rt_) if h < 8 else None
                    op("act", lambda e, kg=kg, W_=W_, psc=psc: e.activation(out=scores[:, kg * 512:kg * 512 + W_], in_=psc[:, 0:W_], func=AF.Copy), r=[pscn], w=[scn])
                    yield

            def gen_T(i):
                sl = i % 2
                nkt = 2 * i + 2; Lk = nkt * 128
                scores = scoresL[sl]; scn = "scores%d" % sl
                op("dve", lambda e: e.tensor_tensor(out=scores[:, Lk - 256:Lk], in0=scores[:, Lk - 256:Lk], in1=C["amask"][:], op=ALU.add),
                   r=[scn, "k_amask"], w=[scn])
                if i == 0:
                    op("dve", lambda e: e.memset(bs[:, 0:1], float(-BIG / 2)), w=["bs"])
                else:
                    op("dve", lambda e: e.tensor_reduce(out=bs[:, 1:2], in_=scores[:, 0:Lk], axis=AX.X, op=ALU.max), r=[scn], w=["bs"])
                    op("dve", lambda e: e.tensor_reduce(out=bs[:, 0:1], in_=scores[:, 0:Lk - 256], axis=AX.X, op=ALU.min), r=[scn, "bs"], w=["bs"])
                    op("dve", lambda e: e.scalar_tensor_tensor(out=bs[:, 1:2], in0=bs[:, 1:2], scalar=1.0, in1=bs[:, 0:1], op0=ALU.add, op1=ALU.subtract), r=["bs"], w=["bs"])
                    op("dve", lambda e: e.tensor_scalar(out=Wk[:], in0=p2k[:], scalar1=bs[:, 1:2], scalar2=None, op0=ALU.mult), r=["bs", "p2k"], w=["Wk"])
                    yield
                    for k in range(NBIS):
                        op("dve", lambda e, k=k: e.tensor_tensor(out=bs[:, 2:3], in0=bs[:, 0:1], in1=Wk[:, k:k + 1], op=ALU.add), r=["bs", "Wk"], w=["bs"])
                        op("dve", lambda e: e.tensor_scalar(out=junk[:, 0:Lk], in0=scores[:, 0:Lk], scalar1=bs[:, 2:3], scalar2=None, op0=ALU.is_ge, op1=ALU.add,
                                                            accum_out=bs[:, 3:4]), r=[scn, "bs"], w=["bs", "ajunk"])
                        op("dve", lambda e, k=k: e.scalar_tensor_tensor(out=bs[:, 4:5], in0=bs[:, 3:4], scalar=float(TOPK - 0.5), in1=Wk[:, k:k + 1], op0=ALU.is_ge, op1=ALU.mult),
                           r=["bs", "Wk"], w=["bs"])
                        op("dve", lambda e: e.tensor_tensor(out=bs[:, 0:1], in0=bs[:, 0:1], in1=bs[:, 4:5], op=ALU.add), r=["bs"], w=["bs"])
                        yield
                op("dve", lambda e: e.tensor_scalar(out=maskb[sl][:, 0:Lk], in0=scores[:, 0:Lk], scalar1=bs[:, 0:1], scalar2=1.0, op0=ALU.is_ge, op1=ALU.subtract),
                   r=[scn, "bs"], w=["maskb%d" % sl])
                yield

            def gen_A(i):
                sl = i % 2
                nkt = 2 * i + 2
                mb = maskb[sl]; mbn = "maskb%d" % sl
                op("sp", lambda e: e.dma_start(out=qTb[sl][:], in_=qT_d[:, :, i * 128:(i + 1) * 128].rearrange("c p t -> p c t")),
                   r=["qT_d"], w=["qTb%d" % sl], dsem=qsm2[sl])
                for j in range(2):
                    prev = None
                    for kt in range(nkt + 1):
                        if kt < nkt:
                            si = cnts["E"] % 2
                            ps_, psn_ = PS[si], psn[si]
                            op("pe", lambda e, kt=kt, ps_=ps_, j=j: e.matmul(ps_[:], lhsT=kT[:, j, kt * 128:(kt + 1) * 128], rhs=qTb[sl][:, 4 * j:4 * j + 4, :],
                                                                           start=True, stop=False), r=["kT", "qTb%d" % sl], w=[psn_])
                            op("pe", lambda e, kt=kt, ps_=ps_: e.matmul(ps_[:], lhsT=mb[:, kt * 128:(kt + 1) * 128], rhs=iselb[:].rearrange("p h q -> p (h q)"),
                                                                      start=False, stop=True), r=[mbn, "iselb"], w=[psn_])
                            et = cnts["E"] % 3; cnts["E"] += 1
                            op("act", lambda e, ps_=ps_, et=et: e.activation(out=Et[et][:], in_=ps_[:], func=AF.Exp, scale=SCALE), r=[psn_], w=["Et%d" % et])
                        if prev is not None:
                            pkt, pet = prev
                            op("pe", lambda e, pkt=pkt, pet=pet, j=j: e.matmul(PS[2][:], lhsT=Vs[:, pkt, j * 128:(j + 1) * 128], rhs=Et[pet][:], start=(pkt == 0), stop=(pkt == nkt - 1)),
                               r=["Vs", "Et%d" % pet], w=[psn[2]])
                            op("pe", lambda e, pkt=pkt, pet=pet: e.matmul(PS[3][:], lhsT=CB["ones"][:], rhs=Et[pet][:], start=(pkt == 0), stop=(pkt == nkt - 1)),
                               r=["kb_ones", "Et%d" % pet], w=[psn[3]])
                        prev = (kt, et) if kt < nkt else None
                        yield
                    op("act", lambda e: e.activation(out=rden[:], in_=PS[3][:], func=AF.Ln), r=[psn[3]], w=["rden"])
                    op("act", lambda e: e.activation(out=rden[:], in_=rden[:], func=AF.Exp, scale=-1.0), r=["rden"], w=["rden"])
                    op("dve", lambda e, j=j: e.tensor_tensor(out=ybst[sl][:, 4 * j:4 * j + 4, :], in0=PS[2][:].rearrange("p (h q) -> p h q", q=128),
                                                            in1=rden[:].rearrange("p (h q) -> p h q", q=128), op=ALU.mult),
                       r=[psn[2], "rden"], w=["ybst0"])
                op("sp", lambda e: e.dma_start(out=ybT_d[:, :, i * 128:(i + 1) * 128].rearrange("c p t -> p c t"), in_=ybst[sl][:]),
                   r=["ybst0"], w=["ybT_d"], dsem=ybsem[sl])
                yield

            def n_I(i):
                return 1 + (2 * i + 2 + 3) // 4

            def n_T(i):
                return (0 if i == 0 else 1 + NBIS) + 1

            def n_A(i):
                return 2 * (2 * i + 3) + 1

            def merge(gens):
                st_ = [[g, n, 0, False] for g, n in gens if g is not None]
                while any(not x[3] for x in st_):
                    best = None
                    for x in st_:
                        if x[3]: continue
                        frac = x[2] / float(x[1])
                        if best is None or frac < best[0]: best = (frac, x)
                    x = best[1]
                    try:
                        next(x[0]); x[2] += 1
                    except StopIteration:
                        x[3] = True

            NQ = min(SO // 128, NQ_DEBUG)
            for s_ in range(-1, NQ + 1):
                gl = []
                if 0 <= s_ - 1 < NQ: gl.append((gen_A(s_ - 1), n_A(s_ - 1)))
                if 0 <= s_ < NQ: gl.append((gen_T(s_), n_T(s_)))
                if 0 <= s_ + 1 < NQ: gl.append((gen_I(s_ + 1), n_I(s_ + 1)))
                merge(gl)
            sc.barrier()

    NT = SO // 128
    if stage >= 4:
        NTd = min(NT, NG3_DEBUG * 4)
        rt_ = es.enter_context(ExitStack())
        sel1 = sb(rt_, "sel1", [128, NT, 32], F32); sel2 = sb(rt_, "sel2", [128, NT, 32], F32)
        rank = sb(rt_, "rank", [128, NT, 32], F32); gates = sb(rt_, "gates", [128, NT, 2], F32)
        Asum = sb(rt_, "Asum", [128, 32], BF16)
        d1i = sb(rt_, "d1i", [128, NT], I32); d2i = sb(rt_, "d2i", [128, NT], I32)
        op("dve", lambda e: e.memset(Asum[:], 0.0), w=["Asum"])
        op("dve", lambda e: e.memset(sel1[:], 0.0), w=["sel1"])
        op("dve", lambda e: e.memset(sel2[:], 0.0), w=["sel2"])
        op("dve", lambda e: e.memset(rank[:], 0.0), w=["rank"])
        with ExitStack() as st:
            WA = sb(st, "WA", [128, 8, 1024], BF16); WB = sb(st, "WB", [128, 8, 1024], BF16); WO = sb(st, "WO", [128, 8, 1024], BF16)
            load_w_cols(WA, "WA", I["w_lru_out"][0], 0, 1024, None, None, sc.dsem())
            load_w_cols(WB, "WB", I["w_attn_out"][0], 0, 1024, None, None, sc.dsem())
            load_w_cols(WO, "WO", I["w_o"][0], 0, 1024, None, None, sc.dsem())
            WR = sb(st, "WR", [128, 8, 36], F32)
            op("sp", [lambda e: e.dma_start(out=WR[:, :, 0:4], in_=I["w_grp"][0].rearrange("(c p) n -> p c n", p=128)),
                      lambda e: e.dma_start(out=WR[:, :, 4:36], in_=I["w_exp"][0].rearrange("(c p) n -> p c n", p=128))], w=["WR"], dsem=sc.dsem())
            g2bc = sb(st, "g2bc", [128, 1024], F32); bbc = sb(st, "bbc", [128, 36], F32)
            op("sp", [lambda e: e.dma_start(out=g2bc[:], in_=I["norm2_g"][0].partition_broadcast(128)),
                      lambda e: e.dma_start(out=bbc[:, 0:4], in_=I["b_grp"][0].partition_broadcast(128)),
                      lambda e: e.dma_start(out=bbc[:, 4:36], in_=I["b_exp"][0].partition_broadcast(128))], w=["g2bc", "bbc"], dsem=sc.dsem())
            ya_sL = [sb(st, "ya_s%d" % i, [128, 8, 512], BF16) for i in range(2)]; yb_sL = [sb(st, "yb_s%d" % i, [128, 8, 512], BF16) for i in range(2)]
            mg_sL = [sb(st, "mg_s%d" % i, [128, 16, 512], BF16) for i in range(2)]
            insem = [sc.dsem() for _ in range(2)]
            mixTL = [sb(st, "mixT%d" % i, [128, 8, 512], BF16) for i in range(2)]
            t1 = sb(st, "ot1", [128, 512], F32); t2 = sb(st, "ot2", [128, 512], F32)
            xin = [sb(st, "oxin%d" % i, [128, 1024], F32) for i in range(2)]; xsem = [sc.dsem() for _ in range(2)]
            x1 = [sb(st, "ox1_%d" % i, [128, 1024], F32) for i in range(2)]; x1sem = [sc.dsem() for _ in range(2)]
            h2L = [sb(st, "oh2_%d" % i, [128, 1024], F32) for i in range(2)]
            h2b = [sb(st, "oh2b%d" % i, [128, 1024], BF16) for i in range(2)]; h2sem = [sc.dsem() for _ in range(2)]
            h2TL = [sb(st, "oh2T%d" % i, [128, 8, 128], F32) for i in range(2)]
            ojunk = sb(st, "ojunk", [128, 1024], BF16)
            RS = []
            for i_ in range(2):
                RS.append(dict(rs=sb(st, "ors%d" % i_, [128, 16], F32), lg=sb(st, "olg%d" % i_, [128, 36], F32), ohg=sb(st, "ohg%d" % i_, [128, 4], F32),
                               tmp32=sb(st, "otmp32_%d" % i_, [128, 32], F32), ig=sb(st, "oig%d" % i_, [128, 8], F32), m8=sb(st, "om8_%d" % i_, [128, 8], F32),
                               A_b=sb(st, "oAb%d" % i_, [128, 32], BF16)))
            zb = sb(st, "ozb", [128, 1024], BF16)
            op("dve", lambda e: e.memset(zb[:], 0.0), w=["ozb"])
            op("sp", lambda e: e.dma_start(out=H2_d[SO:SO + 128, :], in_=zb[:]), r=["ozb"], w=["H2_d_pad"], dsem=sc.dsem())
            if NG3_DEBUG < 1000:
                op("sp", [lambda e, t=t: e.dma_start(out=H2_d[t * 128:(t + 1) * 128, :], in_=zb[:]) for t in range(NT)], r=["ozb"], w=["H2_d"], dsem=sc.dsem())

            def mix_chain(g):
                k_ = g % 2
                ya_s, yb_s, mg_s, mixT = ya_sL[k_], yb_sL[k_], mg_sL[k_], mixTL[k_]
                yan, ybn, mgn, mxn = "ya_s%d" % k_, "yb_s%d" % k_, "mg_s%d" % k_, "mixT%d" % k_
                op("sp", [lambda e: e.dma_start(out=ya_s[:], in_=yaT_d[:, :, g * 512:(g + 1) * 512].rearrange("c p t -> p c t")),
                          lambda e: e.dma_start(out=yb_s[:], in_=ybT_d[:, :, g * 512:(g + 1) * 512].rearrange("c p t -> p c t")),
                          lambda e: e.dma_start(out=mg_s[:], in_=mgT_d[:, :, g * 512:(g + 1) * 512].rearrange("c p t -> p c t"))],
                   r=["yaT_d", "ybT_d", "mgT_d"], w=[yan, ybn, mgn], dsem=insem[k_])
                for m in range(8):
                    pa, pan = nextbank(); pb, pbn = nextbank()
                    for c in range(8):
                        op("pe", lambda e, c=c, m=m, pa=pa: e.matmul(pa[:], lhsT=WA[:, c, m * 128:(m + 1) * 128], rhs=ya_s[:, c, :], start=(c == 0), stop=(c == 7)),
                           r=[yan, "WA_%d" % c], w=[pan])
                    for c in range(8):
                        op("pe", lambda e, c=c, m=m, pb=pb: e.matmul(pb[:], lhsT=WB[:, c, m * 128:(m + 1) * 128], rhs=yb_s[:, c, :], start=(c == 0), stop=(c == 7)),
                           r=[ybn, "WB_%d" % c], w=[pbn])
                    op("dve", lambda e, m=m, pa=pa: e.tensor_tensor(out=t1[:], in0=pa[:], in1=mg_s[:, m, :], op=ALU.mult), r=[pan, mgn], w=["ot1"])
                    op("dve", lambda e, m=m, pb=pb: e.tensor_tensor(out=t2[:], in0=pb[:], in1=mg_s[:, 8 + m, :], op=ALU.mult), r=[pbn, mgn], w=["ot2"])
                    op("dve", lambda e, m=m: e.tensor_tensor(out=mixT[:, m, :], in0=t1[:], in1=t2[:], op=ALU.add), r=["ot1", "ot2"], w=[mxn])
                    yield

            def tile_chain(g, t, k_):
                mixT = mixTL[g % 2]; mxn = "mixT%d" % (g % 2)
                R_ = RS[k_]; rs, lg, ohg, tmp32, ig, m8, A_b = R_["rs"], R_["lg"], R_["ohg"], R_["tmp32"], R_["ig"], R_["m8"], R_["A_b"]
                rsn, lgn, ohgn, tmpn, ign, m8n, Abn = ("ors%d" % k_, "olg%d" % k_, "ohg%d" % k_, "otmp32_%d" % k_, "oig%d" % k_, "om8_%d" % k_, "oAb%d" % k_)
                h2 = h2L[k_]; h2n = "oh2_%d" % k_; h2T = h2TL[k_]; h2Tn = "oh2T%d" % k_
                ti = g * 4 + t; sl = k_
                op("sp", lambda e: e.dma_start(out=xin[sl][:], in_=I["x_own"][ti * 128:(ti + 1) * 128, :]), w=["oxin%d" % sl], dsem=xsem[sl])
                for nh in range(2):
                    po, pon = nextbank()
                    for c in range(8):
                        op("pe", lambda e, c=c, nh=nh, po=po: e.matmul(po[:], lhsT=mixT[:, c, t * 128:(t + 1) * 128], rhs=WO[:, c, nh * 512:(nh + 1) * 512], start=(c == 0), stop=(c == 7)),
                           r=[mxn, "WO_%d" % c], w=[pon])
                    op("dve", lambda e, nh=nh, po=po: e.tensor_tensor(out=x1[sl][:, nh * 512:(nh + 1) * 512], in0=po[:], in1=xin[sl][:, nh * 512:(nh + 1) * 512], op=ALU.add),
                       r=[pon, "oxin%d" % sl], w=["ox1_%d" % sl])
                    yield
                op("sp", lambda e: e.dma_start(out=X1_d[ti * 128:(ti + 1) * 128, :], in_=x1[sl][:]), r=["ox1_%d" % sl], w=["X1_d"], dsem=x1sem[sl])
                op("act", lambda e: e.activation(out=ojunk[:], in_=x1[sl][:], func=AF.Square, accum_out=rs[:, 0:1]), r=["ox1_%d" % sl], w=["ojunk", rsn])
                op("act", lambda e: e.activation(out=rs[:, 1:2], in_=rs[:, 0:1], func=AF.Ln, scale=float(1.0 / D), bias=float(EPS)), r=[rsn], w=[rsn])
                op("act", lambda e: e.activation(out=rs[:, 1:2], in_=rs[:, 1:2], func=AF.Exp, scale=-0.5), r=[rsn], w=[rsn])
                yield
                op("dve", lambda e: e.scalar_tensor_tensor(out=h2[:], in0=x1[sl][:], scalar=rs[:, 1:2], in1=g2bc[:], op0=ALU.mult, op1=ALU.mult),
                   r=["ox1_%d" % sl, rsn, "g2bc"], w=[h2n])
                yield
                op("act", lambda e: e.activation(out=h2b[sl][:], in_=h2[:], func=AF.Copy), r=[h2n], w=["oh2b%d" % sl])
                op("sp", lambda e: e.dma_start(out=H2_d[ti * 128:(ti + 1) * 128, :], in_=h2b[sl][:]), r=["oh2b%d" % sl], w=["H2_d"], dsem=h2sem[sl])
                yield
                for half in range(2):
                    bank, bn = nextbank()
                    for c4 in range(4):
                        c = half * 4 + c4
                        op("pe", lambda e, c=c, c4=c4, bank=bank: e.transpose(out=bank[:, c4 * 128:(c4 + 1) * 128], in_=h2[:, c * 128:(c + 1) * 128], identity=C["ident"][:]),
                           r=[h2n, "k_ident"], w=[bn])
                    op("act", lambda e, half=half, bank=bank: e.activation(out=h2T[:, half * 4:half * 4 + 4, :], in_=bank[:].rearrange("p (c q) -> p c q", q=128), func=AF.Copy),
                       r=[bn], w=[h2Tn])
                    yield
                pl, pln = nextbank()
                for c in range(8):
                    op("pe", lambda e, c=c: e.matmul(pl[:, 0:36], lhsT=h2T[:, c, :], rhs=WR[:, c, :], start=(c == 0), stop=(c == 7)), r=[h2Tn, "WR"], w=[pln])
                op("dve", lambda e: e.tensor_tensor(out=lg[:], in0=pl[:, 0:36], in1=bbc[:], op=ALU.add), r=[pln, "bbc"], w=[lgn])
                yield
                op("dve", lambda e: e.tensor_reduce(out=rs[:, 2:3], in_=lg[:, 0:4], axis=AX.X, op=ALU.max), r=[lgn], w=[rsn])
                op("dve", lambda e: e.tensor_scalar(out=ohg[:], in0=lg[:, 0:4], scalar1=rs[:, 2:3], scalar2=None, op0=ALU.is_ge), r=[lgn, rsn], w=[ohgn])
                yield
                op("dve", lambda e: e.tensor_scalar(out=rs[:, 3:4], in0=rs[:, 2:3], scalar1=-1.0, scalar2=None, op0=ALU.mult), r=[rsn], w=[rsn])
                op("act", lambda e: e.activation(out=ig[:, 0:4], in_=lg[:, 0:4], func=AF.Exp, bias=rs[:, 3:4], accum_out=rs[:, 4:5]), r=[lgn, rsn], w=[ign, rsn])
                yield
                op("dve", lambda e: e.reciprocal(out=rs[:, 5:6], in_=rs[:, 4:5]), r=[rsn], w=[rsn])
                op("dve", lambda e: e.tensor_tensor(out=tmp32[:].rearrange("p (g x) -> p g x", x=8), in0=lg[:, 4:36].rearrange("p (g x) -> p g x", x=8),
                                                    in1=ohg[:].unsqueeze(2).broadcast_to([128, 4, 8]), op=ALU.mult), r=[lgn, ohgn], w=[tmpn])
                yield
                op("dve", lambda e: e.tensor_reduce(out=ig[:], in_=tmp32[:].rearrange("p (g x) -> p x g", x=8), axis=AX.X, op=ALU.add), r=[tmpn, ign], w=[ign])
                op("dve", lambda e: e.max(out=m8[:], in_=ig[:]), r=[ign], w=[m8n])
                yield
                op("dve", lambda e: e.tensor_tensor(out=rs[:, 6:7], in0=m8[:, 1:2], in1=m8[:, 0:1], op=ALU.subtract), r=[m8n, rsn], w=[rsn])
                op("act", lambda e: e.activation(out=rs[:, 7:8], in_=rs[:, 6:7], func=AF.Exp), r=[rsn], w=[rsn])
                yield
                op("dve", lambda e: e.tensor_scalar(out=rs[:, 8:9], in0=rs[:, 7:8], scalar1=1.0, scalar2=None, op0=ALU.add), r=[rsn], w=[rsn])
                op("dve", lambda e: e.reciprocal(out=rs[:, 8:9], in_=rs[:, 8:9]), r=[rsn], w=[rsn])
                yield
                op("dve", lambda e: e.tensor_tensor(out=gates[:, ti, 0:1], in0=rs[:, 8:9], in1=rs[:, 5:6], op=ALU.mult), r=[rsn], w=["gates"])
                op("dve", lambda e: e.tensor_tensor(out=gates[:, ti, 1:2], in0=gates[:, ti, 0:1], in1=rs[:, 7:8], op=ALU.mult), r=[rsn, "gates"], w=["gates"])
                yield
                for kk, sel in ((0, sel1), (1, sel2)):
                    op("dve", lambda e, kk=kk: e.tensor_scalar(out=tmp32[:], in0=lg[:, 4:36], scalar1=m8[:, kk:kk + 1], scalar2=None, op0=ALU.is_equal), r=[lgn, m8n], w=[tmpn])
                    op("dve", lambda e, sel=sel: e.tensor_tensor(out=sel[:, ti, :].rearrange("p (g x) -> p g x", x=8), in0=tmp32[:].rearrange("p (g x) -> p g x", x=8),
                                                                in1=ohg[:].unsqueeze(2).broadcast_to([128, 4, 8]), op=ALU.mult), r=[tmpn, ohgn], w=["sel%d" % (kk + 1)])
                    yield
                op("dve", lambda e: e.tensor_tensor(out=A_b[:], in0=sel1[:, ti, :], in1=sel2[:, ti, :], op=ALU.add), r=["sel1", "sel2"], w=[Abn])
                pr_, prn_ = nextbank()
                op("pe", lambda e: e.matmul(pr_[:, 0:32], lhsT=CB["triu"][:], rhs=A_b[:], start=True, stop=False), r=[Abn, "kb_triu"], w=[prn_])
                op("pe", lambda e: e.matmul(pr_[:, 0:32], lhsT=CB["ones"][:], rhs=Asum[:], start=False, stop=True), r=["Asum", "kb_ones"], w=[prn_])
                op("act", lambda e: e.activation(out=rank[:, ti, :], in_=pr_[:, 0:32], func=AF.Copy), r=[prn_], w=["rank"])
                op("dve", lambda e: e.tensor_tensor(out=Asum[:], in0=Asum[:], in1=A_b[:], op=ALU.add), r=["Asum", Abn], w=["Asum"])
                yield

            def seq(*gens):
                for g_ in gens:
                    for _ in g_:
                        yield

            def rr_merge3(gens):
                gens = [g_ for g_ in gens if g_ is not None]
                while gens:
                    for g_ in list(gens):
                        try:
                            next(g_)
                        except StopIteration:
                            gens.remove(g_)

            NG3 = min(SO // 512, NG3_DEBUG)
            if NG3 > 0:
                for _ in mix_chain(0): pass
            for g in range(NG3):
                rr_merge3([mix_chain(g + 1) if g + 1 < NG3 else None,
                           seq(tile_chain(g, 0, 0), tile_chain(g, 2, 0)), seq(tile_chain(g, 1, 1), tile_chain(g, 3, 1))])
            sc.barrier()

    if stage >= 5:
        with ExitStack() as st:
            toki = sb(st, "mtoki", [128, NT], I32)
            sti = sb(st, "msti", [128, NSLOT // 128], I32)
            wi13i = sb(st, "mwi13i", [128, NBLK], I32)
            sidxA = sb(st, "msidxA", [128, NBLK * NS_], I32)
            tmp = ExitStack()
            cnt = sb(tmp, "mcnt", [128, 32], F32); nb_i = sb(tmp, "mnbi", [128, 32], I32); padc = sb(tmp, "mpadc", [128, 32], F32)
            pend = sb(tmp, "mpend", [128, 32], F32); pstart = sb(tmp, "mpstart", [128, 32], F32); z32 = sb(tmp, "mz32", [128, 32], F32)
            big = sb(tmp, "mbig", [128, NT, 32], F32); dd = sb(tmp, "mdd", [128, NT], F32)
            tokf = sb(tmp, "mtokf", [128, NT], F32)
            cmpb = sb(tmp, "mcmp", [128, NBLK, 32], F32); be = sb(tmp, "mbe", [128, NBLK], F32)
            wi13 = sb(tmp, "mwi13", [128, NBLK], F32)
            pc_, pcn_ = nextbank()
            op("pe", lambda e: e.matmul(pc_[:, 0:32], lhsT=CB["ones"][:], rhs=Asum[:], start=True, stop=True), r=["Asum", "kb_ones"], w=[pcn_])
            op("dve", lambda e: e.tensor_scalar(out=cnt[:], in0=pc_[:, 0:32], scalar1=float(BLK - 1), scalar2=float(1.0 / BLK), op0=ALU.add, op1=ALU.mult), r=[pcn_], w=["mcnt"])
            op("dve", lambda e: e.tensor_scalar(out=nb_i[:], in0=cnt[:], scalar1=-0.498, scalar2=None, op0=ALU.add), r=["mcnt"], w=["mnbi"])
            op("dve", lambda e: e.tensor_copy(out=padc[:], in_=nb_i[:]), r=["mnbi"], w=["mpadc"])
            op("dve", lambda e: e.tensor_scalar(out=padc[:], in0=padc[:], scalar1=float(BLK), scalar2=None, op0=ALU.mult), r=["mpadc"], w=["mpadc"])
            op("dve", lambda e: e.memset(z32[:], 0.0), w=["mz32"])
            op("dve", lambda e: e.tensor_tensor_scan(out=pend[:], data0=z32[:], data1=padc[:], initial=0.0, op0=ALU.add, op1=ALU.add), r=["mz32", "mpadc"], w=["mpend"])
            op("dve", lambda e: e.tensor_tensor(out=pstart[:], in0=pend[:], in1=padc[:], op=ALU.subtract), r=["mpend", "mpadc"], w=["mpstart"])
            for kk, (sel, di) in enumerate(((sel1, d1i), (sel2, d2i))):
                op("dve", lambda e: e.tensor_tensor(out=big[:], in0=rank[:], in1=pstart[:].unsqueeze(1).broadcast_to([128, NT, 32]), op=ALU.add), r=["rank", "mpstart"], w=["mbig"])
                op("dve", lambda e, sel=sel: e.tensor_tensor(out=big[:], in0=big[:], in1=sel[:], op=ALU.mult), r=["mbig", "sel%d" % (kk + 1)], w=["mbig"])
                op("dve", lambda e: e.tensor_reduce(out=dd[:], in_=big[:], axis=AX.X, op=ALU.add), r=["mbig"], w=["mdd"])
                op("dve", lambda e, di=di: e.tensor_copy(out=di[:], in_=dd[:]), r=["mdd"], w=["d%di" % (kk + 1)])
            for t in range(NT):
                op("dve", lambda e, t=t: e.tensor_scalar(out=tokf[:, t:t + 1], in0=C["piota"][:], scalar1=float(t * 128), scalar2=None, op0=ALU.add), r=["k_piota"], w=["mtokf"])
            op("dve", lambda e: e.tensor_copy(out=toki[:], in_=tokf[:]), r=["mtokf"], w=["mtoki"])
            op("dve", lambda e: e.memset(sti[:], SO), w=["msti"])
            op("sp", lambda e: e.dma_start(out=ST_d.rearrange("(p n) o -> p (n o)", p=128), in_=sti[:]), r=["msti"], w=["ST_d"], dsem=sc.dsem())
            NTd = min(NT, NG3_DEBUG * 4)
            fl = []
            for t in range(NTd):
                for di in (d1i, d2i):
                    fl.append(lambda e, t=t, di=di: e.indirect_dma_start(out=ST_d[:, :], out_offset=bass.IndirectOffsetOnAxis(ap=di[:, t:t + 1], axis=0),
                                                                         in_=toki[:, t:t + 1], in_offset=None))
            op("pool", fl, r=["d1i", "d2i", "mtoki", "ST_d"], w=["ST_d"], dsem=sc.dsem())
            op("dve", lambda e: e.tensor_tensor(out=cmpb[:], in0=pend[:].unsqueeze(1).broadcast_to([128, NBLK, 32]),
                                                in1=C["blkpos"][:].unsqueeze(2).broadcast_to([128, NBLK, 32]), op=ALU.is_le), r=["mpend", "k_blkpos"], w=["mcmp"])
            op("dve", lambda e: e.tensor_reduce(out=be[:], in_=cmpb[:], axis=AX.X, op=ALU.add), r=["mcmp"], w=["mbe"])
            op("dve", lambda e: e.tensor_scalar(out=wi13[:], in0=be[:], scalar1=128.0, scalar2=C["piota"][:, 0:1], op0=ALU.mult, op1=ALU.add), r=["mbe", "k_piota"], w=["mwi13"])
            op("dve", lambda e: e.tensor_copy(out=wi13i[:], in_=wi13[:]), r=["mwi13"], w=["mwi13i"])
            sc.barrier()
            tmp.close()
            w13f = I["w13"][0].rearrange("e (p c) n -> (e p) (c n)", c=8); w2f = I["w2"][0].rearrange("e (p j) n -> (e p) (j n)", j=4)
            op("sp", lambda e: e.dma_start(out=sidxA[:], in_=ST_d.rearrange("(b p s) o -> p b (s o)", p=128, s=NS_)), r=["ST_d"], w=["msidxA"], dsem=sc.dsem())
            bst = ExitStack()
            wf13L = [sb(bst, "mwf13_%d" % i, [128, 8, 1024], F32) for i in range(2)]; wf2L = [sb(bst, "mwf2_%d" % i, [128, 4, 1024], F32) for i in range(2)]
            wb13L = [sb(bst, "mwb13_%d" % i, [128, 8, 1024], BF16) for i in range(2)]; wb2L = [sb(bst, "mwb2_%d" % i, [128, 4, 1024], BF16) for i in range(2)]
            wsem13 = [sc.dsem() for _ in range(2)]; wsem2 = [sc.dsem() for _ in range(2)]
            for i_ in range(2):
                op("dve", lambda e, i_=i_: e.memset(wf13L[i_][:], 0.0), w=["mwf13_%d" % i_])
                op("dve", lambda e, i_=i_: e.memset(wf2L[i_][:], 0.0), w=["mwf2_%d" % i_])
            xg = [sb(bst, "mxg%d" % i, [128, NS_, 1024], BF16) for i in range(2)]; xgsem = [sc.dsem() for _ in range(2)]
            xgT = sb(bst, "mxgT", [128, 8, BLK], BF16)
            sg = sb(bst, "msg", [128, BLK], F32); actT = sb(bst, "mactT", [128, 4, BLK], BF16)
            yst2 = [sb(bst, "myst%d" % i, [128, 1024], BF16) for i in range(2)]; ysem2 = [sc.dsem() for _ in range(2)]
            ny = 0
            if NB_DEBUG < 1000:
                zf = sb(bst, "mzf", [128, 1024], BF16)
                op("dve", lambda e: e.memset(zf[:], 0.0), w=["mzf"])
                op("sp", [lambda e, t=t: e.dma_start(out=Y_d[t * 128:(t + 1) * 128, :], in_=zf[:]) for t in range(NSLOT // 128)], r=["mzf"], w=["Y_d"], dsem=sc.dsem())
            xgTL = [xgT, sb(bst, "mxgT1", [128, 8, BLK], BF16)]
            NBk = min(NBLK, NB_DEBUG)

            def issue_xg(blk):
                sl = blk % 2
                op("pool", [lambda e, s_=s_: e.indirect_dma_start(out=xg[sl][:, s_, :], out_offset=None, in_=H2_d[:, :],
                                                                  in_offset=bass.IndirectOffsetOnAxis(ap=sidxA[:, blk * NS_ + s_:blk * NS_ + s_ + 1], axis=0)) for s_ in range(NS_)],
                   r=["msidxA", "H2_d", "H2_d_pad"], w=["mxg%d" % sl], dsem=xgsem[sl])

            def issue_w(blk):
                sl = blk % 2
                wf13 = wf13L[sl]; wf2 = wf2L[sl]
                op("pool", lambda e: e.indirect_dma_start(out=wf13[:].rearrange("p c n -> p (c n)"), out_offset=None, in_=w13f,
                                                          in_offset=bass.IndirectOffsetOnAxis(ap=wi13i[:, blk:blk + 1], axis=0),
                                                          bounds_check=breg(e, 4095), oob_is_err=False),
                   r=["mwi13i"], w=["mwf13_%d" % sl], dsem=wsem13[sl])
                op("pool", lambda e: e.indirect_dma_start(out=wf2[:].rearrange("p j n -> p (j n)"), out_offset=None, in_=w2f,
                                                          in_offset=bass.IndirectOffsetOnAxis(ap=wi13i[:, blk:blk + 1], axis=0),
                                                          bounds_check=breg(e, 4095), oob_is_err=False),
                   r=["mwi13i"], w=["mwf2_%d" % sl], dsem=wsem2[sl])

            def stage_W(blk):
                sl = blk % 2
                wf13 = wf13L[sl]; wf2 = wf2L[sl]; wb13 = wb13L[sl]; wb2 = wb2L[sl]
                for c in range(8):
                    if c % 2 == 0:
                        op("act", lambda e, c=c: e.activation(out=wb13[:, c, :], in_=wf13[:, c, :], func=AF.Copy), r=["mwf13_%d" % sl], w=["mwb13s%d_%d" % (sl, c)])
                    else:
                        op("dve", lambda e, c=c: e.tensor_copy(out=wb13[:, c, :], in_=wf13[:, c, :]), r=["mwf13_%d" % sl], w=["mwb13s%d_%d" % (sl, c)])
                for j in range(4):
                    if j % 2 == 0:
                        op("act", lambda e, j=j: e.activation(out=wb2[:, j, :], in_=wf2[:, j, :], func=AF.Copy), r=["mwf2_%d" % sl], w=["mwb2s%d_%d" % (sl, j)])
                    else:
                        op("dve", lambda e, j=j: e.tensor_copy(out=wb2[:, j, :], in_=wf2[:, j, :]), r=["mwf2_%d" % sl], w=["mwb2s%d_%d" % (sl, j)])

            def stage_X(blk):
                sl = blk % 2
                xT_ = xgTL[sl]; xTn_ = "mxgT%d" % sl
                for s_ in range(NS_):
                    for half in range(2):
                        bank = PS[half]; bn = psn[half]
                        bv = bank[:].bitcast(BF16)
                        for c4 in range(4):
                            c = half * 4 + c4
                            op("pe", lambda e, c=c, c4=c4, s_=s_, bv=bv: e.transpose(out=bv[:, c4 * 128:(c4 + 1) * 128], in_=xg[sl][:, s_, :].rearrange("p (q c) -> p c q", c=8)[:, c, :], identity=CB["ident"][:]),
                               r=["mxg%d" % sl, "kb_ident"], w=[bn])
                        op("act", lambda e, half=half, s_=s_, bv=bv: e.activation(out=xT_[:, half * 4:half * 4 + 4, s_ * 128:(s_ + 1) * 128], in_=bv[:, 0:512].rearrange("p (c q) -> p c q", q=128), func=AF.Copy),
                           r=[bn], w=[xTn_])

            def stage_G(blk):
                sl = blk % 2
                xT_ = xgTL[sl]; xTn_ = "mxgT%d" % sl; wb13 = wb13L[sl]
                for m in range(4):
                    pg, pgn = nextbank(); pu, pun = nextbank()
                    for c in range(8):
                        op("pe", lambda e, c=c, m=m, pg=pg: e.matmul(pg[:, 0:BLK], lhsT=wb13[:, c, 0:512].rearrange("p (q j) -> p j q", j=4)[:, m, :], rhs=xT_[:, c, :], start=(c == 0), stop=(c == 7)),
                           r=[xTn_, "mwb13s%d_%d" % (sl, c)], w=[pgn])
                    for c in range(8):
                        op("pe", lambda e, c=c, m=m, pu=pu: e.matmul(pu[:, 0:BLK], lhsT=wb13[:, c, 512:1024].rearrange("p (q j) -> p j q", j=4)[:, m, :], rhs=xT_[:, c, :], start=(c == 0), stop=(c == 7)),
                           r=[xTn_, "mwb13s%d_%d" % (sl, c)], w=[pun])
                    op("act", lambda e, pg=pg: e.activation(out=sg[:], in_=pg[:, 0:BLK], func=AF.Silu), r=[pgn], w=["msg"])
                    op("dve", lambda e, m=m, pu=pu: e.tensor_tensor(out=actT[:, m, :], in0=sg[:], in1=pu[:, 0:BLK], op=ALU.mult), r=["msg", pun], w=["mactT"])

            nyc = {"n": 0}
            def stage_Y(blk):
                sl = blk % 2
                wb2 = wb2L[sl]
                for s_ in range(NS_):
                    ys = nyc["n"] % 2; nyc["n"] += 1
                    for nh in range(2):
                        py, pyn = nextbank()
                        for j in range(4):
                            op("pe", lambda e, j=j, s_=s_, nh=nh, py=py: e.matmul(py[:], lhsT=actT[:, j, s_ * 128:(s_ + 1) * 128], rhs=wb2[:, j, nh * 512:(nh + 1) * 512], start=(j == 0), stop=(j == 3)),
                               r=["mactT", "mwb2s%d_%d" % (sl, j)], w=[pyn])
                        op("act", lambda e, nh=nh, ys=ys, py=py: e.activation(out=yst2[ys][:, nh * 512:(nh + 1) * 512], in_=py[:], func=AF.Copy), r=[pyn], w=["myst%d" % ys])
                    op("sp", lambda e, s_=s_, ys=ys: e.dma_start(out=Y_d[blk * BLK:(blk + 1) * BLK, :].rearrange("(p s) d -> p s d", s=NS_)[:, s_, :], in_=yst2[ys][:]),
                       r=["myst%d" % ys], w=["Y_d"], dsem=ysem2[ys])

            if NBk > 0:
                issue_xg(0); issue_w(0)
                if NBk > 1: issue_w(1)
                stage_W(0); stage_X(0)
                if NBk > 1: issue_xg(1)
            for blk in range(NBk):
                if blk + 1 < NBk: stage_W(blk + 1)
                stage_G(blk)
                if blk + 1 < NBk: stage_X(blk + 1)
                stage_Y(blk)
                if blk + 2 < NBk:
                    issue_xg(blk + 2); issue_w(blk + 2)
            sc.barrier()
            bst.close()
            y1 = [sb(st, "my1_%d" % i, [128, 1024], BF16) for i in range(2)]; y2 = [sb(st, "my2_%d" % i, [128, 1024], BF16) for i in range(2)]
            xx = [sb(st, "mxx%d" % i, [128, 1024], F32) for i in range(2)]
            csem = [sc.dsem() for _ in range(2)]; c2sem = [sc.dsem() for _ in range(2)]; osem2 = [sc.dsem() for _ in range(2)]
            for t in range(NTd):
                sl = t % 2
                op("pool", [lambda e, t=t, sl=sl: e.indirect_dma_start(out=y1[sl][:], out_offset=None, in_=Y_d[:, :], in_offset=bass.IndirectOffsetOnAxis(ap=d1i[:, t:t + 1], axis=0)),
                            lambda e, t=t, sl=sl: e.indirect_dma_start(out=y2[sl][:], out_offset=None, in_=Y_d[:, :], in_offset=bass.IndirectOffsetOnAxis(ap=d2i[:, t:t + 1], axis=0))],
                   r=["Y_d", "d1i", "d2i"], w=["my1_%d" % sl, "my2_%d" % sl], dsem=csem[sl])
                op("sp", lambda e, t=t, sl=sl: e.dma_start(out=xx[sl][:], in_=X1_d[t * 128:(t + 1) * 128, :]), r=["X1_d"], w=["mxx%d" % sl], dsem=c2sem[sl])
                op("dve", lambda e, t=t, sl=sl: e.scalar_tensor_tensor(out=xx[sl][:], in0=y1[sl][:], scalar=gates[:, t, 0:1], in1=xx[sl][:], op0=ALU.mult, op1=ALU.add),
                   r=["my1_%d" % sl, "gates", "mxx%d" % sl], w=["mxx%d" % sl])
                op("dve", lambda e, t=t, sl=sl: e.scalar_tensor_tensor(out=xx[sl][:], in0=y2[sl][:], scalar=gates[:, t, 1:2], in1=xx[sl][:], op0=ALU.mult, op1=ALU.add),
                   r=["my2_%d" % sl, "gates", "mxx%d" % sl], w=["mxx%d" % sl])
                op("sp", lambda e, t=t, sl=sl: e.dma_start(out=out[t * 128:(t + 1) * 128, :], in_=xx[sl][:]), r=["mxx%d" % sl], w=["out"], dsem=osem2[sl])
            sc.barrier()

    sc.barrier()
    sc.emit()
    es.close()
    return nc


def make_in_maps(inputs):
    maps = []
    for core in range(8):
        b, par = core // 2, core % 2
        x = np.ascontiguousarray(inputs["x"][b])
        pos = np.ascontiguousarray(inputs["positions"][b]).astype(np.int32)
        own = np.arange(S).reshape(S // 128, 128)[par::2].reshape(-1)
        m = {"x_full": x, "x_own": np.ascontiguousarray(x[own]), "pos_full": pos.reshape(1, S),
             "pos_own": np.ascontiguousarray(pos[own]).reshape(1, SO)}
        for k in PARAMS:
            m[k] = np.ascontiguousarray(inputs[k]).astype(np.float32)
        for k, v in host_consts(par).items():
            m["c_" + k] = v
        maps.append(m)
    return maps


def kernel(**inputs):
    inputs = {k: np.asarray(v) for k, v in inputs.items()}
    nc = build()
    maps = make_in_maps(inputs)
    res = run_bass_kernel_spmd(nc, maps, core_ids=list(range(8)))
    outp = np.zeros((4, S, D), np.float32)
    for core in range(8):
        b, par = core // 2, core % 2
        own = np.arange(S).reshape(S // 128, 128)[par::2].reshape(-1)
        outp[b, own] = res.results[core]["out"]
    return outp
```
